# Optimizing a Trainium2 kernel written in Bass

```python
import math
import jax, jax.numpy as jnp
from jax import lax
import numpy as np

D_MODEL = 1024
BATCH = 4
SEQ = 4096
DEPTH = 4

N_EVEN = (DEPTH + 1) // 2
N_ODD = DEPTH // 2
EPS = 1e-6

W_A = 1024
S5_GROUP = 16
S5_GROUPS = W_A // S5_GROUP
S5_STATE = 64
S5_DT_MIN = 1e-3
S5_DT_MAX = 1e-1
W_B = 1024
CONV_K = 31
EV_INNER = W_A + W_B
EV_IN = 2 * W_A + 3 * W_B

GLA_HEADS = 4
GLA_DV = 2048
GLA_DK = GLA_DV // 2
GLA_HK = GLA_DK // GLA_HEADS
GLA_HV = GLA_DV // GLA_HEADS
GLA_LOWRANK = 16
GLA_TAU = 16.0
GLA_CHUNK = 64
OD_IN = 2 * GLA_DK + 2 * GLA_DV + GLA_LOWRANK

kernel_name = "hybrid_s5_conformer_gla_sandwich"


def rms_norm(x, g):
    xf = x.astype(jnp.float32)
    y = xf * lax.rsqrt(jnp.mean(xf * xf, axis=-1, keepdims=True) + EPS)
    return (y * g.astype(jnp.float32)).astype(x.dtype)


def layer_norm(x, g, b):
    xf = x.astype(jnp.float32)
    mu = jnp.mean(xf, axis=-1, keepdims=True)
    xc = xf - mu
    y = xc * lax.rsqrt(jnp.mean(xc * xc, axis=-1, keepdims=True) + EPS)
    return (y * g.astype(jnp.float32) + b.astype(jnp.float32)).astype(x.dtype)


def s5_layer(u, lam_re, lam_im, log_dt, b_re, b_im, c_re, c_im, d):
    f32 = jnp.float32
    bsz, seq, _ = u.shape
    uf = u.astype(f32).reshape(bsz, seq, S5_GROUPS, S5_GROUP)
    lam = lax.complex(lam_re.astype(f32), lam_im.astype(f32))
    dt = jnp.exp(log_dt.astype(f32))[:, None]
    lam_bar = jnp.exp(lam * dt)
    b_mat = lax.complex(b_re.astype(f32), b_im.astype(f32))
    b_bar = ((lam_bar - 1.0) / lam)[..., None] * b_mat
    bu = jnp.einsum('gnp,blgp->blgn', b_bar, uf)
    a = jnp.broadcast_to(lam_bar, (1, seq) + lam_bar.shape)

    def combine(e1, e2):
        a1, b1 = e1
        a2, b2 = e2
        return a2 * a1, a2 * b1 + b2

    _, states = lax.associative_scan(combine, (a, bu), axis=1)
    c_mat = lax.complex(c_re.astype(f32), c_im.astype(f32))
    y = jnp.einsum('gpn,blgn->blgp', c_mat, states).real
    y = y + d.astype(f32).reshape(S5_GROUPS, S5_GROUP) * uf
    return y.reshape(bsz, seq, W_A).astype(u.dtype)


def conformer_conv(val, gate, w_dw, b_dw, ln_g, ln_b, w_pw, b_pw):
    h = val * jax.nn.sigmoid(gate)
    h = lax.conv_general_dilated(
        h, w_dw[:, None, :].astype(h.dtype), window_strides=(1,),
        padding=((CONV_K - 1, 0),), dimension_numbers=('NWC', 'WIO', 'NWC'),
        feature_group_count=W_B) + b_dw
    h = jax.nn.silu(layer_norm(h, ln_g, ln_b))
    return h @ w_pw + b_pw


def gla_chunked(q, k, v, g):
    bsz, seq, nh, dk = q.shape
    dv = v.shape[-1]
    nc = seq // GLA_CHUNK

    def chunks(t):
        return t.reshape(bsz, nc, GLA_CHUNK, nh, t.shape[-1]).transpose(1, 0, 3, 2, 4)

    qc, kc, vc = chunks(q), chunks(k), chunks(v)
    bc = jnp.cumsum(chunks(g), axis=3)
    causal = jnp.tril(jnp.ones((GLA_CHUNK, GLA_CHUNK), dtype=bool))

    def step(state, inp):
        qi, ki, vi, bi = inp
        b_last = bi[:, :, -1:, :]
        q_dec = qi * jnp.exp(bi)
        attn = jnp.einsum('bhik,bhjk->bhij', q_dec, ki * jnp.exp(-bi))
        attn = jnp.where(causal, attn, 0.0)
        o = (jnp.einsum('bhij,bhjv->bhiv', attn, vi)
             + jnp.einsum('bhik,bhkv->bhiv', q_dec, state))
        state = (jnp.exp(b_last[:, :, 0, :, None]) * state
                 + jnp.einsum('bhjk,bhjv->bhkv', ki * jnp.exp(b_last - bi), vi))
        return state, o

    s0 = jnp.zeros((bsz, nh, dk, dv), jnp.float32)
    _, o = lax.scan(step, s0, (qc, kc, vc, bc))
    return o.transpose(1, 0, 3, 2, 4).reshape(bsz, seq, nh, dv)


def gla_branch(p, w_gate_up, b_gate, norm_g):
    f32 = jnp.float32
    bsz, seq, _ = p.shape
    q, k, v, r, lr = jnp.split(
        p, [GLA_DK, 2 * GLA_DK, 2 * GLA_DK + GLA_DV, 2 * GLA_DK + 2 * GLA_DV], axis=-1)
    g = jax.nn.log_sigmoid((lr @ w_gate_up + b_gate).astype(f32)) / GLA_TAU

    def heads(t, dh):
        return t.astype(f32).reshape(bsz, seq, GLA_HEADS, dh)

    o = gla_chunked(heads(q, GLA_HK) * (GLA_HK ** -0.5), heads(k, GLA_HK),
                    heads(v, GLA_HV), heads(g, GLA_HK))
    o = rms_norm(o, norm_g)
    return o.reshape(bsz, seq, GLA_DV).astype(p.dtype) * jax.nn.silu(r)


def setup_inputs(seed: int = 0) -> dict:
    key = jax.random.key(seed)
    ks = jax.random.split(key, 28)
    f32 = jnp.float32

    def nrm(k, shape, scale):
        return jax.random.normal(k, shape, f32) * scale

    NE, NO = N_EVEN, N_ODD
    n_idx = jnp.arange(S5_STATE, dtype=f32)
    return {
        "x": nrm(ks[0], (BATCH, SEQ, D_MODEL), 1.0),
        "norm_pre": 1.0 + nrm(ks[1], (DEPTH, D_MODEL), 0.05),
        "norm_post": 1.0 + nrm(ks[2], (DEPTH, D_MODEL), 0.05),
        "ev_w_in": nrm(ks[3], (NE, D_MODEL, EV_IN), D_MODEL ** -0.5),
        "s5_lambda_re": -0.5 + nrm(ks[4], (NE, S5_GROUPS, S5_STATE), 0.01),
        "s5_lambda_im": jnp.pi * n_idx + nrm(ks[5], (NE, S5_GROUPS, S5_STATE), 0.01),
        "s5_log_dt": jax.random.uniform(ks[6], (NE, S5_GROUPS), f32,
                                        minval=math.log(S5_DT_MIN), maxval=math.log(S5_DT_MAX)),
        "s5_b_re": nrm(ks[7], (NE, S5_GROUPS, S5_STATE, S5_GROUP), (2 * S5_GROUP) ** -0.5),
        "s5_b_im": nrm(ks[8], (NE, S5_GROUPS, S5_STATE, S5_GROUP), (2 * S5_GROUP) ** -0.5),
        "s5_c_re": nrm(ks[9], (NE, S5_GROUPS, S5_GROUP, S5_STATE), S5_STATE ** -0.5),
        "s5_c_im": nrm(ks[10], (NE, S5_GROUPS, S5_GROUP, S5_STATE), S5_STATE ** -0.5),
        "s5_d": nrm(ks[11], (NE, W_A), 1.0),
        "s5_w_glu": nrm(ks[12], (NE, W_A, W_A), W_A ** -0.5),
        "s5_b_glu": nrm(ks[13], (NE, W_A), 0.02),
        "conv_w": nrm(ks[14], (NE, CONV_K, W_B), CONV_K ** -0.5),
        "conv_b": nrm(ks[15], (NE, W_B), 0.02),
        "conv_ln_g": 1.0 + nrm(ks[16], (NE, W_B), 0.05),
        "conv_ln_b": nrm(ks[17], (NE, W_B), 0.02),
        "conv_w_pw": nrm(ks[18], (NE, W_B, W_B), W_B ** -0.5),
        "conv_b_pw": nrm(ks[19], (NE, W_B), 0.02),
        "ev_w_out": nrm(ks[20], (NE, EV_INNER, D_MODEL), EV_INNER ** -0.5),
        "od_w_in": nrm(ks[21], (NO, D_MODEL, OD_IN), D_MODEL ** -0.5),
        "gla_w_gate_up": nrm(ks[22], (NO, GLA_LOWRANK, GLA_DK), GLA_LOWRANK ** -0.5),
        "gla_b_gate": nrm(ks[23], (NO, GLA_DK), 0.1),
        "gla_norm_g": 1.0 + nrm(ks[24], (NO, GLA_HV), 0.05),
        "od_w_out": nrm(ks[25], (NO, GLA_DV, D_MODEL), GLA_DV ** -0.5),
    }


def reference(x, norm_pre, norm_post, ev_w_in, s5_lambda_re, s5_lambda_im, s5_log_dt,
              s5_b_re, s5_b_im, s5_c_re, s5_c_im, s5_d, s5_w_glu, s5_b_glu,
              conv_w, conv_b, conv_ln_g, conv_ln_b, conv_w_pw, conv_b_pw, ev_w_out,
              od_w_in, gla_w_gate_up, gla_b_gate, gla_norm_g, od_w_out):
    for i in range(DEPTH):
        u = rms_norm(x, norm_pre[i])
        if i % 2 == 0:
            j = i // 2
            p = u @ ev_w_in[j]
            a_in, a_z, b_val, b_gate, b_z = jnp.split(
                p, [W_A, 2 * W_A, 2 * W_A + W_B, 2 * W_A + 2 * W_B], axis=-1)
            ya = s5_layer(a_in, s5_lambda_re[j], s5_lambda_im[j], s5_log_dt[j],
                          s5_b_re[j], s5_b_im[j], s5_c_re[j], s5_c_im[j], s5_d[j])
            ya = jax.nn.gelu(ya)
            ya = ya * jax.nn.sigmoid(ya @ s5_w_glu[j] + s5_b_glu[j])
            ya = ya * jax.nn.silu(a_z)
            yb = conformer_conv(b_val, b_gate, conv_w[j], conv_b[j], conv_ln_g[j],
                                conv_ln_b[j], conv_w_pw[j], conv_b_pw[j])
            yb = yb * jax.nn.silu(b_z)
            y = jnp.concatenate([ya, yb], axis=-1) @ ev_w_out[j]
        else:
            j = i // 2
            p = u @ od_w_in[j]
            y = gla_branch(p, gla_w_gate_up[j], gla_b_gate[j], gla_norm_g[j]) @ od_w_out[j]
        x = x + rms_norm(y, norm_post[i])
    return x
```

```python
import math
import os
from contextlib import ExitStack

_DBG = int(os.environ.get("KDBG", "99"))
_SUB = int(os.environ.get("KSUB", "99"))
_VAR = os.environ.get("KVAR", "")

import numpy as np
import concourse.bass as bass
import concourse.mybir as mybir
from concourse.bass_utils import run_bass_kernel_spmd

F32 = mybir.dt.float32
BF16 = mybir.dt.bfloat16
ALU = mybir.AluOpType
AF = mybir.ActivationFunctionType

D = 1024
KC = 8
T = 512
EPS = 1e-6
CONV_K = 31
HALO = CONV_K - 1
PAD = 264
TWO_PI = 2.0 * math.pi


class Reg:
    __slots__ = ("name", "w", "r", "rd")

    def __init__(self, name):
        self.name = name
        self.w = None
        self.r = {}
        self.rd = []


class Op:
    __slots__ = ("eng", "fn", "deps", "need_inc", "val", "dsem", "is_dma")

    def __init__(self, eng, fn, dsem=None):
        self.eng = eng
        self.fn = fn
        self.deps = []
        self.need_inc = False
        self.val = 0
        self.dsem = dsem
        self.is_dma = dsem is not None


ENGS = ("pe", "act", "dve", "pool", "sp")


class Sched:
    def __init__(self):
        self.ops = {e: [] for e in ENGS}
        self.dsem_names = []
        self.pending = {}

    def fence(self, engs=("pe", "act", "dve", "pool")):
        last = [self.ops[e][-1] for e in engs if self.ops[e]]
        for e in engs:
            self.pending.setdefault(e, []).extend(last)

    def _add_dep(self, o, d):
        if d is None or d is o:
            return
        if (not d.is_dma) and d.eng == "pe" and o.eng == "pe" and not o.is_dma:
            return
        o.deps.append(d)

    def op(self, eng, fn, reads=(), writes=(), dsem=None):
        o = Op(eng, fn, dsem)
        if eng in self.pending and not o.is_dma:
            for d in self.pending.pop(eng):
                if d.eng != eng:
                    self._add_dep(o, d)
        for r in reads:
            self._add_dep(o, r.w)
        for w in writes:
            self._add_dep(o, w.w)
            for d in w.r.values():
                self._add_dep(o, d)
            for d in w.rd:
                self._add_dep(o, d)
        for r in reads:
            if o.is_dma:
                r.rd.append(o)
            else:
                r.r[eng] = o
        for w in writes:
            w.w = o
            w.r = {}
            w.rd = []
        if dsem is not None and dsem not in self.dsem_names:
            self.dsem_names.append(dsem)
        self.ops[eng].append(o)
        return o

    def emit(self, nc, final_waits):
        with ExitStack() as es:
            esem = {e: es.enter_context(nc.semaphore("s_" + e)) for e in ENGS}
            dsem = {n: es.enter_context(nc.semaphore("d_" + n)) for n in self.dsem_names}
            for e in ENGS:
                for o in self.ops[e]:
                    for d in o.deps:
                        if not d.is_dma:
                            d.need_inc = True
            for e in ENGS:
                c = 0
                for o in self.ops[e]:
                    if o.is_dma:
                        continue
                    if o.need_inc:
                        c += 1
                        o.val = c
            dcount = {n: 0 for n in self.dsem_names}
            for e in ENGS:
                for o in self.ops[e]:
                    if o.is_dma:
                        dcount[o.dsem] += 16
                        o.val = dcount[o.dsem]
            block = es.enter_context(nc.Block())

            def run(engname, eobj):
                waited = {}
                for o in self.ops[engname]:
                    need = {}
                    for d in o.deps:
                        s = dsem[d.dsem] if d.is_dma else esem[d.eng]
                        k = id(s)
                        if k not in need or need[k][1] < d.val:
                            need[k] = (s, d.val)
                    for k, (s, v) in need.items():
                        if waited.get(k, 0) < v:
                            eobj.wait_ge(s, v)
                            waited[k] = v
                    ins = o.fn(eobj)
                    if o.is_dma:
                        ins.then_inc(dsem[o.dsem], 16)
                    elif o.need_inc:
                        ins.then_inc(esem[engname], 1)
                if engname == "sp":
                    for n in final_waits:
                        eobj.wait_ge(dsem[n], dcount[n])

            @block.tensor
            def _(e):
                run("pe", e)

            @block.scalar
            def _(e):
                run("act", e)

            @block.vector
            def _(e):
                run("dve", e)

            @block.gpsimd
            def _(e):
                run("pool", e)

            @block.sync
            def _(e):
                run("sp", e)


class Buf:
    def __init__(self, t, name, nslots=0):
        self.t = t
        self.reg = Reg(name)
        self.slots = [Reg("%s.%d" % (name, i)) for i in range(nslots)]

    def __getitem__(self, k):
        return self.t[k]


def build_program(L, layers):
    assert L % T == 0
    NT = L // T
    n_e = sum(1 for k in layers if k == "e")
    n_o = sum(1 for k in layers if k == "o")
    nl = len(layers)
    nc = bass.Bass("TRN2", target_bir_lowering=False)
    S = Sched()

    def din(name, shape, dt=F32):
        return nc.dram_tensor(name, list(shape), dt, kind="ExternalInput").ap()

    xT_d = din("xT", [128, KC, L])
    yT_d = nc.dram_tensor("yT", [128, KC, L], F32, kind="ExternalOutput").ap()
    npre_d = din("npre", [128, max(nl, 1), KC])
    npost_d = din("npost", [128, max(nl, 1), KC])
    ident_d = din("ident", [128, 128])
    tri_d = din("tri", [128, 3, 128])
    ne1, no1 = max(n_e, 1), max(n_o, 1)
    evec_d = din("evec", [128, ne1, 6, KC])
    convw_d = din("convw", [128, ne1, KC, CONV_K])
    s5p_d = din("s5p", [128, ne1, 3, 32])
    cpad_d = din("cpad", [ne1, 128, 32 * 2 * 128])
    w_in_e = din("w_in_e", [ne1, 20, 128, 2048])
    bpad_d = din("bpad", [ne1, 8, 128, 1024])
    w_glu_d = din("w_glu", [ne1, 4, 128, 2048])
    w_pw_d = din("w_pw", [ne1, 4, 128, 2048])
    w_out_e = din("w_out_e", [ne1, 8, 128, 2048])
    w_in_o = din("w_in_o", [no1, 24, 128, 2048])
    w_out_o = din("w_out_o", [no1, 8, 128, 2048])
    wlr_d = din("wlr", [128, no1, KC, 16])
    wgaug_d = din("wgaug", [32, no1, 1024])
    gng_d = din("gng", [128, no1, 4])
    cprime_d = nc.dram_tensor("cprime", [ne1, 8, 128, 1024], F32, kind="ExternalOutput").ap()
    cprime_reg = [Reg("cprime%d" % j) for j in range(ne1)]
    NUQ = ne1 * (20 + 8 + 4 + 4 + 8) + no1 * (24 + 8)
    wq_d = nc.dram_tensor("wq", [NUQ, 128, 2048], BF16, kind="Internal").ap()
    wq_reg = [Reg("wq%d" % i) for i in range(NUQ)]

    es = ExitStack()

    def sb(name, shape, dt=F32, nslots=0):
        return Buf(es.enter_context(nc.sbuf_tensor(name, list(shape), dt)), name, nslots)

    xT = sb("xTs", [128, KC, T])
    un = sb("un", [128, KC, T], BF16)
    sq = un
    rstd = sb("rstd", [128, T])
    big32 = sb("big32", [128, KC, T], nslots=2)
    valb = acc = ybuf = big32
    inner = sb("inner", [128, 16, T], BF16, nslots=2)
    ones_m = sb("ones_m", [128, 2, 128], BF16)
    eps_col = sb("eps_col", [128, 2])
    ident = sb("identb", [128, 128], BF16)
    tri = sb("trib", [128, 3, 128], BF16)
    NW = 4
    wbf = sb("wbf", [128, NW, 2048], BF16, nslots=NW)
    sct = [sb("sct%d" % i, [128, PAD + T]) for i in range(2)]
    npre = sb("npre_s", [128, max(nl, 1), KC])
    npost = sb("npost_s", [128, max(nl, 1), KC])
    evec = sb("evec_s", [128, ne1, 6, KC])
    convw = sb("convw_s", [128, ne1, KC, CONV_K])
    halo = [sb("halo%d" % j, [128, KC, HALO], BF16) for j in range(ne1)]
    carry = [sb("carry%d" % j, [128, 32, 2]) for j in range(ne1)]
    pwr = [sb("pwr%d" % j, [128, 9, 32]) for j in range(ne1)]
    pwi = [sb("pwi%d" % j, [128, 9, 32]) for j in range(ne1)]
    npwi = [sb("npwi%d" % j, [128, 9, 32]) for j in range(ne1)]
    wlr = sb("wlr_s", [128, no1, KC, 16], BF16)
    wgaug = sb("wgaug_s", [32, no1, 1024], BF16)
    gng = sb("gng_s", [128, no1, 4])
    gs = sb("gs", [128, 8, 512])
    gsb = sb("gsb", [128, 8, 512], BF16)
    gstate_d = nc.dram_tensor("gstate", [no1, 128, 8 * 512], F32, kind="ExternalOutput").ap()
    gstate_reg = [Reg("gstate%d" % j) for j in range(no1)]
    G = [sb("G%d" % i, [128, KC, T], BF16) for i in range(7)]
    a_in, saz, sbz, accb, ya = G[0], G[1], G[2], G[3], G[4]
    qT, kT, ktok_b, vtok_b, rT_b = G[0], G[1], G[2], (G[3], G[4]), (G[5], G[6])

    def phase_alloc(specs):
        out = {}
        with ExitStack() as pes:
            for name, shape, dt in specs:
                out[name] = Buf(pes.enter_context(nc.sbuf_tensor(name, list(shape), dt)), name)
        return out

    EV = phase_alloc([
        ("hpad", [128, KC, HALO + T], BF16),
        ("tmpA", [128, T], F32), ("tmpB", [128, T], F32),
        ("hs0", [128, PAD + T], F32), ("hs1", [128, PAD + T], F32),
        ("hs2", [128, PAD + T], F32), ("hs3", [128, PAD + T], F32),
        ("sbre", [128, T], BF16), ("sbim", [128, T], BF16),
    ])
    hpad, tmpA, tmpB = EV["hpad"], EV["tmpA"], EV["tmpB"]
    hs = [EV["hs0"], EV["hs1"], EV["hs2"], EV["hs3"]]

    class _MeanView:
        reg = hs[2].reg

        def __getitem__(self, k):
            return hs[2][:, 0:T]
    mean = _MeanView()
    sbre, sbim = EV["sbre"], EV["sbim"]
    OD = phase_alloc([
        ("lraug", [32, T], BF16), ("sptok", [128, 1024], BF16), ("etmp", [128, 1024], F32),
        ("k2", [128, 1024], BF16), ("qdec", [128, KC, 128], BF16), ("kdec", [128, KC, 128], BF16),
        ("eb", [128, KC, 128], F32), ("einv", [128, KC, 128], F32), ("attn", [128, 4, 128], BF16),
        ("osb", [128, 4, 128], F32), ("osq", [128, 4, 128], BF16), ("orstd", [128, 128], F32),
        ("s5t", [128, 16, 32], F32), ("s5p_s", [128, ne1, 3, 32], F32),
        ("s5i", [128, 32], mybir.dt.int32),
    ])
    s5t, s5p, s5i = OD["s5t"], OD["s5p_s"], OD["s5i"]
    lraug, sptok, etmp, k2, qdec, kdec = (OD[k] for k in ("lraug", "sptok", "etmp", "k2", "qdec", "kdec"))
    eb, einv, attn, osb, osq, orstd = (OD[k] for k in ("eb", "einv", "attn", "osb", "osq", "orstd"))

    NPS = 8
    ps = [Buf(es.enter_context(nc.psum_tensor("ps%d" % i, [128, 512], F32)), "ps%d" % i)
          for i in range(NPS)]
    ps_i = [0]

    def psum():
        p = ps[ps_i[0] % NPS]
        ps_i[0] += 1
        return p

    uniq = [0]

    def dma(out_ap, in_ap, reads, writes, sem):
        if sem in ("c0", "c1"):
            sem = "c%d" % uniq[0]
            uniq[0] += 1
        return S.op("sp", lambda e: e.dma_start(out=out_ap, in_=in_ap), reads, writes, dsem=sem)

    def mm(out_ap, lhsT, rhs, start, stop, reads, writes):
        return S.op("pe", lambda e: e.matmul(out_ap, lhsT, rhs, start=start, stop=stop),
                    reads, writes)

    def act(out_ap, in_ap, func, reads, writes, bias=None, scale=None):
        kw = {}
        if bias is not None:
            kw["bias"] = bias
        if scale is not None:
            kw["scale"] = scale
        return S.op("act", lambda e: e.activation(out_ap, in_ap, func, **kw), reads, writes)

    def tt(eng, out_ap, a, b, op, reads, writes):
        return S.op(eng, lambda e: e.tensor_tensor(out_ap, a, b, op), reads, writes)

    def ts(eng, out_ap, a, s1, s2, op0, op1, reads, writes):
        if s2 is None:
            return S.op(eng, lambda e: e.tensor_scalar(out_ap, a, s1, None, op0), reads, writes)
        return S.op(eng, lambda e: e.tensor_scalar(out_ap, a, s1, s2, op0, op1), reads, writes)

    def stt(eng, out_ap, a, sc, b, op0, op1, reads, writes):
        eng = "dve"
        return S.op(eng, lambda e: e.scalar_tensor_tensor(out_ap, a, sc, b, op0, op1),
                    reads, writes)

    def cp(eng, out_ap, in_ap, reads, writes):
        if eng == "act":
            return S.op("act", lambda e: e.copy(out_ap, in_ap), reads, writes)
        return S.op(eng, lambda e: e.tensor_copy(out_ap, in_ap), reads, writes)

    def mset(eng, ap, v, writes):
        return S.op(eng, lambda e: e.memset(ap, v), (), writes)

    class WStream:
        def __init__(self):
            self.blocks = []
            self.n_dma = 0
            self.n_get = 0

        def push(self, u):
            self.blocks.append(u)

        def _dma_to(self, i):
            while self.n_dma <= i and self.n_dma < len(self.blocks):
                b = self.n_dma
                u = self.blocks[b]
                sl = b % NW
                dma(wbf[:, sl, :], wq_d[u, :, :], [wq_reg[u]], [wbf.slots[sl]], "w%d" % sl)
                self.n_dma += 1

        def get(self):
            i = self.n_get
            self.n_get += 1
            self._dma_to(i + NW - 1)
            return i % NW

    W = WStream()
    cast_rr = [0]

    def precast(u, parts, regs=()):
        k = cast_rr[0] % 2
        cast_rr[0] += 1
        st_ = bass.AP(big32.t, k * 2048, [[KC * T, 128], [1, 2048]])
        ob_ = bass.AP(inner.t, k * 2048, [[16 * T, 128], [1, 2048]])
        sreg, oreg = big32.slots[k], inner.slots[k]
        for ap, c0, n in parts:
            dma(bass.AP(big32.t, k * 2048 + c0, [[KC * T, 128], [1, n]]), ap, list(regs), [sreg], "pc%d" % k)
        eng = ("pool", "act", "dve")[cast_rr[0] % 3]
        cp(eng, ob_, st_, [sreg], [oreg])
        dma(wq_d[u, :, :], ob_, [oreg], [wq_reg[u]], "pw%d" % k)

    def load_small(dst, src_ap, sem="c0"):
        dma(dst.t[:], src_ap, (), [dst.reg], sem)

    load_small(npre, npre_d)
    load_small(npost, npost_d)
    st_i = bass.AP(big32.t, 0, [[KC * T, 128], [1, 128]])
    dma(st_i, ident_d, (), [big32.reg], "c0")
    cp("dve", ident[:, :], st_i, [big32.reg], [ident.reg])
    st_t = bass.AP(big32.t, 0, [[KC * T, 128], [1, 384]])
    dma(st_t, tri_d.rearrange("p a b -> p (a b)"), [], [big32.reg], "c0")
    cp("dve", bass.AP(tri.t, 0, [[384, 128], [1, 384]]), st_t, [big32.reg], [tri.reg])
    mset("dve", eps_col[:, :], EPS, [eps_col.reg])
    mset("dve", ones_m[:, 0, :], 1.0 / 1024.0, [ones_m.reg])
    mset("dve", ones_m[:, 1, :], 1.0 / 512.0, [ones_m.reg])
    if n_e:
        load_small(evec, evec_d)
        load_small(convw, convw_d)
        load_small(s5p, s5p_d)
        for j in range(n_e):
            mset("pool", halo[j][:, :, :], 0.0, [halo[j].reg])
            mset("pool", carry[j][:, :, :], 0.0, [carry[j].reg])
    if n_o:
        nw = no1 * KC * 16
        wl_f = bass.AP(big32.t, 0, [[KC * T, 128], [1, nw]])
        dma(wl_f, wlr_d.rearrange("p j k c -> p (j k c)"), [], [big32.reg], "c0")
        cp("dve", bass.AP(wlr.t, 0, [[nw, 128], [1, nw]]), wl_f, [big32.reg], [wlr.reg])
        ng = no1 * 1024
        wg_f = bass.AP(big32.t, 0, [[KC * T, 32], [1, ng]])
        dma(wg_f, wgaug_d.rearrange("p j c -> p (j c)"), [], [big32.reg], "c0")
        cp("dve", bass.AP(wgaug.t, 0, [[ng, 32], [1, ng]]), wg_f, [big32.reg], [wgaug.reg])
        load_small(gng, gng_d)

    def s5_prologue(j):
        R = [s5t.reg]
        st = lambda k: s5t[:, k, :]
        lre, lim, ldt = s5p[:, j, 0, :], s5p[:, j, 1, :], s5p[:, j, 2, :]
        act(st(0), ldt, AF.Exp, [s5p.reg], R)
        tt("dve", st(1), lre, st(0), ALU.mult, [s5p.reg] + R, R)
        tt("dve", st(2), lim, st(0), ALU.mult, [s5p.reg] + R, R)
        act(st(3), st(1), AF.Exp, R, R)
        def reduce_to_pi(shift):
            RI = R + [s5i.reg]
            ts("dve", st(10), st(2), shift, None, ALU.add, None, R, R)
            ts("dve", st(15), st(10), 1.0 / TWO_PI, None, ALU.mult, None, R, R)
            cp("dve", s5i[:, :], st(15), R, [s5i.reg])
            cp("dve", st(15), s5i[:, :], [s5i.reg], R)
            stt("dve", st(4), st(15), -TWO_PI, st(10), ALU.mult, ALU.add, RI, R)
            ts("dve", st(15), st(4), math.pi, TWO_PI, ALU.is_gt, ALU.mult, R, R)
            tt("dve", st(4), st(4), st(15), ALU.subtract, R, R)
            ts("dve", st(15), st(4), -math.pi, TWO_PI, ALU.is_lt, ALU.mult, R, R)
            tt("dve", st(4), st(4), st(15), ALU.add, R, R)

        reduce_to_pi(0.0)
        act(st(5), st(4), AF.Sin, R, R)
        reduce_to_pi(0.5 * math.pi)
        act(st(6), st(4), AF.Sin, R, R)
        tt("dve", st(7), st(3), st(6), ALU.mult, R, R)
        tt("dve", st(8), st(3), st(5), ALU.mult, R, R)
        P0 = [pwr[j].reg, pwi[j].reg, npwi[j].reg]
        cp("dve", pwr[j][:, 0, :], st(7), R, P0)
        cp("dve", pwi[j][:, 0, :], st(8), R, P0)
        for k in range(8):
            tt("dve", st(10), pwr[j][:, k, :], pwr[j][:, k, :], ALU.mult, P0 + R, R)
            tt("dve", st(15), pwi[j][:, k, :], pwi[j][:, k, :], ALU.mult, P0 + R, R)
            tt("dve", pwr[j][:, k + 1, :], st(10), st(15), ALU.subtract, R + P0, P0)
            tt("dve", st(10), pwr[j][:, k, :], pwi[j][:, k, :], ALU.mult, P0 + R, R)
            ts("dve", pwi[j][:, k + 1, :], st(10), 2.0, None, ALU.mult, None, R + P0, P0)
        ts("dve", npwi[j][:, :, :], pwi[j][:, :, :], -1.0, None, ALU.mult, None, P0, P0)
        ts("dve", st(7), st(7), -1.0, None, ALU.add, None, R, R)
        tt("dve", st(9), lre, lre, ALU.mult, [s5p.reg] + R, R)
        tt("dve", st(10), lim, lim, ALU.mult, [s5p.reg] + R, R)
        tt("dve", st(9), st(9), st(10), ALU.add, R, R)
        S.op("dve", lambda e: e.reciprocal(st(9), st(9)), R, R)
        tt("dve", st(11), st(7), lre, ALU.mult, [s5p.reg] + R, R)
        tt("dve", st(10), st(8), lim, ALU.mult, [s5p.reg] + R, R)
        tt("dve", st(11), st(11), st(10), ALU.add, R, R)
        tt("dve", st(12), st(8), lre, ALU.mult, [s5p.reg] + R, R)
        tt("dve", st(10), st(7), lim, ALU.mult, [s5p.reg] + R, R)
        tt("dve", st(12), st(12), st(10), ALU.subtract, R, R)
        tt("dve", st(13), st(11), st(9), ALU.mult, R, R)
        tt("dve", st(14), st(12), st(9), ALU.mult, R, R)
        ts("dve", st(11), st(13), -1.0, None, ALU.mult, None, R, R)
        ts("dve", st(12), st(14), -1.0, None, ALU.mult, None, R, R)
        for q in range(8):
            cq = big32
            dma(bass.AP(cq.t, 0, [[KC * T, 128], [1, 1024]]),
                cpad_d[j, :, q * 1024:(q + 1) * 1024], [], [cq.reg], "c0")
            dst = big32
            for pp in range(4):
                P = q * 4 + pp
                cre = bass.AP(cq.t, pp * 256, [[KC * T, 128], [1, 128]])
                cim = bass.AP(cq.t, pp * 256 + 128, [[KC * T, 128], [1, 128]])
                ore = bass.AP(dst.t, 2048 + pp * 256, [[KC * T, 128], [1, 128]])
                oim = bass.AP(dst.t, 2048 + pp * 256 + 128, [[KC * T, 128], [1, 128]])
                ts("dve", ore, cre, s5t[:, 13, P:P + 1], None, ALU.mult, None,
                   [cq.reg] + R, [dst.reg])
                stt("dve", ore, cim, s5t[:, 12, P:P + 1], ore, ALU.mult, ALU.add,
                    [cq.reg] + R, [dst.reg])
                ts("dve", oim, cre, s5t[:, 12, P:P + 1], None, ALU.mult, None,
                   [cq.reg] + R, [dst.reg])
                stt("dve", oim, cim, s5t[:, 11, P:P + 1], oim, ALU.mult, ALU.add,
                    [cq.reg] + R, [dst.reg])
            dma(cprime_d[j, q, :, :], bass.AP(dst.t, 2048, [[KC * T, 128], [1, 1024]]),
                [dst.reg], [cprime_reg[j]], "c1")

    for j in range(n_e):
        s5_prologue(j)

    def handoff(buf):
        S.op("dve", lambda e: e.memset(bass.AP(buf.t, 0, [[buf_stride[buf], 128], [1, 1]]), 0.0),
             (), [buf.reg] + buf.slots)
    buf_stride = {big32: KC * T, inner: 16 * T}
    handoff(big32)
    handoff(inner)
    for j in range(n_e):
        base = j * 44
        for b in range(20):
            precast(base + b, [(w_in_e[j, b, :, :], 0, 2048)])
        for q in range(8):
            precast(base + 20 + q, [(bpad_d[j, q, :, :], 0, 1024), (cprime_d[j, q, :, :], 1024, 1024)],
                    [cprime_reg[j]])
        for b in range(4):
            precast(base + 28 + b, [(w_glu_d[j, b, :, :], 0, 2048)])
        for b in range(4):
            precast(base + 32 + b, [(w_pw_d[j, b, :, :], 0, 2048)])
        for b in range(8):
            precast(base + 36 + b, [(w_out_e[j, b, :, :], 0, 2048)])
    for j in range(n_o):
        base = ne1 * 44 + j * 32
        for b in range(24):
            precast(base + b, [(w_in_o[j, b, :, :], 0, 2048)])
        for b in range(8):
            precast(base + 24 + b, [(w_out_o[j, b, :, :], 0, 2048)])
    handoff(big32)
    handoff(inner)

    def norm_stats(src_sq, nchunks, ones_idx, out_rstd, out_reg):
        p = psum()
        for kc in range(nchunks):
            mm(p[:, :], ones_m[:, ones_idx, :], src_sq[:, kc, :], kc == 0, kc == nchunks - 1,
               [ones_m.reg, src_sq.reg], [p.reg])
        rsqrt_eps(out_rstd, p[:, :], [p.reg], out_reg)

    def rsqrt_eps(out_ap, in_ap, reads, out_reg):
        act(out_ap, in_ap, AF.Ln, reads, [out_reg], bias=eps_col[:, 0:1])
        act(out_ap, out_ap, AF.Exp, [out_reg], [out_reg], scale=-0.5)

    def pre_norm(li):
        for kc in range(KC):
            act(sq[:, kc, :], xT[:, kc, :], AF.Square, [xT.reg], [sq.reg])
        norm_stats(sq, KC, 0, rstd[:, :], rstd.reg)
        for kc in range(KC):
            stt("dve", un[:, kc, :], xT[:, kc, :], npre[:, li, kc:kc + 1], rstd[:, :],
                ALU.mult, ALU.mult, [xT.reg, npre.reg, rstd.reg], [un.reg])

    def proj_fm(slot, m, src, nk, evac):
        p = psum()
        cw = 2048 // nk
        for kc in range(nk):
            mm(p[:, :], wbf[:, slot, kc * cw + m * 128: kc * cw + (m + 1) * 128], src[:, kc, :],
               kc == 0, kc == nk - 1, [wbf.slots[slot], src.reg], [p.reg])
        evac(p)

    def out_proj_and_residual(li):
        for b in range(8):
            slot = W.get()

            def ev(p, b=b):
                cp("act", ybuf[:, b, :], p[:, :], [p.reg], [ybuf.reg])
                act(sq[:, b, :], p[:, :], AF.Square, [p.reg], [sq.reg])
            proj_fm(slot, 0, inner, 16, ev)
        norm_stats(sq, KC, 0, rstd[:, :], rstd.reg)
        for kc in range(KC):
            stt("dve", ybuf[:, kc, :], ybuf[:, kc, :], npost[:, li, kc:kc + 1], rstd[:, :],
                ALU.mult, ALU.mult, [ybuf.reg, npost.reg, rstd.reg], [ybuf.reg])
            tt("pool", xT[:, kc, :], xT[:, kc, :], ybuf[:, kc, :], ALU.add,
               [xT.reg, ybuf.reg], [xT.reg])

    GELU_C = 2.0 * math.sqrt(2.0 / math.pi)

    def even_layer(li, j):
        S.fence()
        pre_norm(li)
        hp = hpad
        for i in range(4):
            mset("pool", hs[i][:, 0:PAD], 0.0, [hs[i].reg])
        cp("pool", hp[:, :, 0:HALO], halo[j][:, :, :], [halo[j].reg], [hp.reg])
        for b in range(20):
            slot = W.get()
            sec, bb = divmod(b, 4)
            for m in range(2):
                oc = bb * 2 + m
                if sec == 0:
                    ev = lambda p, oc=oc: cp("act", a_in[:, oc, :], p[:, :], [p.reg], [a_in.reg])
                elif sec == 1:
                    ev = lambda p, oc=oc: act(saz[:, oc, :], p[:, :], AF.Silu, [p.reg], [saz.reg])
                elif sec == 2:
                    ev = lambda p, oc=oc: cp("dve", valb[:, oc, :], p[:, :], [p.reg], [valb.reg])
                elif sec == 3:
                    def ev(p, oc=oc):
                        act(tmpA[:, :], p[:, :], AF.Sigmoid, [p.reg], [tmpA.reg])
                        tt("dve", hp[:, oc, HALO:HALO + T], valb[:, oc, :], tmpA[:, :], ALU.mult,
                           [valb.reg, tmpA.reg], [hp.reg])
                else:
                    ev = lambda p, oc=oc: act(sbz[:, oc, :], p[:, :], AF.Silu, [p.reg], [sbz.reg])
                proj_fm(slot, m, un, KC, ev)
        def conv_ops():
            for kc in range(KC):
                act(acc[:, kc, :], hp[:, kc, 0:T], AF.Identity, [hp.reg, convw.reg, evec.reg], [acc.reg],
                    bias=evec[:, j, 2, kc:kc + 1], scale=convw[:, j, kc, 0:1])
                yield
            n = 0
            for k in range(1, CONV_K):
                for kc in range(KC):
                    tmp = sct[n % 2]
                    n += 1
                    S.op("act", lambda e, o=tmp[:, 0:T], i=hp[:, kc, k:k + T], sc=convw[:, j, kc, k:k + 1]:
                         e.activation(o, i, AF.Copy, scale=sc), [hp.reg, convw.reg], [tmp.reg])
                    tt("pool", acc[:, kc, :], acc[:, kc, :], tmp[:, 0:T], ALU.add, [acc.reg, tmp.reg], [acc.reg])
                    yield
        conv_gen = conv_ops()

        def conv_slice(n):
            for _ in range(n):
                if next(conv_gen, "done") == "done":
                    return
        cp("act", halo[j][:, :, :], hp[:, :, T:T + HALO], [hp.reg], [halo[j].reg])
        bslot = {}
        cslot = {}

        def bu(P):
            q = P // 4
            if P % 4 == 0:
                bslot[q] = W.get()
            sl = bslot[q]
            kc = P // 4
            pr, pi_ = psum(), psum()
            off = (P % 4) * 256
            mm(pr[:, :], wbf[:, sl, off:off + 128], a_in[:, kc, :], True, True,
               [wbf.slots[sl], a_in.reg], [pr.reg])
            mm(pi_[:, :], wbf[:, sl, off + 128:off + 256], a_in[:, kc, :], True, True,
               [wbf.slots[sl], a_in.reg], [pi_.reg])
            cp("act", hs[0][:, PAD:PAD + T], pr[:, :], [pr.reg], [hs[0].reg])
            cp("act", hs[1][:, PAD:PAD + T], pi_[:, :], [pi_.reg], [hs[1].reg])
            cp("act", hs[0][:, PAD - 1:PAD], carry[j][:, P, 0:1], [carry[j].reg], [hs[0].reg])
            cp("act", hs[1][:, PAD - 1:PAD], carry[j][:, P, 1:2], [carry[j].reg], [hs[1].reg])

        def scan(P):
            A = (hs[0], hs[1])
            B = (hs[2], hs[3])
            lo, hi = PAD - 1, PAD + T
            for k in range(9):
                d = 1 << k
                rsh, ish = A[0][:, lo - d:hi - d], A[1][:, lo - d:hi - d]
                rd = [A[0].reg, A[1].reg, pwr[j].reg, pwi[j].reg, npwi[j].reg]
                pr_, pi_, npi_ = pwr[j][:, k, P:P + 1], pwi[j][:, k, P:P + 1], npwi[j][:, k, P:P + 1]
                stt("dve", B[0][:, lo:hi], rsh, pr_, A[0][:, lo:hi], ALU.mult, ALU.add, rd, [B[0].reg])
                stt("dve", B[0][:, lo:hi], ish, npi_, B[0][:, lo:hi], ALU.mult, ALU.add,
                    rd + [B[0].reg], [B[0].reg])
                stt("dve", B[1][:, lo:hi], rsh, pi_, A[1][:, lo:hi], ALU.mult, ALU.add, rd, [B[1].reg])
                stt("dve", B[1][:, lo:hi], ish, pr_, B[1][:, lo:hi], ALU.mult, ALU.add,
                    rd + [B[1].reg], [B[1].reg])
                A, B = B, A
            cp("act", sbre[:, :], A[0][:, PAD:PAD + T], [A[0].reg], [sbre.reg])
            cp("act", sbim[:, :], A[1][:, PAD:PAD + T], [A[1].reg], [sbim.reg])
            cp("act", carry[j][:, P, 0:1], A[0][:, PAD + T - 1:PAD + T], [A[0].reg], [carry[j].reg])
            cp("act", carry[j][:, P, 1:2], A[1][:, PAD + T - 1:PAD + T], [A[1].reg], [carry[j].reg])

        ypsum = {}

        def yout(P):
            sl = bslot[P // 4]
            kc = P // 4
            if P % 4 == 0:
                ypsum[kc] = psum()
            yp = ypsum[kc]
            off = 1024 + (P % 4) * 256
            mm(yp[:, :], wbf[:, sl, off:off + 128], sbre[:, :], P % 4 == 0, False,
               [wbf.slots[sl], sbre.reg], [yp.reg])
            mm(yp[:, :], wbf[:, sl, off + 128:off + 256], sbim[:, :], False, P % 4 == 3,
               [wbf.slots[sl], sbim.reg], [yp.reg])
            if P % 4 == 3:
                stt("dve", tmpA[:, :], a_in[:, kc, :], evec[:, j, 0, kc:kc + 1], yp[:, :],
                    ALU.mult, ALU.add, [a_in.reg, evec.reg, yp.reg], [tmpA.reg])
                tt("dve", tmpB[:, :], tmpA[:, :], tmpA[:, :], ALU.mult, [tmpA.reg], [tmpB.reg])
                ts("dve", tmpB[:, :], tmpB[:, :], 0.044715, 1.0, ALU.mult, ALU.add, [tmpB.reg], [tmpB.reg])
                tt("dve", tmpB[:, :], tmpB[:, :], tmpA[:, :], ALU.mult, [tmpA.reg, tmpB.reg], [tmpB.reg])
                act(tmpB[:, :], tmpB[:, :], AF.Sigmoid, [tmpB.reg], [tmpB.reg], scale=GELU_C)
                tt("dve", ya[:, kc, :], tmpA[:, :], tmpB[:, :], ALU.mult, [tmpA.reg, tmpB.reg], [ya.reg])

        for P in range(32):
            bu(P)
            scan(P)
            conv_slice(8)
            yout(P)
        conv_slice(10 ** 6)
        for b in range(4):
            slot = W.get()
            for m in range(2):
                oc = b * 2 + m

                def ev(p, oc=oc):
                    act(tmpA[:, :], p[:, :], AF.Sigmoid, [p.reg, evec.reg], [tmpA.reg],
                        bias=evec[:, j, 1, oc:oc + 1])
                    tt("dve", tmpA[:, :], tmpA[:, :], ya[:, oc, :], ALU.mult, [tmpA.reg, ya.reg], [tmpA.reg])
                    tt("dve", inner[:, oc, :], tmpA[:, :], saz[:, oc, :], ALU.mult,
                       [tmpA.reg, saz.reg], [inner.reg])
                proj_fm(slot, m, ya, KC, ev)
        for kc in range(KC):
            cp("act", accb[:, kc, :], acc[:, kc, :], [acc.reg], [accb.reg])
            act(sq[:, kc, :], acc[:, kc, :], AF.Square, [acc.reg], [sq.reg])
        pm = psum()
        for kc in range(KC):
            mm(pm[:, :], ones_m[:, 0, :], accb[:, kc, :], kc == 0, kc == KC - 1,
               [ones_m.reg, accb.reg], [pm.reg])
        cp("dve", mean[:, :], pm[:, :], [pm.reg], [mean.reg])
        p2 = psum()
        for kc in range(KC):
            mm(p2[:, :], ones_m[:, 0, :], sq[:, kc, :], kc == 0, kc == KC - 1,
               [ones_m.reg, sq.reg], [p2.reg])
        tt("dve", tmpA[:, :], mean[:, :], mean[:, :], ALU.mult, [mean.reg], [tmpA.reg])
        tt("dve", tmpA[:, :], p2[:, :], tmpA[:, :], ALU.subtract, [p2.reg, tmpA.reg], [tmpA.reg])
        rsqrt_eps(rstd[:, :], tmpA[:, :], [tmpA.reg], rstd.reg)
        for kc in range(KC):
            tt("dve", tmpB[:, :], acc[:, kc, :], mean[:, :], ALU.subtract, [acc.reg, mean.reg], [tmpB.reg])
            tt("dve", tmpB[:, :], tmpB[:, :], rstd[:, :], ALU.mult, [tmpB.reg, rstd.reg], [tmpB.reg])
            act(accb[:, kc, :], tmpB[:, :], AF.Silu, [tmpB.reg, evec.reg], [accb.reg],
                bias=evec[:, j, 4, kc:kc + 1], scale=evec[:, j, 3, kc:kc + 1])
        for b in range(4):
            slot = W.get()
            for m in range(2):
                oc = b * 2 + m

                def ev(p, oc=oc):
                    stt("dve", inner[:, 8 + oc, :], p[:, :], evec[:, j, 5, oc:oc + 1], sbz[:, oc, :],
                        ALU.add, ALU.mult, [p.reg, evec.reg, sbz.reg], [inner.reg])
                proj_fm(slot, m, accb, KC, ev)
        out_proj_and_residual(li)

    def ktok_ap(s, c0, n):
        return G[2][:, 2 * s + c0 // 512, c0 % 512:c0 % 512 + n]

    def vtok_ap(s, c0, n):
        return G[3 + s // 2][:, (s % 2) * 4 + c0 // 512, c0 % 512:c0 % 512 + n]

    def rT_ap(oc, sl=slice(0, T)):
        return G[5 + oc // 8][:, oc % 8, sl]

    vregs = [G[3].reg, G[4].reg]
    rregs = [G[5].reg, G[6].reg]

    def odd_layer(li, j, tile_idx):
        S.fence()
        pre_norm(li)
        mset("dve", lraug[:, :], 1.0, [lraug.reg])
        if tile_idx == 0:
            mset("pool", gs[:, :, :], 0.0, [gs.reg])
            mset("pool", gsb[:, :, :], 0.0, [gsb.reg])
        else:
            dma(bass.AP(gs.t, 0, [[8 * 512, 128], [1, 8 * 512]]), gstate_d[j, :, :],
                [gstate_reg[j]], [gs.reg], "g")
            for kc in range(8):
                cp("pool", gsb[:, kc, :], gs[:, kc, :], [gs.reg], [gsb.reg])
        if _DBG <= 1:
            return
        for b in range(24):
            slot = W.get()
            if b < 4:
                for m in range(2):
                    oc = b * 2 + m
                    proj_fm(slot, m, un, KC,
                            lambda p, oc=oc: cp("act", qT[:, oc, :], p[:, :], [p.reg], [qT.reg]))
            elif b < 8:
                bb = b - 4
                for m in range(2):
                    oc = bb * 2 + m
                    proj_fm(slot, m, un, KC,
                            lambda p, oc=oc: cp("act", kT[:, oc, :], p[:, :], [p.reg], [kT.reg]))
                for s in range(4):
                    p = psum()
                    for kc in range(KC):
                        mm(p[:, 0:256], un[:, kc, s * 128:(s + 1) * 128], wbf[:, slot, kc * 256:(kc + 1) * 256],
                           kc == 0, kc == KC - 1, [un.reg, wbf.slots[slot]], [p.reg])
                    cp("dve", ktok_ap(s, bb * 256, 256), p[:, 0:256], [p.reg], [G[2].reg])
            elif b < 16:
                bb = b - 8
                for s in range(4):
                    p = psum()
                    for kc in range(KC):
                        mm(p[:, 0:256], un[:, kc, s * 128:(s + 1) * 128], wbf[:, slot, kc * 256:(kc + 1) * 256],
                           kc == 0, kc == KC - 1, [un.reg, wbf.slots[slot]], [p.reg])
                    cp("act" if s % 2 else "dve", vtok_ap(s, bb * 256, 256), p[:, 0:256],
                       [p.reg], [G[3 + s // 2].reg])
            else:
                bb = b - 16
                for m in range(2):
                    oc = bb * 2 + m
                    proj_fm(slot, m, un, KC,
                            lambda p, oc=oc: act(rT_ap(oc), p[:, :], AF.Silu, [p.reg], [G[5 + oc // 8].reg]))
        if _DBG <= 2:
            return
        p = psum()
        for kc in range(KC):
            mm(p[0:16, :], wlr[:, j, kc, :], un[:, kc, :], kc == 0, kc == KC - 1,
               [wlr.reg, un.reg], [p.reg])
        cp("act", lraug[0:16, :], p[0:16, :], [p.reg], [lraug.reg])
        if _DBG <= 3:
            return
        for s in range(4):
            tsl = slice(s * 128, (s + 1) * 128)
            for h2 in range(2):
                p = psum()
                mm(p[:, :], lraug[:, tsl], wgaug[:, j, h2 * 512:(h2 + 1) * 512], True, True,
                   [lraug.reg, wgaug.reg], [p.reg])
                act(etmp[:, h2 * 512:(h2 + 1) * 512], p[:, :], AF.Exp, [p.reg], [etmp.reg], scale=-1.0)
            act(sptok[:, :], etmp[:, :], AF.Ln, [etmp.reg], [sptok.reg], bias=1.0)
            for g4 in range(2):
                p = psum()
                for c in range(4):
                    kc = g4 * 4 + c
                    mm(p[:, c * 128:(c + 1) * 128], sptok[:, kc * 128:(kc + 1) * 128], tri[:, 0, :],
                       True, True, [sptok.reg, tri.reg], [p.reg])
                ebv = bass.AP(eb.t, g4 * 512, [[KC * 128, 128], [1, 512]])
                eiv = bass.AP(einv.t, g4 * 512, [[KC * 128, 128], [1, 512]])
                act(ebv, p[:, :], AF.Exp, [p.reg], [eb.reg], scale=-1.0 / 16.0)
                act(eiv, p[:, :], AF.Exp, [p.reg], [einv.reg], scale=1.0 / 16.0)
            for kc in range(KC):
                stt("dve", qdec[:, kc, :], qT[:, kc, tsl], 1.0 / 16.0, eb[:, kc, :], ALU.mult, ALU.mult,
                    [qT.reg, eb.reg], [qdec.reg])
                tt("pool", kdec[:, kc, :], kT[:, kc, tsl], einv[:, kc, :], ALU.mult,
                   [kT.reg, einv.reg], [kdec.reg])
            for h2 in range(2):
                p = psum()
                mm(p[:, :], tri[:, 1, :], sptok[:, h2 * 512:(h2 + 1) * 512], True, True,
                   [tri.reg, sptok.reg], [p.reg])
                act(etmp[:, h2 * 512:(h2 + 1) * 512], p[:, :], AF.Exp, [p.reg], [etmp.reg], scale=-1.0 / 16.0)
            for h2 in range(2):
                tt("dve", k2[:, h2 * 512:(h2 + 1) * 512], ktok_ap(s, h2 * 512, 512),
                   etmp[:, h2 * 512:(h2 + 1) * 512], ALU.mult, [G[2].reg, etmp.reg], [k2.reg])
            if _DBG <= 4:
                continue
            pa = psum()
            for h in range(4):
                for c in range(2):
                    mm(pa[:, h * 128:(h + 1) * 128], kdec[:, 2 * h + c, :], qdec[:, 2 * h + c, :],
                       c == 0, c == 1, [kdec.reg, qdec.reg], [pa.reg])
            for h in range(4):
                tt("dve", attn[:, h, :], pa[:, h * 128:(h + 1) * 128], tri[:, 2, :], ALU.mult,
                   [pa.reg, tri.reg], [attn.reg])
            if _SUB <= 1:
                continue
            for h in range(4):
                po = psum()
                for vc in range(4):
                    vcol = h * 512 + vc * 128
                    mm(po[:, vc * 128:(vc + 1) * 128], vtok_ap(s, vcol, 128), attn[:, h, :],
                       True, "a" in _VAR, vregs + [attn.reg], [po.reg])
                    if "a" in _VAR:
                        continue
                    for c in range(2):
                        mm(po[:, vc * 128:(vc + 1) * 128], gsb[:, 2 * h + c, vc * 128:(vc + 1) * 128],
                           qdec[:, 2 * h + c, :], False, c == 1, [gsb.reg, qdec.reg], [po.reg])
                osb_v = bass.AP(osb.t, 0, [[512, 128], [1, 512]])
                osq_v = bass.AP(osq.t, 0, [[512, 128], [1, 512]])
                if "b" in _VAR:
                    continue
                cp("dve", osb_v, po[:, :], [po.reg], [osb.reg])
                if "c" in _VAR:
                    continue
                act(osq_v, osb_v, AF.Square, [osb.reg], [osq.reg])
                if _SUB <= 2:
                    continue
                pn = psum()
                for vc in range(4):
                    mm(pn[:, 0:128], ones_m[:, 1, :], osq[:, vc, :], vc == 0, vc == 3,
                       [ones_m.reg, osq.reg], [pn.reg])
                rsqrt_eps(orstd[:, :], pn[:, 0:128], [pn.reg], orstd.reg)
                for vc in range(4):
                    stt("dve", osb[:, vc, :], osb[:, vc, :], gng[:, j, vc:vc + 1], orstd[:, :],
                        ALU.mult, ALU.mult, [osb.reg, gng.reg, orstd.reg], [osb.reg])
                    tt("pool", inner[:, h * 4 + vc, tsl], osb[:, vc, :], rT_ap(h * 4 + vc, tsl), ALU.mult,
                       [osb.reg] + rregs, [inner.reg])
                if _SUB <= 3:
                    continue
                for c in range(2):
                    kc = 2 * h + c
                    pst = psum()
                    mm(pst[:, :], k2[:, kc * 128:(kc + 1) * 128], vtok_ap(s, h * 512, 512),
                       True, True, [k2.reg] + vregs, [pst.reg])
                    stt("dve", gs[:, kc, :], gs[:, kc, :], eb[:, kc, 127:128], pst[:, :],
                        ALU.mult, ALU.add, [gs.reg, eb.reg, pst.reg], [gs.reg])
                    cp("act", gsb[:, kc, :], gs[:, kc, :], [gs.reg], [gsb.reg])
        if _DBG <= 5:
            return
        dma(gstate_d[j, :, :], bass.AP(gs.t, 0, [[8 * 512, 128], [1, 8 * 512]]),
            [gs.reg], [gstate_reg[j]], "g")
        if _DBG <= 6:
            return
        out_proj_and_residual(li)

    for t in range(NT):
        ie = io = 0
        for kind in layers:
            if kind == "e":
                for b in range(44):
                    W.push(ie * 44 + b)
                ie += 1
            else:
                for b in range(32):
                    W.push(ne1 * 44 + io * 32 + b)
                io += 1

    for t in range(NT):
        for kc in range(KC):
            dma(xT[:, kc, :], xT_d[:, kc, t * T:(t + 1) * T], [], [xT.reg], "x")
        ie = io = 0
        for li, kind in enumerate(layers):
            if kind == "e":
                even_layer(li, ie)
                ie += 1
            else:
                odd_layer(li, io, t)
                io += 1
        for kc in range(KC):
            dma(yT_d[:, kc, t * T:(t + 1) * T], xT[:, kc, :], [xT.reg], [], "y")

    S.emit(nc, ["y"])
    es.close()
    return nc


def _wblocks(w, cw):
    din, dout = w.shape
    kc = din // 128
    nb = dout // cw
    a = w.reshape(kc, 128, nb, cw).transpose(2, 1, 0, 3)
    return np.ascontiguousarray(a.reshape(nb, 128, kc * cw))


def _vec(v):
    return np.ascontiguousarray(v.reshape(-1, 128).T)


def _pack(inp, layers):
    f = np.float32
    nl = len(layers)
    even = [i for i, k in enumerate(layers) if k == "e"]
    odd = [i for i, k in enumerate(layers) if k == "o"]
    ne1, no1 = max(len(even), 1), max(len(odd), 1)
    out = {}
    out["npre"] = np.ascontiguousarray(np.stack([_vec(inp["norm_pre"][i]) for i in range(max(nl, 1))], 1)).astype(f)
    out["npost"] = np.ascontiguousarray(np.stack([_vec(inp["norm_post"][i]) for i in range(max(nl, 1))], 1)).astype(f)
    out["ident"] = np.eye(128, dtype=f)
    jj, tt_ = np.meshgrid(np.arange(128), np.arange(128), indexing="ij")
    tri = np.zeros((128, 3, 128), f)
    tri[:, 0, :] = (jj <= tt_)
    tri[:, 1, :] = (jj > tt_)
    tri[:, 2, :] = (jj <= tt_)
    out["tri"] = tri
    evec = np.zeros((128, ne1, 6, KC), f)
    convw = np.zeros((128, ne1, KC, CONV_K), f)
    s5p = np.zeros((128, ne1, 3, 32), f)
    cpad = np.zeros((ne1, 128, 32, 2, 128), f)
    bpad = np.zeros((ne1, 8, 128, 4, 2, 128), f)
    w_in_e = np.zeros((ne1, 20, 128, 2048), f)
    w_glu = np.zeros((ne1, 4, 128, 2048), f)
    w_pw = np.zeros((ne1, 4, 128, 2048), f)
    w_out_e = np.zeros((ne1, 8, 128, 2048), f)
    for j in range(len(even)):
        for k, nm in enumerate(["s5_d", "s5_b_glu", "conv_b", "conv_ln_g", "conv_ln_b", "conv_b_pw"]):
            evec[:, j, k, :] = _vec(inp[nm][j])
        convw[:, j, :, :] = inp["conv_w"][j].T.reshape(KC, 128, CONV_K).transpose(1, 0, 2)
        for a in range(2):
            s5p[64 * a:64 * a + 64, j, 0, :] = inp["s5_lambda_re"][j][a::2, :].T
            s5p[64 * a:64 * a + 64, j, 1, :] = inp["s5_lambda_im"][j][a::2, :].T
            s5p[64 * a:64 * a + 64, j, 2, :] = inp["s5_log_dt"][j][a::2][None, :]
        for P in range(32):
            r = P % 4
            for a in range(2):
                g = 2 * P + a
                for ri, (bn, cn) in enumerate([("s5_b_re", "s5_c_re"), ("s5_b_im", "s5_c_im")]):
                    bpad[j, P // 4, 32 * r + 16 * a:32 * r + 16 * a + 16, P % 4, ri, 64 * a:64 * a + 64] = \
                        inp[bn][j, g].T
                    cpad[j, 64 * a:64 * a + 64, P, ri, 32 * r + 16 * a:32 * r + 16 * a + 16] = \
                        inp[cn][j, g].T
        w_in_e[j] = _wblocks(inp["ev_w_in"][j], 256)
        w_glu[j] = _wblocks(inp["s5_w_glu"][j], 256)
        w_pw[j] = _wblocks(inp["conv_w_pw"][j], 256)
        w_out_e[j] = _wblocks(inp["ev_w_out"][j], 128)
    out.update(evec=evec, convw=convw, s5p=s5p, cpad=cpad.reshape(ne1, 128, 32 * 2 * 128),
               bpad=bpad.reshape(ne1, 8, 128, 1024), w_in_e=w_in_e, w_glu=w_glu, w_pw=w_pw,
               w_out_e=w_out_e)
    w_in_o = np.zeros((no1, 24, 128, 2048), f)
    w_out_o = np.zeros((no1, 8, 128, 2048), f)
    wlr = np.zeros((128, no1, KC, 16), f)
    wgaug = np.zeros((32, no1, 1024), f)
    gng = np.zeros((128, no1, 4), f)
    for j in range(len(odd)):
        w = inp["od_w_in"][j]
        w_in_o[j] = _wblocks(np.ascontiguousarray(w[:, :6144]), 256)
        wlr[:, j, :, :] = w[:, 6144:6160].reshape(KC, 128, 16).transpose(1, 0, 2)
        wgaug[0:16, j, :] = inp["gla_w_gate_up"][j]
        wgaug[16, j, :] = inp["gla_b_gate"][j]
        gng[:, j, :] = _vec(inp["gla_norm_g"][j])
        w_out_o[j] = _wblocks(inp["od_w_out"][j], 128)
    out.update(w_in_o=w_in_o, w_out_o=w_out_o, wlr=wlr, wgaug=wgaug, gng=gng)
    return out


_LAYERS = ["e", "o", "e", "o"]


def run_layers(inp, layers, n_cores=8):
    inp = {k: np.asarray(v, dtype=np.float32) for k, v in inp.items()}
    x = inp["x"]
    B, L, _ = x.shape
    shared = _pack(inp, layers)
    nc = build_program(L, layers)
    in_maps = []
    for c in range(n_cores):
        b = c % B
        m = dict(shared)
        m["xT"] = np.ascontiguousarray(x[b].T.reshape(KC, 128, L).transpose(1, 0, 2))
        in_maps.append(m)
    res = run_bass_kernel_spmd(nc, in_maps, core_ids=list(range(n_cores)))
    out = np.empty((B, L, D), np.float32)
    for b in range(B):
        yT = np.asarray(res.results[b]["yT"])
        out[b] = yT.transpose(1, 0, 2).reshape(D, L).T
    return out


def kernel(**inputs):
    return run_layers(inputs, _LAYERS)
```

```python
import math
import os
from contextlib import ExitStack

_DBG = int(os.environ.get("KDBG", "99"))
_SUB = int(os.environ.get("KSUB", "99"))
_VAR = os.environ.get("KVAR", "")

import numpy as np
import concourse.bass as bass
import concourse.mybir as mybir
from concourse.bass_utils import run_bass_kernel_spmd

F32 = mybir.dt.float32
BF16 = mybir.dt.bfloat16
ALU = mybir.AluOpType
AF = mybir.ActivationFunctionType

D = 1024
KC = 8
T = 512
EPS = 1e-6
CONV_K = 31
HALO = CONV_K - 1
PAD = 264
TWO_PI = 2.0 * math.pi


class Reg:
    __slots__ = ("name", "w", "r", "rd")

    def __init__(self, name):
        self.name = name
        self.w = None
        self.r = {}
        self.rd = []


class Op:
    __slots__ = ("eng", "fn", "deps", "need_inc", "val", "dsem", "is_dma")

    def __init__(self, eng, fn, dsem=None):
        self.eng = eng
        self.fn = fn
        self.deps = []
        self.need_inc = False
        self.val = 0
        self.dsem = dsem
        self.is_dma = dsem is not None


ENGS = ("pe", "act", "dve", "pool", "sp")


class Sched:
    def __init__(self):
        self.ops = {e: [] for e in ENGS}
        self.dsem_names = []
        self.pending = {}

    def fence(self, engs=("pe", "act", "dve", "pool")):
        last = [self.ops[e][-1] for e in engs if self.ops[e]]
        for e in engs:
            self.pending.setdefault(e, []).extend(last)

    def _add_dep(self, o, d):
        if d is None or d is o:
            return
        if (not d.is_dma) and d.eng == "pe" and o.eng == "pe" and not o.is_dma:
            return
        o.deps.append(d)

    def op(self, eng, fn, reads=(), writes=(), dsem=None):
        o = Op(eng, fn, dsem)
        if eng in self.pending and not o.is_dma:
            for d in self.pending.pop(eng):
                if d.eng != eng:
                    self._add_dep(o, d)
        for r in reads:
            self._add_dep(o, r.w)
        for w in writes:
            self._add_dep(o, w.w)
            for d in w.r.values():
                self._add_dep(o, d)
            for d in w.rd:
                self._add_dep(o, d)
        for r in reads:
            if o.is_dma:
                r.rd.append(o)
            else:
                r.r[eng] = o
        for w in writes:
            w.w = o
            w.r = {}
            w.rd = []
        if dsem is not None and dsem not in self.dsem_names:
            self.dsem_names.append(dsem)
        self.ops[eng].append(o)
        return o

    def emit(self, nc, final_waits):
        with ExitStack() as es:
            esem = {e: es.enter_context(nc.semaphore("s_" + e)) for e in ENGS}
            dsem = {n: es.enter_context(nc.semaphore("d_" + n)) for n in self.dsem_names}
            for e in ENGS:
                for o in self.ops[e]:
                    for d in o.deps:
                        if not d.is_dma:
                            d.need_inc = True
            for e in ENGS:
                c = 0
                for o in self.ops[e]:
                    if o.is_dma:
                        continue
                    if o.need_inc:
                        c += 1
                        o.val = c
            dcount = {n: 0 for n in self.dsem_names}
            for e in ENGS:
                for o in self.ops[e]:
                    if o.is_dma:
                        dcount[o.dsem] += 16
                        o.val = dcount[o.dsem]
            block = es.enter_context(nc.Block())

            def run(engname, eobj):
                waited = {}
                for o in self.ops[engname]:
                    need = {}
                    for d in o.deps:
                        s = dsem[d.dsem] if d.is_dma else esem[d.eng]
                        k = id(s)
                        if k not in need or need[k][1] < d.val:
                            need[k] = (s, d.val)
                    for k, (s, v) in need.items():
                        if waited.get(k, 0) < v:
                            eobj.wait_ge(s, v)
                            waited[k] = v
                    ins = o.fn(eobj)
                    if o.is_dma:
                        ins.then_inc(dsem[o.dsem], 16)
                    elif o.need_inc:
                        ins.then_inc(esem[engname], 1)
                if engname == "sp":
                    for n in final_waits:
                        eobj.wait_ge(dsem[n], dcount[n])

            @block.tensor
            def _(e):
                run("pe", e)

            @block.scalar
            def _(e):
                run("act", e)

            @block.vector
            def _(e):
                run("dve", e)

            @block.gpsimd
            def _(e):
                run("pool", e)

            @block.sync
            def _(e):
                run("sp", e)


class Buf:
    def __init__(self, t, name, nslots=0):
        self.t = t
        self.reg = Reg(name)
        self.slots = [Reg("%s.%d" % (name, i)) for i in range(nslots)]

    def __getitem__(self, k):
        return self.t[k]


def build_program(L, layers):
    assert L % T == 0
    NT = L // T
    n_e = sum(1 for k in layers if k == "e")
    n_o = sum(1 for k in layers if k == "o")
    nl = len(layers)
    nc = bass.Bass("TRN2", target_bir_lowering=False)
    S = Sched()

    def din(name, shape, dt=F32):
        return nc.dram_tensor(name, list(shape), dt, kind="ExternalInput").ap()

    xT_d = din("xT", [128, KC, L])
    yT_d = nc.dram_tensor("yT", [128, KC, L], F32, kind="ExternalOutput").ap()
    npre_d = din("npre", [128, max(nl, 1), KC])
    npost_d = din("npost", [128, max(nl, 1), KC])
    ident_d = din("ident", [128, 128])
    tri_d = din("tri", [128, 3, 128])
    ne1, no1 = max(n_e, 1), max(n_o, 1)
    evec_d = din("evec", [128, ne1, 6, KC])
    convw_d = din("convw", [128, ne1, KC, CONV_K])
    s5p_d = din("s5p", [128, ne1, 3, 32])
    cpad_d = din("cpad", [ne1, 128, 32 * 2 * 128])
    w_in_e = din("w_in_e", [ne1, 20, 128, 2048])
    bpad_d = din("bpad", [ne1, 8, 128, 1024])
    w_glu_d = din("w_glu", [ne1, 4, 128, 2048])
    w_pw_d = din("w_pw", [ne1, 4, 128, 2048])
    w_out_e = din("w_out_e", [ne1, 8, 128, 2048])
    w_in_o = din("w_in_o", [no1, 24, 128, 2048])
    w_out_o = din("w_out_o", [no1, 8, 128, 2048])
    wlr_d = din("wlr", [128, no1, KC, 16])
    wgaug_d = din("wgaug", [32, no1, 1024])
    gng_d = din("gng", [128, no1, 4])
    cprime_d = nc.dram_tensor("cprime", [ne1, 8, 128, 1024], F32, kind="ExternalOutput").ap()
    cprime_reg = [Reg("cprime%d" % j) for j in range(ne1)]
    NUQ = ne1 * (20 + 8 + 4 + 4 + 8) + no1 * (24 + 8)
    wq_d = nc.dram_tensor("wq", [NUQ, 128, 2048], BF16, kind="Internal").ap()
    wq_reg = [Reg("wq%d" % i) for i in range(NUQ)]

    es = ExitStack()

    def sb(name, shape, dt=F32, nslots=0):
        return Buf(es.enter_context(nc.sbuf_tensor(name, list(shape), dt)), name, nslots)

    xT = sb("xTs", [128, KC, T])
    un = sb("un", [128, KC, T], BF16)
    sq = un
    rstd = sb("rstd", [128, T])
    big32 = sb("big32", [128, KC, T], nslots=2)
    valb = acc = ybuf = big32
    inner = sb("inner", [128, 16, T], BF16, nslots=2)
    ones_m = sb("ones_m", [128, 2, 128], BF16)
    eps_col = sb("eps_col", [128, 2])
    ident = sb("identb", [128, 128], BF16)
    tri = sb("trib", [128, 3, 128], BF16)
    NW = 4
    wbf = sb("wbf", [128, NW, 2048], BF16, nslots=NW)
    sct = [sb("sct%d" % i, [128, PAD + T]) for i in range(2)]
    npre = sb("npre_s", [128, max(nl, 1), KC])
    npost = sb("npost_s", [128, max(nl, 1), KC])
    evec = sb("evec_s", [128, ne1, 6, KC])
    convw = sb("convw_s", [128, ne1, KC, CONV_K])
    halo = [sb("halo%d" % j, [128, KC, HALO], BF16) for j in range(ne1)]
    carry = [sb("carry%d" % j, [128, 32, 2]) for j in range(ne1)]
    pwr = [sb("pwr%d" % j, [128, 9, 32]) for j in range(ne1)]
    pwi = [sb("pwi%d" % j, [128, 9, 32]) for j in range(ne1)]
    npwi = [sb("npwi%d" % j, [128, 9, 32]) for j in range(ne1)]
    wlr = sb("wlr_s", [128, no1, KC, 16], BF16)
    wgaug = sb("wgaug_s", [32, no1, 1024], BF16)
    gng = sb("gng_s", [128, no1, 4])
    gs = sb("gs", [128, 8, 512])
    gsb = sb("gsb", [128, 8, 512], BF16)
    gstate_d = nc.dram_tensor("gstate", [no1, 128, 8 * 512], F32, kind="ExternalOutput").ap()
    gstate_reg = [Reg("gstate%d" % j) for j in range(no1)]
    G = [sb("G%d" % i, [128, KC, T], BF16) for i in range(7)]
    a_in, saz, sbz, accb, ya = G[0], G[1], G[2], G[3], G[4]
    qT, kT, ktok_b, vtok_b, rT_b = G[0], G[1], G[2], (G[3], G[4]), (G[5], G[6])

    def phase_alloc(specs):
        out = {}
        with ExitStack() as pes:
            for name, shape, dt in specs:
                out[name] = Buf(pes.enter_context(nc.sbuf_tensor(name, list(shape), dt)), name)
        return out

    EV = phase_alloc([
        ("hpad", [128, KC, HALO + T], BF16),
        ("tmpA", [128, T], F32), ("tmpB", [128, T], F32),
        ("hs0", [128, PAD + T], F32), ("hs1", [128, PAD + T], F32),
        ("hs2", [128, PAD + T], F32), ("hs3", [128, PAD + T], F32),
        ("sbre", [128, T], BF16), ("sbim", [128, T], BF16),
    ])
    hpad, tmpA, tmpB = EV["hpad"], EV["tmpA"], EV["tmpB"]
    hs = [EV["hs0"], EV["hs1"], EV["hs2"], EV["hs3"]]

    class _MeanView:
        reg = hs[2].reg

        def __getitem__(self, k):
            return hs[2][:, 0:T]
    mean = _MeanView()
    sbre, sbim = EV["sbre"], EV["sbim"]
    OD = phase_alloc([
        ("lraug", [32, T], BF16), ("sptok", [128, 1024], BF16), ("etmp", [128, 1024], F32),
        ("k2", [128, 1024], BF16), ("qdec", [128, KC, 128], BF16), ("kdec", [128, KC, 128], BF16),
        ("eb", [128, KC, 128], F32), ("einv", [128, KC, 128], F32), ("attn", [128, 4, 128], BF16),
        ("osb", [128, 4, 128], F32), ("osq", [128, 4, 128], BF16), ("orstd", [128, 128], F32),
        ("s5t", [128, 16, 32], F32), ("s5p_s", [128, ne1, 3, 32], F32),
        ("s5i", [128, 32], mybir.dt.int32),
    ])
    s5t, s5p, s5i = OD["s5t"], OD["s5p_s"], OD["s5i"]
    lraug, sptok, etmp, k2, qdec, kdec = (OD[k] for k in ("lraug", "sptok", "etmp", "k2", "qdec", "kdec"))
    eb, einv, attn, osb, osq, orstd = (OD[k] for k in ("eb", "einv", "attn", "osb", "osq", "orstd"))

    NPS = 8
    ps = [Buf(es.enter_context(nc.psum_tensor("ps%d" % i, [128, 512], F32)), "ps%d" % i)
          for i in range(NPS)]
    ps_i = [0]

    def psum():
        p = ps[ps_i[0] % NPS]
        ps_i[0] += 1
        return p

    uniq = [0]

    def dma(out_ap, in_ap, reads, writes, sem):
        if sem in ("c0", "c1"):
            sem = "c%d" % uniq[0]
            uniq[0] += 1
        return S.op("sp", lambda e: e.dma_start(out=out_ap, in_=in_ap), reads, writes, dsem=sem)

    def mm(out_ap, lhsT, rhs, start, stop, reads, writes):
        return S.op("pe", lambda e: e.matmul(out_ap, lhsT, rhs, start=start, stop=stop),
                    reads, writes)

    def act(out_ap, in_ap, func, reads, writes, bias=None, scale=None):
        kw = {}
        if bias is not None:
            kw["bias"] = bias
        if scale is not None:
            kw["scale"] = scale
        return S.op("act", lambda e: e.activation(out_ap, in_ap, func, **kw), reads, writes)

    def tt(eng, out_ap, a, b, op, reads, writes):
        return S.op(eng, lambda e: e.tensor_tensor(out_ap, a, b, op), reads, writes)

    def ts(eng, out_ap, a, s1, s2, op0, op1, reads, writes):
        if s2 is None:
            return S.op(eng, lambda e: e.tensor_scalar(out_ap, a, s1, None, op0), reads, writes)
        return S.op(eng, lambda e: e.tensor_scalar(out_ap, a, s1, s2, op0, op1), reads, writes)

    def stt(eng, out_ap, a, sc, b, op0, op1, reads, writes):
        eng = "dve"
        return S.op(eng, lambda e: e.scalar_tensor_tensor(out_ap, a, sc, b, op0, op1),
                    reads, writes)

    def cp(eng, out_ap, in_ap, reads, writes):
        if eng == "act":
            return S.op("act", lambda e: e.copy(out_ap, in_ap), reads, writes)
        return S.op(eng, lambda e: e.tensor_copy(out_ap, in_ap), reads, writes)

    def mset(eng, ap, v, writes):
        return S.op(eng, lambda e: e.memset(ap, v), (), writes)

    class WStream:
        def __init__(self):
            self.blocks = []
            self.n_dma = 0
            self.n_get = 0

        def push(self, u):
            self.blocks.append(u)

        def _dma_to(self, i):
            while self.n_dma <= i and self.n_dma < len(self.blocks):
                b = self.n_dma
                u = self.blocks[b]
                sl = b % NW
                dma(wbf[:, sl, :], wq_d[u, :, :], [wq_reg[u]], [wbf.slots[sl]], "w%d" % sl)
                self.n_dma += 1

        def get(self):
            i = self.n_get
            self.n_get += 1
            self._dma_to(i + NW - 1)
            return i % NW

    W = WStream()
    cast_rr = [0]

    def precast(u, parts, regs=()):
        k = cast_rr[0] % 2
        cast_rr[0] += 1
        st_ = bass.AP(big32.t, k * 2048, [[KC * T, 128], [1, 2048]])
        ob_ = bass.AP(inner.t, k * 2048, [[16 * T, 128], [1, 2048]])
        sreg, oreg = big32.slots[k], inner.slots[k]
        for ap, c0, n in parts:
            dma(bass.AP(big32.t, k * 2048 + c0, [[KC * T, 128], [1, n]]), ap, list(regs), [sreg], "pc%d" % k)
        eng = ("pool", "act", "dve")[cast_rr[0] % 3]
        cp(eng, ob_, st_, [sreg], [oreg])
        dma(wq_d[u, :, :], ob_, [oreg], [wq_reg[u]], "pw%d" % k)

    def load_small(dst, src_ap, sem="c0"):
        dma(dst.t[:], src_ap, (), [dst.reg], sem)

    load_small(npre, npre_d)
    load_small(npost, npost_d)
    st_i = bass.AP(big32.t, 0, [[KC * T, 128], [1, 128]])
    dma(st_i, ident_d, (), [big32.reg], "c0")
    cp("dve", ident[:, :], st_i, [big32.reg], [ident.reg])
    st_t = bass.AP(big32.t, 0, [[KC * T, 128], [1, 384]])
    dma(st_t, tri_d.rearrange("p a b -> p (a b)"), [], [big32.reg], "c0")
    cp("dve", bass.AP(tri.t, 0, [[384, 128], [1, 384]]), st_t, [big32.reg], [tri.reg])
    mset("dve", eps_col[:, :], EPS, [eps_col.reg])
    mset("dve", ones_m[:, 0, :], 1.0 / 1024.0, [ones_m.reg])
    mset("dve", ones_m[:, 1, :], 1.0 / 512.0, [ones_m.reg])
    if n_e:
        load_small(evec, evec_d)
        load_small(convw, convw_d)
        load_small(s5p, s5p_d)
        for j in range(n_e):
            mset("pool", halo[j][:, :, :], 0.0, [halo[j].reg])
            mset("pool", carry[j][:, :, :], 0.0, [carry[j].reg])
    if n_o:
        nw = no1 * KC * 16
        wl_f = bass.AP(big32.t, 0, [[KC * T, 128], [1, nw]])
        dma(wl_f, wlr_d.rearrange("p j k c -> p (j k c)"), [], [big32.reg], "c0")
        cp("dve", bass.AP(wlr.t, 0, [[nw, 128], [1, nw]]), wl_f, [big32.reg], [wlr.reg])
        ng = no1 * 1024
        wg_f = bass.AP(big32.t, 0, [[KC * T, 32], [1, ng]])
        dma(wg_f, wgaug_d.rearrange("p j c -> p (j c)"), [], [big32.reg], "c0")
        cp("dve", bass.AP(wgaug.t, 0, [[ng, 32], [1, ng]]), wg_f, [big32.reg], [wgaug.reg])
        load_small(gng, gng_d)

    def s5_prologue(j):
        R = [s5t.reg]
        st = lambda k: s5t[:, k, :]
        lre, lim, ldt = s5p[:, j, 0, :], s5p[:, j, 1, :], s5p[:, j, 2, :]
        act(st(0), ldt, AF.Exp, [s5p.reg], R)
        tt("dve", st(1), lre, st(0), ALU.mult, [s5p.reg] + R, R)
        tt("dve", st(2), lim, st(0), ALU.mult, [s5p.reg] + R, R)
        act(st(3), st(1), AF.Exp, R, R)
        def reduce_to_pi(shift):
            RI = R + [s5i.reg]
            ts("dve", st(10), st(2), shift, None, ALU.add, None, R, R)
            ts("dve", st(15), st(10), 1.0 / TWO_PI, None, ALU.mult, None, R, R)
            cp("dve", s5i[:, :], st(15), R, [s5i.reg])
            cp("dve", st(15), s5i[:, :], [s5i.reg], R)
            stt("dve", st(4), st(15), -TWO_PI, st(10), ALU.mult, ALU.add, RI, R)
            ts("dve", st(15), st(4), math.pi, TWO_PI, ALU.is_gt, ALU.mult, R, R)
            tt("dve", st(4), st(4), st(15), ALU.subtract, R, R)
            ts("dve", st(15), st(4), -math.pi, TWO_PI, ALU.is_lt, ALU.mult, R, R)
            tt("dve", st(4), st(4), st(15), ALU.add, R, R)

        reduce_to_pi(0.0)
        act(st(5), st(4), AF.Sin, R, R)
        reduce_to_pi(0.5 * math.pi)
        act(st(6), st(4), AF.Sin, R, R)
        tt("dve", st(7), st(3), st(6), ALU.mult, R, R)
        tt("dve", st(8), st(3), st(5), ALU.mult, R, R)
        P0 = [pwr[j].reg, pwi[j].reg, npwi[j].reg]
        cp("dve", pwr[j][:, 0, :], st(7), R, P0)
        cp("dve", pwi[j][:, 0, :], st(8), R, P0)
        for k in range(8):
            tt("dve", st(10), pwr[j][:, k, :], pwr[j][:, k, :], ALU.mult, P0 + R, R)
            tt("dve", st(15), pwi[j][:, k, :], pwi[j][:, k, :], ALU.mult, P0 + R, R)
            tt("dve", pwr[j][:, k + 1, :], st(10), st(15), ALU.subtract, R + P0, P0)
            tt("dve", st(10), pwr[j][:, k, :], pwi[j][:, k, :], ALU.mult, P0 + R, R)
            ts("dve", pwi[j][:, k + 1, :], st(10), 2.0, None, ALU.mult, None, R + P0, P0)
        ts("dve", npwi[j][:, :, :], pwi[j][:, :, :], -1.0, None, ALU.mult, None, P0, P0)
        ts("dve", st(7), st(7), -1.0, None, ALU.add, None, R, R)
        tt("dve", st(9), lre, lre, ALU.mult, [s5p.reg] + R, R)
        tt("dve", st(10), lim, lim, ALU.mult, [s5p.reg] + R, R)
        tt("dve", st(9), st(9), st(10), ALU.add, R, R)
        S.op("dve", lambda e: e.reciprocal(st(9), st(9)), R, R)
        tt("dve", st(11), st(7), lre, ALU.mult, [s5p.reg] + R, R)
        tt("dve", st(10), st(8), lim, ALU.mult, [s5p.reg] + R, R)
        tt("dve", st(11), st(11), st(10), ALU.add, R, R)
        tt("dve", st(12), st(8), lre, ALU.mult, [s5p.reg] + R, R)
        tt("dve", st(10), st(7), lim, ALU.mult, [s5p.reg] + R, R)
        tt("dve", st(12), st(12), st(10), ALU.subtract, R, R)
        tt("dve", st(13), st(11), st(9), ALU.mult, R, R)
        tt("dve", st(14), st(12), st(9), ALU.mult, R, R)
        ts("dve", st(11), st(13), -1.0, None, ALU.mult, None, R, R)
        ts("dve", st(12), st(14), -1.0, None, ALU.mult, None, R, R)
        for q in range(8):
            cq = big32
            dma(bass.AP(cq.t, 0, [[KC * T, 128], [1, 1024]]),
                cpad_d[j, :, q * 1024:(q + 1) * 1024], [], [cq.reg], "c0")
            dst = big32
            for pp in range(4):
                P = q * 4 + pp
                cre = bass.AP(cq.t, pp * 256, [[KC * T, 128], [1, 128]])
                cim = bass.AP(cq.t, pp * 256 + 128, [[KC * T, 128], [1, 128]])
                ore = bass.AP(dst.t, 2048 + pp * 256, [[KC * T, 128], [1, 128]])
                oim = bass.AP(dst.t, 2048 + pp * 256 + 128, [[KC * T, 128], [1, 128]])
                ts("dve", ore, cre, s5t[:, 13, P:P + 1], None, ALU.mult, None,
                   [cq.reg] + R, [dst.reg])
                stt("dve", ore, cim, s5t[:, 12, P:P + 1], ore, ALU.mult, ALU.add,
                    [cq.reg] + R, [dst.reg])
                ts("dve", oim, cre, s5t[:, 12, P:P + 1], None, ALU.mult, None,
                   [cq.reg] + R, [dst.reg])
                stt("dve", oim, cim, s5t[:, 11, P:P + 1], oim, ALU.mult, ALU.add,
                    [cq.reg] + R, [dst.reg])
            dma(cprime_d[j, q, :, :], bass.AP(dst.t, 2048, [[KC * T, 128], [1, 1024]]),
                [dst.reg], [cprime_reg[j]], "c1")

    for j in range(n_e):
        s5_prologue(j)

    def handoff(buf):
        S.op("dve", lambda e: e.memset(bass.AP(buf.t, 0, [[buf_stride[buf], 128], [1, 1]]), 0.0),
             (), [buf.reg] + buf.slots)
    buf_stride = {big32: KC * T, inner: 16 * T}
    handoff(big32)
    handoff(inner)
    for j in range(n_e):
        base = j * 44
        for b in range(20):
            precast(base + b, [(w_in_e[j, b, :, :], 0, 2048)])
        for q in range(8):
            precast(base + 20 + q, [(bpad_d[j, q, :, :], 0, 1024), (cprime_d[j, q, :, :], 1024, 1024)],
                    [cprime_reg[j]])
        for b in range(4):
            precast(base + 28 + b, [(w_glu_d[j, b, :, :], 0, 2048)])
        for b in range(4):
            precast(base + 32 + b, [(w_pw_d[j, b, :, :], 0, 2048)])
        for b in range(8):
            precast(base + 36 + b, [(w_out_e[j, b, :, :], 0, 2048)])
    for j in range(n_o):
        base = ne1 * 44 + j * 32
        for b in range(24):
            precast(base + b, [(w_in_o[j, b, :, :], 0, 2048)])
        for b in range(8):
            precast(base + 24 + b, [(w_out_o[j, b, :, :], 0, 2048)])
    handoff(big32)
    handoff(inner)

    def norm_stats(src_sq, nchunks, ones_idx, out_rstd, out_reg):
        p = psum()
        for kc in range(nchunks):
            mm(p[:, :], ones_m[:, ones_idx, :], src_sq[:, kc, :], kc == 0, kc == nchunks - 1,
               [ones_m.reg, src_sq.reg], [p.reg])
        rsqrt_eps(out_rstd, p[:, :], [p.reg], out_reg)

    def rsqrt_eps(out_ap, in_ap, reads, out_reg):
        act(out_ap, in_ap, AF.Ln, reads, [out_reg], bias=eps_col[:, 0:1])
        act(out_ap, out_ap, AF.Exp, [out_reg], [out_reg], scale=-0.5)

    def pre_norm(li):
        for kc in range(KC):
            act(sq[:, kc, :], xT[:, kc, :], AF.Square, [xT.reg], [sq.reg])
        norm_stats(sq, KC, 0, rstd[:, :], rstd.reg)
        for kc in range(KC):
            stt("dve", un[:, kc, :], xT[:, kc, :], npre[:, li, kc:kc + 1], rstd[:, :],
                ALU.mult, ALU.mult, [xT.reg, npre.reg, rstd.reg], [un.reg])

    def proj_fm(slot, m, src, nk, evac):
        p = psum()
        cw = 2048 // nk
        for kc in range(nk):
            mm(p[:, :], wbf[:, slot, kc * cw + m * 128: kc * cw + (m + 1) * 128], src[:, kc, :],
               kc == 0, kc == nk - 1, [wbf.slots[slot], src.reg], [p.reg])
        evac(p)

    def out_proj_and_residual(li):
        for b in range(8):
            slot = W.get()

            def ev(p, b=b):
                cp("act", ybuf[:, b, :], p[:, :], [p.reg], [ybuf.reg])
                act(sq[:, b, :], p[:, :], AF.Square, [p.reg], [sq.reg])
            proj_fm(slot, 0, inner, 16, ev)
        norm_stats(sq, KC, 0, rstd[:, :], rstd.reg)
        for kc in range(KC):
            stt("dve", ybuf[:, kc, :], ybuf[:, kc, :], npost[:, li, kc:kc + 1], rstd[:, :],
                ALU.mult, ALU.mult, [ybuf.reg, npost.reg, rstd.reg], [ybuf.reg])
            tt("pool", xT[:, kc, :], xT[:, kc, :], ybuf[:, kc, :], ALU.add,
               [xT.reg, ybuf.reg], [xT.reg])

    GELU_C = 2.0 * math.sqrt(2.0 / math.pi)

    def even_layer(li, j):
        S.fence()
        pre_norm(li)
        hp = hpad
        for i in range(4):
            mset("pool", hs[i][:, 0:PAD], 0.0, [hs[i].reg])
        cp("pool", hp[:, :, 0:HALO], halo[j][:, :, :], [halo[j].reg], [hp.reg])
        for b in range(20):
            slot = W.get()
            sec, bb = divmod(b, 4)
            for m in range(2):
                oc = bb * 2 + m
                if sec == 0:
                    ev = lambda p, oc=oc: cp("act", a_in[:, oc, :], p[:, :], [p.reg], [a_in.reg])
                elif sec == 1:
                    ev = lambda p, oc=oc: act(saz[:, oc, :], p[:, :], AF.Silu, [p.reg], [saz.reg])
                elif sec == 2:
                    ev = lambda p, oc=oc: cp("dve", valb[:, oc, :], p[:, :], [p.reg], [valb.reg])
                elif sec == 3:
                    def ev(p, oc=oc):
                        act(tmpA[:, :], p[:, :], AF.Sigmoid, [p.reg], [tmpA.reg])
                        tt("dve", hp[:, oc, HALO:HALO + T], valb[:, oc, :], tmpA[:, :], ALU.mult,
                           [valb.reg, tmpA.reg], [hp.reg])
                else:
                    ev = lambda p, oc=oc: act(sbz[:, oc, :], p[:, :], AF.Silu, [p.reg], [sbz.reg])
                proj_fm(slot, m, un, KC, ev)
        def conv_ops():
            for kc in range(KC):
                act(acc[:, kc, :], hp[:, kc, 0:T], AF.Identity, [hp.reg, convw.reg, evec.reg], [acc.reg],
                    bias=evec[:, j, 2, kc:kc + 1], scale=convw[:, j, kc, 0:1])
                yield
            n = 0
            for k in range(1, CONV_K):
                for kc in range(KC):
                    tmp = sct[n % 2]
                    n += 1
                    S.op("act", lambda e, o=tmp[:, 0:T], i=hp[:, kc, k:k + T], sc=convw[:, j, kc, k:k + 1]:
                         e.activation(o, i, AF.Copy, scale=sc), [hp.reg, convw.reg], [tmp.reg])
                    tt("pool", acc[:, kc, :], acc[:, kc, :], tmp[:, 0:T], ALU.add, [acc.reg, tmp.reg], [acc.reg])
                    yield
        conv_gen = conv_ops()

        def conv_slice(n):
            for _ in range(n):
                if next(conv_gen, "done") == "done":
                    return
        cp("act", halo[j][:, :, :], hp[:, :, T:T + HALO], [hp.reg], [halo[j].reg])
        bslot = {}
        cslot = {}

        def bu(P):
            q = P // 4
            if P % 4 == 0:
                bslot[q] = W.get()
            sl = bslot[q]
            kc = P // 4
            pr, pi_ = psum(), psum()
            off = (P % 4) * 256
            mm(pr[:, :], wbf[:, sl, off:off + 128], a_in[:, kc, :], True, True,
               [wbf.slots[sl], a_in.reg], [pr.reg])
            mm(pi_[:, :], wbf[:, sl, off + 128:off + 256], a_in[:, kc, :], True, True,
               [wbf.slots[sl], a_in.reg], [pi_.reg])
            cp("act", hs[0][:, PAD:PAD + T], pr[:, :], [pr.reg], [hs[0].reg])
            cp("act", hs[1][:, PAD:PAD + T], pi_[:, :], [pi_.reg], [hs[1].reg])
            cp("act", hs[0][:, PAD - 1:PAD], carry[j][:, P, 0:1], [carry[j].reg], [hs[0].reg])
            cp("act", hs[1][:, PAD - 1:PAD], carry[j][:, P, 1:2], [carry[j].reg], [hs[1].reg])

        def scan(P):
            A = (hs[0], hs[1])
            B = (hs[2], hs[3])
            lo, hi = PAD - 1, PAD + T
            for k in range(9):
                d = 1 << k
                rsh, ish = A[0][:, lo - d:hi - d], A[1][:, lo - d:hi - d]
                rd = [A[0].reg, A[1].reg, pwr[j].reg, pwi[j].reg, npwi[j].reg]
                pr_, pi_, npi_ = pwr[j][:, k, P:P + 1], pwi[j][:, k, P:P + 1], npwi[j][:, k, P:P + 1]
                stt("dve", B[0][:, lo:hi], rsh, pr_, A[0][:, lo:hi], ALU.mult, ALU.add, rd, [B[0].reg])
                stt("dve", B[0][:, lo:hi], ish, npi_, B[0][:, lo:hi], ALU.mult, ALU.add,
                    rd + [B[0].reg], [B[0].reg])
                stt("dve", B[1][:, lo:hi], rsh, pi_, A[1][:, lo:hi], ALU.mult, ALU.add, rd, [B[1].reg])
                stt("dve", B[1][:, lo:hi], ish, pr_, B[1][:, lo:hi], ALU.mult, ALU.add,
                    rd + [B[1].reg], [B[1].reg])
                A, B = B, A
            cp("act", sbre[:, :], A[0][:, PAD:PAD + T], [A[0].reg], [sbre.reg])
            cp("act", sbim[:, :], A[1][:, PAD:PAD + T], [A[1].reg], [sbim.reg])
            cp("act", carry[j][:, P, 0:1], A[0][:, PAD + T - 1:PAD + T], [A[0].reg], [carry[j].reg])
            cp("act", carry[j][:, P, 1:2], A[1][:, PAD + T - 1:PAD + T], [A[1].reg], [carry[j].reg])

        ypsum = {}

        def yout(P):
            sl = bslot[P // 4]
            kc = P // 4
            if P % 4 == 0:
                ypsum[kc] = psum()
            yp = ypsum[kc]
            off = 1024 + (P % 4) * 256
            mm(yp[:, :], wbf[:, sl, off:off + 128], sbre[:, :], P % 4 == 0, False,
               [wbf.slots[sl], sbre.reg], [yp.reg])
            mm(yp[:, :], wbf[:, sl, off + 128:off + 256], sbim[:, :], False, P % 4 == 3,
               [wbf.slots[sl], sbim.reg], [yp.reg])
            if P % 4 == 3:
                stt("dve", tmpA[:, :], a_in[:, kc, :], evec[:, j, 0, kc:kc + 1], yp[:, :],
                    ALU.mult, ALU.add, [a_in.reg, evec.reg, yp.reg], [tmpA.reg])
                tt("dve", tmpB[:, :], tmpA[:, :], tmpA[:, :], ALU.mult, [tmpA.reg], [tmpB.reg])
                ts("dve", tmpB[:, :], tmpB[:, :], 0.044715, 1.0, ALU.mult, ALU.add, [tmpB.reg], [tmpB.reg])
                tt("dve", tmpB[:, :], tmpB[:, :], tmpA[:, :], ALU.mult, [tmpA.reg, tmpB.reg], [tmpB.reg])
                act(tmpB[:, :], tmpB[:, :], AF.Sigmoid, [tmpB.reg], [tmpB.reg], scale=GELU_C)
                tt("dve", ya[:, kc, :], tmpA[:, :], tmpB[:, :], ALU.mult, [tmpA.reg, tmpB.reg], [ya.reg])

        for P in range(32):
            bu(P)
            conv_slice(8)
            scan(P)
            yout(P)
        conv_slice(10 ** 6)
        for b in range(4):
            slot = W.get()
            for m in range(2):
                oc = b * 2 + m

                def ev(p, oc=oc):
                    act(tmpA[:, :], p[:, :], AF.Sigmoid, [p.reg, evec.reg], [tmpA.reg],
                        bias=evec[:, j, 1, oc:oc + 1])
                    tt("dve", tmpA[:, :], tmpA[:, :], ya[:, oc, :], ALU.mult, [tmpA.reg, ya.reg], [tmpA.reg])
                    tt("dve", inner[:, oc, :], tmpA[:, :], saz[:, oc, :], ALU.mult,
                       [tmpA.reg, saz.reg], [inner.reg])
                proj_fm(slot, m, ya, KC, ev)
        for kc in range(KC):
            cp("act", accb[:, kc, :], acc[:, kc, :], [acc.reg], [accb.reg])
            act(sq[:, kc, :], acc[:, kc, :], AF.Square, [acc.reg], [sq.reg])
        pm = psum()
        for kc in range(KC):
            mm(pm[:, :], ones_m[:, 0, :], accb[:, kc, :], kc == 0, kc == KC - 1,
               [ones_m.reg, accb.reg], [pm.reg])
        cp("dve", mean[:, :], pm[:, :], [pm.reg], [mean.reg])
        p2 = psum()
        for kc in range(KC):
            mm(p2[:, :], ones_m[:, 0, :], sq[:, kc, :], kc == 0, kc == KC - 1,
               [ones_m.reg, sq.reg], [p2.reg])
        tt("dve", tmpA[:, :], mean[:, :], mean[:, :], ALU.mult, [mean.reg], [tmpA.reg])
        tt("dve", tmpA[:, :], p2[:, :], tmpA[:, :], ALU.subtract, [p2.reg, tmpA.reg], [tmpA.reg])
        rsqrt_eps(rstd[:, :], tmpA[:, :], [tmpA.reg], rstd.reg)
        for kc in range(KC):
            tt("dve", tmpB[:, :], acc[:, kc, :], mean[:, :], ALU.subtract, [acc.reg, mean.reg], [tmpB.reg])
            tt("dve", tmpB[:, :], tmpB[:, :], rstd[:, :], ALU.mult, [tmpB.reg, rstd.reg], [tmpB.reg])
            act(accb[:, kc, :], tmpB[:, :], AF.Silu, [tmpB.reg, evec.reg], [accb.reg],
                bias=evec[:, j, 4, kc:kc + 1], scale=evec[:, j, 3, kc:kc + 1])
        for b in range(4):
            slot = W.get()
            for m in range(2):
                oc = b * 2 + m

                def ev(p, oc=oc):
                    stt("dve", inner[:, 8 + oc, :], p[:, :], evec[:, j, 5, oc:oc + 1], sbz[:, oc, :],
                        ALU.add, ALU.mult, [p.reg, evec.reg, sbz.reg], [inner.reg])
                proj_fm(slot, m, accb, KC, ev)
        out_proj_and_residual(li)

    def ktok_ap(s, c0, n):
        return G[2][:, 2 * s + c0 // 512, c0 % 512:c0 % 512 + n]

    def vtok_ap(s, c0, n):
        return G[3 + s // 2][:, (s % 2) * 4 + c0 // 512, c0 % 512:c0 % 512 + n]

    def rT_ap(oc, sl=slice(0, T)):
        return G[5 + oc // 8][:, oc % 8, sl]

    vregs = [G[3].reg, G[4].reg]
    rregs = [G[5].reg, G[6].reg]

    def odd_layer(li, j, tile_idx):
        S.fence()
        pre_norm(li)
        mset("dve", lraug[:, :], 1.0, [lraug.reg])
        if tile_idx == 0:
            mset("pool", gs[:, :, :], 0.0, [gs.reg])
            mset("pool", gsb[:, :, :], 0.0, [gsb.reg])
        else:
            dma(bass.AP(gs.t, 0, [[8 * 512, 128], [1, 8 * 512]]), gstate_d[j, :, :],
                [gstate_reg[j]], [gs.reg], "g")
            for kc in range(8):
                cp("pool", gsb[:, kc, :], gs[:, kc, :], [gs.reg], [gsb.reg])
        if _DBG <= 1:
            return
        for b in range(24):
            slot = W.get()
            if b < 4:
                for m in range(2):
                    oc = b * 2 + m
                    proj_fm(slot, m, un, KC,
                            lambda p, oc=oc: cp("act", qT[:, oc, :], p[:, :], [p.reg], [qT.reg]))
            elif b < 8:
                bb = b - 4
                for m in range(2):
                    oc = bb * 2 + m
                    proj_fm(slot, m, un, KC,
                            lambda p, oc=oc: cp("act", kT[:, oc, :], p[:, :], [p.reg], [kT.reg]))
                for s in range(4):
                    p = psum()
                    for kc in range(KC):
                        mm(p[:, 0:256], un[:, kc, s * 128:(s + 1) * 128], wbf[:, slot, kc * 256:(kc + 1) * 256],
                           kc == 0, kc == KC - 1, [un.reg, wbf.slots[slot]], [p.reg])
                    cp("dve", ktok_ap(s, bb * 256, 256), p[:, 0:256], [p.reg], [G[2].reg])
            elif b < 16:
                bb = b - 8
                for s in range(4):
                    p = psum()
                    for kc in range(KC):
                        mm(p[:, 0:256], un[:, kc, s * 128:(s + 1) * 128], wbf[:, slot, kc * 256:(kc + 1) * 256],
                           kc == 0, kc == KC - 1, [un.reg, wbf.slots[slot]], [p.reg])
                    cp("act" if s % 2 else "dve", vtok_ap(s, bb * 256, 256), p[:, 0:256],
                       [p.reg], [G[3 + s // 2].reg])
            else:
                bb = b - 16
                for m in range(2):
                    oc = bb * 2 + m
                    proj_fm(slot, m, un, KC,
                            lambda p, oc=oc: act(rT_ap(oc), p[:, :], AF.Silu, [p.reg], [G[5 + oc // 8].reg]))
        if _DBG <= 2:
            return
        p = psum()
        for kc in range(KC):
            mm(p[0:16, :], wlr[:, j, kc, :], un[:, kc, :], kc == 0, kc == KC - 1,
               [wlr.reg, un.reg], [p.reg])
        cp("act", lraug[0:16, :], p[0:16, :], [p.reg], [lraug.reg])
        if _DBG <= 3:
            return
        for s in range(4):
            tsl = slice(s * 128, (s + 1) * 128)
            for h2 in range(2):
                p = psum()
                mm(p[:, :], lraug[:, tsl], wgaug[:, j, h2 * 512:(h2 + 1) * 512], True, True,
                   [lraug.reg, wgaug.reg], [p.reg])
                act(etmp[:, h2 * 512:(h2 + 1) * 512], p[:, :], AF.Exp, [p.reg], [etmp.reg], scale=-1.0)
            act(sptok[:, :], etmp[:, :], AF.Ln, [etmp.reg], [sptok.reg], bias=1.0)
            for g4 in range(2):
                p = psum()
                for c in range(4):
                    kc = g4 * 4 + c
                    mm(p[:, c * 128:(c + 1) * 128], sptok[:, kc * 128:(kc + 1) * 128], tri[:, 0, :],
                       True, True, [sptok.reg, tri.reg], [p.reg])
                ebv = bass.AP(eb.t, g4 * 512, [[KC * 128, 128], [1, 512]])
                eiv = bass.AP(einv.t, g4 * 512, [[KC * 128, 128], [1, 512]])
                act(ebv, p[:, :], AF.Exp, [p.reg], [eb.reg], scale=-1.0 / 16.0)
                act(eiv, p[:, :], AF.Exp, [p.reg], [einv.reg], scale=1.0 / 16.0)
            for kc in range(KC):
                stt("dve", qdec[:, kc, :], qT[:, kc, tsl], 1.0 / 16.0, eb[:, kc, :], ALU.mult, ALU.mult,
                    [qT.reg, eb.reg], [qdec.reg])
                tt("pool", kdec[:, kc, :], kT[:, kc, tsl], einv[:, kc, :], ALU.mult,
                   [kT.reg, einv.reg], [kdec.reg])
            for h2 in range(2):
                p = psum()
                mm(p[:, :], tri[:, 1, :], sptok[:, h2 * 512:(h2 + 1) * 512], True, True,
                   [tri.reg, sptok.reg], [p.reg])
                act(etmp[:, h2 * 512:(h2 + 1) * 512], p[:, :], AF.Exp, [p.reg], [etmp.reg], scale=-1.0 / 16.0)
            for h2 in range(2):
                tt("dve", k2[:, h2 * 512:(h2 + 1) * 512], ktok_ap(s, h2 * 512, 512),
                   etmp[:, h2 * 512:(h2 + 1) * 512], ALU.mult, [G[2].reg, etmp.reg], [k2.reg])
            if _DBG <= 4:
                continue
            pa = psum()
            for h in range(4):
                for c in range(2):
                    mm(pa[:, h * 128:(h + 1) * 128], kdec[:, 2 * h + c, :], qdec[:, 2 * h + c, :],
                       c == 0, c == 1, [kdec.reg, qdec.reg], [pa.reg])
            for h in range(4):
                tt("dve", attn[:, h, :], pa[:, h * 128:(h + 1) * 128], tri[:, 2, :], ALU.mult,
                   [pa.reg, tri.reg], [attn.reg])
            if _SUB <= 1:
                continue
            for h in range(4):
                po = psum()
                for vc in range(4):
                    vcol = h * 512 + vc * 128
                    mm(po[:, vc * 128:(vc + 1) * 128], vtok_ap(s, vcol, 128), attn[:, h, :],
                       True, "a" in _VAR, vregs + [attn.reg], [po.reg])
                    if "a" in _VAR:
                        continue
                    for c in range(2):
                        mm(po[:, vc * 128:(vc + 1) * 128], gsb[:, 2 * h + c, vc * 128:(vc + 1) * 128],
                           qdec[:, 2 * h + c, :], False, c == 1, [gsb.reg, qdec.reg], [po.reg])
                osb_v = bass.AP(osb.t, 0, [[512, 128], [1, 512]])
                osq_v = bass.AP(osq.t, 0, [[512, 128], [1, 512]])
                if "b" in _VAR:
                    continue
                cp("dve", osb_v, po[:, :], [po.reg], [osb.reg])
                if "c" in _VAR:
                    continue
                act(osq_v, osb_v, AF.Square, [osb.reg], [osq.reg])
                if _SUB <= 2:
                    continue
                pn = psum()
                for vc in range(4):
                    mm(pn[:, 0:128], ones_m[:, 1, :], osq[:, vc, :], vc == 0, vc == 3,
                       [ones_m.reg, osq.reg], [pn.reg])
                rsqrt_eps(orstd[:, :], pn[:, 0:128], [pn.reg], orstd.reg)
                for vc in range(4):
                    stt("dve", osb[:, vc, :], osb[:, vc, :], gng[:, j, vc:vc + 1], orstd[:, :],
                        ALU.mult, ALU.mult, [osb.reg, gng.reg, orstd.reg], [osb.reg])
                    tt("pool", inner[:, h * 4 + vc, tsl], osb[:, vc, :], rT_ap(h * 4 + vc, tsl), ALU.mult,
                       [osb.reg] + rregs, [inner.reg])
                if _SUB <= 3:
                    continue
                for c in range(2):
                    kc = 2 * h + c
                    pst = psum()
                    mm(pst[:, :], k2[:, kc * 128:(kc + 1) * 128], vtok_ap(s, h * 512, 512),
                       True, True, [k2.reg] + vregs, [pst.reg])
                    stt("dve", gs[:, kc, :], gs[:, kc, :], eb[:, kc, 127:128], pst[:, :],
                        ALU.mult, ALU.add, [gs.reg, eb.reg, pst.reg], [gs.reg])
                    cp("act", gsb[:, kc, :], gs[:, kc, :], [gs.reg], [gsb.reg])
        if _DBG <= 5:
            return
        dma(gstate_d[j, :, :], bass.AP(gs.t, 0, [[8 * 512, 128], [1, 8 * 512]]),
            [gs.reg], [gstate_reg[j]], "g")
        if _DBG <= 6:
            return
        out_proj_and_residual(li)

    for t in range(NT):
        ie = io = 0
        for kind in layers:
            if kind == "e":
                for b in range(44):
                    W.push(ie * 44 + b)
                ie += 1
            else:
                for b in range(32):
                    W.push(ne1 * 44 + io * 32 + b)
                io += 1

    for t in range(NT):
        for kc in range(KC):
            dma(xT[:, kc, :], xT_d[:, kc, t * T:(t + 1) * T], [], [xT.reg], "x")
        ie = io = 0
        for li, kind in enumerate(layers):
            if kind == "e":
                even_layer(li, ie)
                ie += 1
            else:
                odd_layer(li, io, t)
                io += 1
        for kc in range(KC):
            dma(yT_d[:, kc, t * T:(t + 1) * T], xT[:, kc, :], [xT.reg], [], "y")

    S.emit(nc, ["y"])
    es.close()
    return nc


def _wblocks(w, cw):
    din, dout = w.shape
    kc = din // 128
    nb = dout // cw
    a = w.reshape(kc, 128, nb, cw).transpose(2, 1, 0, 3)
    return np.ascontiguousarray(a.reshape(nb, 128, kc * cw))


def _vec(v):
    return np.ascontiguousarray(v.reshape(-1, 128).T)


def _pack(inp, layers):
    f = np.float32
    nl = len(layers)
    even = [i for i, k in enumerate(layers) if k == "e"]
    odd = [i for i, k in enumerate(layers) if k == "o"]
    ne1, no1 = max(len(even), 1), max(len(odd), 1)
    out = {}
    out["npre"] = np.ascontiguousarray(np.stack([_vec(inp["norm_pre"][i]) for i in range(max(nl, 1))], 1)).astype(f)
    out["npost"] = np.ascontiguousarray(np.stack([_vec(inp["norm_post"][i]) for i in range(max(nl, 1))], 1)).astype(f)
    out["ident"] = np.eye(128, dtype=f)
    jj, tt_ = np.meshgrid(np.arange(128), np.arange(128), indexing="ij")
    tri = np.zeros((128, 3, 128), f)
    tri[:, 0, :] = (jj <= tt_)
    tri[:, 1, :] = (jj > tt_)
    tri[:, 2, :] = (jj <= tt_)
    out["tri"] = tri
    evec = np.zeros((128, ne1, 6, KC), f)
    convw = np.zeros((128, ne1, KC, CONV_K), f)
    s5p = np.zeros((128, ne1, 3, 32), f)
    cpad = np.zeros((ne1, 128, 32, 2, 128), f)
    bpad = np.zeros((ne1, 8, 128, 4, 2, 128), f)
    w_in_e = np.zeros((ne1, 20, 128, 2048), f)
    w_glu = np.zeros((ne1, 4, 128, 2048), f)
    w_pw = np.zeros((ne1, 4, 128, 2048), f)
    w_out_e = np.zeros((ne1, 8, 128, 2048), f)
    for j in range(len(even)):
        for k, nm in enumerate(["s5_d", "s5_b_glu", "conv_b", "conv_ln_g", "conv_ln_b", "conv_b_pw"]):
            evec[:, j, k, :] = _vec(inp[nm][j])
        convw[:, j, :, :] = inp["conv_w"][j].T.reshape(KC, 128, CONV_K).transpose(1, 0, 2)
        for a in range(2):
            s5p[64 * a:64 * a + 64, j, 0, :] = inp["s5_lambda_re"][j][a::2, :].T
            s5p[64 * a:64 * a + 64, j, 1, :] = inp["s5_lambda_im"][j][a::2, :].T
            s5p[64 * a:64 * a + 64, j, 2, :] = inp["s5_log_dt"][j][a::2][None, :]
        for P in range(32):
            r = P % 4
            for a in range(2):
                g = 2 * P + a
                for ri, (bn, cn) in enumerate([("s5_b_re", "s5_c_re"), ("s5_b_im", "s5_c_im")]):
                    bpad[j, P // 4, 32 * r + 16 * a:32 * r + 16 * a + 16, P % 4, ri, 64 * a:64 * a + 64] = \
                        inp[bn][j, g].T
                    cpad[j, 64 * a:64 * a + 64, P, ri, 32 * r + 16 * a:32 * r + 16 * a + 16] = \
                        inp[cn][j, g].T
        w_in_e[j] = _wblocks(inp["ev_w_in"][j], 256)
        w_glu[j] = _wblocks(inp["s5_w_glu"][j], 256)
        w_pw[j] = _wblocks(inp["conv_w_pw"][j], 256)
        w_out_e[j] = _wblocks(inp["ev_w_out"][j], 128)
    out.update(evec=evec, convw=convw, s5p=s5p, cpad=cpad.reshape(ne1, 128, 32 * 2 * 128),
               bpad=bpad.reshape(ne1, 8, 128, 1024), w_in_e=w_in_e, w_glu=w_glu, w_pw=w_pw,
               w_out_e=w_out_e)
    w_in_o = np.zeros((no1, 24, 128, 2048), f)
    w_out_o = np.zeros((no1, 8, 128, 2048), f)
    wlr = np.zeros((128, no1, KC, 16), f)
    wgaug = np.zeros((32, no1, 1024), f)
    gng = np.zeros((128, no1, 4), f)
    for j in range(len(odd)):
        w = inp["od_w_in"][j]
        w_in_o[j] = _wblocks(np.ascontiguousarray(w[:, :6144]), 256)
        wlr[:, j, :, :] = w[:, 6144:6160].reshape(KC, 128, 16).transpose(1, 0, 2)
        wgaug[0:16, j, :] = inp["gla_w_gate_up"][j]
        wgaug[16, j, :] = inp["gla_b_gate"][j]
        gng[:, j, :] = _vec(inp["gla_norm_g"][j])
        w_out_o[j] = _wblocks(inp["od_w_out"][j], 128)
    out.update(w_in_o=w_in_o, w_out_o=w_out_o, wlr=wlr, wgaug=wgaug, gng=gng)
    return out


_LAYERS = ["e", "o", "e", "o"]


def run_layers(inp, layers, n_cores=8):
    inp = {k: np.asarray(v, dtype=np.float32) for k, v in inp.items()}
    x = inp["x"]
    B, L, _ = x.shape
    shared = _pack(inp, layers)
    nc = build_program(L, layers)
    in_maps = []
    for c in range(n_cores):
        b = c % B
        m = dict(shared)
        m["xT"] = np.ascontiguousarray(x[b].T.reshape(KC, 128, L).transpose(1, 0, 2))
        in_maps.append(m)
    res = run_bass_kernel_spmd(nc, in_maps, core_ids=list(range(n_cores)))
    out = np.empty((B, L, D), np.float32)
    for b in range(B):
        yT = np.asarray(res.results[b]["yT"])
        out[b] = yT.transpose(1, 0, 2).reshape(D, L).T
    return out


def kernel(**inputs):
    return run_layers(inputs, _LAYERS)
```

```python
import math
import os
from contextlib import ExitStack

_DBG = int(os.environ.get("KDBG", "99"))
_SUB = int(os.environ.get("KSUB", "99"))
_VAR = os.environ.get("KVAR", "")

import numpy as np
import concourse.bass as bass
import concourse.mybir as mybir
from concourse.bass_utils import run_bass_kernel_spmd

F32 = mybir.dt.float32
BF16 = mybir.dt.bfloat16
ALU = mybir.AluOpType
AF = mybir.ActivationFunctionType

D = 1024
KC = 8
T = 512
EPS = 1e-6
CONV_K = 31
HALO = CONV_K - 1
PAD = 264
TWO_PI = 2.0 * math.pi


class Reg:
    __slots__ = ("name", "w", "r", "rd")

    def __init__(self, name):
        self.name = name
        self.w = None
        self.r = {}
        self.rd = []


class Op:
    __slots__ = ("eng", "fn", "deps", "need_inc", "val", "dsem", "is_dma")

    def __init__(self, eng, fn, dsem=None):
        self.eng = eng
        self.fn = fn
        self.deps = []
        self.need_inc = False
        self.val = 0
        self.dsem = dsem
        self.is_dma = dsem is not None


ENGS = ("pe", "act", "dve", "pool", "sp")


class Sched:
    def __init__(self):
        self.ops = {e: [] for e in ENGS}
        self.dsem_names = []
        self.pending = {}

    def fence(self, engs=("pe", "act", "dve", "pool")):
        last = [self.ops[e][-1] for e in engs if self.ops[e]]
        for e in engs:
            self.pending.setdefault(e, []).extend(last)

    def _add_dep(self, o, d):
        if d is None or d is o:
            return
        if (not d.is_dma) and d.eng == "pe" and o.eng == "pe" and not o.is_dma:
            return
        o.deps.append(d)

    def op(self, eng, fn, reads=(), writes=(), dsem=None):
        o = Op(eng, fn, dsem)
        if eng in self.pending and not o.is_dma:
            for d in self.pending.pop(eng):
                if d.eng != eng:
                    self._add_dep(o, d)
        for r in reads:
            self._add_dep(o, r.w)
        for w in writes:
            self._add_dep(o, w.w)
            for d in w.r.values():
                self._add_dep(o, d)
            for d in w.rd:
                self._add_dep(o, d)
        for r in reads:
            if o.is_dma:
                r.rd.append(o)
            else:
                r.r[eng] = o
        for w in writes:
            w.w = o
            w.r = {}
            w.rd = []
        if dsem is not None and dsem not in self.dsem_names:
            self.dsem_names.append(dsem)
        self.ops[eng].append(o)
        return o

    def emit(self, nc, final_waits):
        with ExitStack() as es:
            esem = {e: es.enter_context(nc.semaphore("s_" + e)) for e in ENGS}
            dsem = {n: es.enter_context(nc.semaphore("d_" + n)) for n in self.dsem_names}
            for e in ENGS:
                for o in self.ops[e]:
                    for d in o.deps:
                        if not d.is_dma:
                            d.need_inc = True
            for e in ENGS:
                c = 0
                for o in self.ops[e]:
                    if o.is_dma:
                        continue
                    if o.need_inc:
                        c += 1
                        o.val = c
            dcount = {n: 0 for n in self.dsem_names}
            for e in ENGS:
                for o in self.ops[e]:
                    if o.is_dma:
                        dcount[o.dsem] += 16
                        o.val = dcount[o.dsem]
            block = es.enter_context(nc.Block())

            def run(engname, eobj):
                waited = {}
                for o in self.ops[engname]:
                    need = {}
                    for d in o.deps:
                        s = dsem[d.dsem] if d.is_dma else esem[d.eng]
                        k = id(s)
                        if k not in need or need[k][1] < d.val:
                            need[k] = (s, d.val)
                    for k, (s, v) in need.items():
                        if waited.get(k, 0) < v:
                            eobj.wait_ge(s, v)
                            waited[k] = v
                    ins = o.fn(eobj)
                    if o.is_dma:
                        ins.then_inc(dsem[o.dsem], 16)
                    elif o.need_inc:
                        ins.then_inc(esem[engname], 1)
                if engname == "sp":
                    for n in final_waits:
                        eobj.wait_ge(dsem[n], dcount[n])

            @block.tensor
            def _(e):
                run("pe", e)

            @block.scalar
            def _(e):
                run("act", e)

            @block.vector
            def _(e):
                run("dve", e)

            @block.gpsimd
            def _(e):
                run("pool", e)

            @block.sync
            def _(e):
                run("sp", e)


class Buf:
    def __init__(self, t, name, nslots=0):
        self.t = t
        self.reg = Reg(name)
        self.slots = [Reg("%s.%d" % (name, i)) for i in range(nslots)]

    def __getitem__(self, k):
        return self.t[k]


def build_program(L, layers):
    assert L % T == 0
    NT = L // T
    n_e = sum(1 for k in layers if k == "e")
    n_o = sum(1 for k in layers if k == "o")
    nl = len(layers)
    nc = bass.Bass("TRN2", target_bir_lowering=False)
    S = Sched()

    def din(name, shape, dt=F32):
        return nc.dram_tensor(name, list(shape), dt, kind="ExternalInput").ap()

    xT_d = din("xT", [128, KC, L])
    yT_d = nc.dram_tensor("yT", [128, KC, L], F32, kind="ExternalOutput").ap()
    npre_d = din("npre", [128, max(nl, 1), KC])
    npost_d = din("npost", [128, max(nl, 1), KC])
    ident_d = din("ident", [128, 128])
    tri_d = din("tri", [128, 3, 128])
    ne1, no1 = max(n_e, 1), max(n_o, 1)
    evec_d = din("evec", [128, ne1, 6, KC])
    convw_d = din("convw", [128, ne1, KC, CONV_K])
    s5p_d = din("s5p", [128, ne1, 3, 32])
    cpad_d = din("cpad", [ne1, 128, 32 * 2 * 128])
    w_in_e = din("w_in_e", [ne1, 20, 128, 2048])
    bpad_d = din("bpad", [ne1, 8, 128, 1024])
    w_glu_d = din("w_glu", [ne1, 4, 128, 2048])
    w_pw_d = din("w_pw", [ne1, 4, 128, 2048])
    w_out_e = din("w_out_e", [ne1, 8, 128, 2048])
    w_in_o = din("w_in_o", [no1, 24, 128, 2048])
    w_out_o = din("w_out_o", [no1, 8, 128, 2048])
    wlr_d = din("wlr", [128, no1, KC, 16])
    wgaug_d = din("wgaug", [32, no1, 1024])
    gng_d = din("gng", [128, no1, 4])
    cprime_d = nc.dram_tensor("cprime", [ne1, 8, 128, 1024], F32, kind="ExternalOutput").ap()
    cprime_reg = [Reg("cprime%d" % j) for j in range(ne1)]
    EB = 60
    NUQ = ne1 * EB + no1 * (24 + 8)
    wq_d = nc.dram_tensor("wq", [NUQ, 128, 2048], BF16, kind="Internal").ap()
    wq_reg = [Reg("wq%d" % i) for i in range(NUQ)]

    es = ExitStack()

    def sb(name, shape, dt=F32, nslots=0):
        return Buf(es.enter_context(nc.sbuf_tensor(name, list(shape), dt)), name, nslots)

    xT = sb("xTs", [128, KC, T])
    un = sb("un", [128, KC, T], BF16)
    sq = un
    rstd = sb("rstd", [128, T])
    big32 = sb("big32", [128, KC, T], nslots=2)
    valb = acc = ybuf = big32
    inner = sb("inner", [128, 16, T], BF16, nslots=2)
    ones_m = sb("ones_m", [128, 2, 128], BF16)
    eps_col = sb("eps_col", [128, 2])
    ident = sb("identb", [128, 128], BF16)
    tri = sb("trib", [128, 3, 128], BF16)
    NW = 4
    wbf = sb("wbf", [128, NW, 2048], BF16, nslots=NW)
    sct = [sb("sct%d" % i, [128, PAD + T]) for i in range(2)]
    npre = sb("npre_s", [128, max(nl, 1), KC])
    npost = sb("npost_s", [128, max(nl, 1), KC])
    evec = sb("evec_s", [128, ne1, 6, KC])
    convw = sb("convw_s", [128, ne1, KC, CONV_K])
    halo = [sb("halo%d" % j, [128, KC, HALO], BF16) for j in range(ne1)]
    carry = [sb("carry%d" % j, [128, 32, 2]) for j in range(ne1)]
    pwr = [sb("pwr%d" % j, [128, 9, 32]) for j in range(ne1)]
    pwi = [sb("pwi%d" % j, [128, 9, 32]) for j in range(ne1)]
    npwi = [sb("npwi%d" % j, [128, 9, 32]) for j in range(ne1)]
    wlr = sb("wlr_s", [128, no1, KC, 16], BF16)
    wgaug = sb("wgaug_s", [32, no1, 1024], BF16)
    gng = sb("gng_s", [128, no1, 4])
    gs = sb("gs", [128, 8, 512])
    gsb = sb("gsb", [128, 8, 512], BF16)
    gstate_d = nc.dram_tensor("gstate", [no1, 128, 8 * 512], F32, kind="ExternalOutput").ap()
    gstate_reg = [Reg("gstate%d" % j) for j in range(no1)]
    G = [sb("G%d" % i, [128, KC, T], BF16) for i in range(7)]
    a_in, saz, sbz, accb, ya = G[0], G[1], G[2], G[3], G[4]
    qT, kT, ktok_b, vtok_b, rT_b = G[0], G[1], G[2], (G[3], G[4]), (G[5], G[6])

    def phase_alloc(specs):
        out = {}
        with ExitStack() as pes:
            for name, shape, dt in specs:
                out[name] = Buf(pes.enter_context(nc.sbuf_tensor(name, list(shape), dt)), name)
        return out

    EV = phase_alloc([
        ("hpad", [128, KC, HALO + T], BF16),
        ("tmpA", [128, T], F32), ("tmpB", [128, T], F32),
        ("hs0", [128, PAD + T], F32), ("hs1", [128, PAD + T], F32),
        ("hs2", [128, PAD + T], F32), ("hs3", [128, PAD + T], F32),
        ("sbre", [128, T], BF16), ("sbim", [128, T], BF16),
    ])
    hpad, tmpA, tmpB = EV["hpad"], EV["tmpA"], EV["tmpB"]
    hs = [EV["hs0"], EV["hs1"], EV["hs2"], EV["hs3"]]

    class _MeanView:
        reg = hs[2].reg

        def __getitem__(self, k):
            return hs[2][:, 0:T]
    mean = _MeanView()
    sbre, sbim = EV["sbre"], EV["sbim"]
    OD = phase_alloc([
        ("lraug", [32, T], BF16), ("sptok", [128, 1024], BF16), ("etmp", [128, 1024], F32),
        ("k2", [128, 1024], BF16), ("qdec", [128, KC, 128], BF16), ("kdec", [128, KC, 128], BF16),
        ("eb", [128, KC, 128], F32), ("einv", [128, KC, 128], F32), ("attn", [128, 4, 128], BF16),
        ("osb", [128, 4, 128], F32), ("osq", [128, 4, 128], BF16), ("orstd", [128, 128], F32),
        ("s5t", [128, 16, 32], F32), ("s5p_s", [128, ne1, 3, 32], F32),
        ("s5i", [128, 32], mybir.dt.int32),
    ])
    s5t, s5p, s5i = OD["s5t"], OD["s5p_s"], OD["s5i"]
    lraug, sptok, etmp, k2, qdec, kdec = (OD[k] for k in ("lraug", "sptok", "etmp", "k2", "qdec", "kdec"))
    eb, einv, attn, osb, osq, orstd = (OD[k] for k in ("eb", "einv", "attn", "osb", "osq", "orstd"))

    NPS = 8
    ps = [Buf(es.enter_context(nc.psum_tensor("ps%d" % i, [128, 512], F32)), "ps%d" % i)
          for i in range(NPS)]
    ps_i = [0]

    def psum():
        p = ps[ps_i[0] % NPS]
        ps_i[0] += 1
        return p

    uniq = [0]

    def dma(out_ap, in_ap, reads, writes, sem):
        if sem in ("c0", "c1"):
            sem = "c%d" % uniq[0]
            uniq[0] += 1
        return S.op("sp", lambda e: e.dma_start(out=out_ap, in_=in_ap), reads, writes, dsem=sem)

    def mm(out_ap, lhsT, rhs, start, stop, reads, writes):
        return S.op("pe", lambda e: e.matmul(out_ap, lhsT, rhs, start=start, stop=stop),
                    reads, writes)

    def act(out_ap, in_ap, func, reads, writes, bias=None, scale=None):
        kw = {}
        if bias is not None:
            kw["bias"] = bias
        if scale is not None:
            kw["scale"] = scale
        return S.op("act", lambda e: e.activation(out_ap, in_ap, func, **kw), reads, writes)

    def tt(eng, out_ap, a, b, op, reads, writes):
        return S.op(eng, lambda e: e.tensor_tensor(out_ap, a, b, op), reads, writes)

    def ts(eng, out_ap, a, s1, s2, op0, op1, reads, writes):
        if s2 is None:
            return S.op(eng, lambda e: e.tensor_scalar(out_ap, a, s1, None, op0), reads, writes)
        return S.op(eng, lambda e: e.tensor_scalar(out_ap, a, s1, s2, op0, op1), reads, writes)

    def stt(eng, out_ap, a, sc, b, op0, op1, reads, writes):
        eng = "dve"
        return S.op(eng, lambda e: e.scalar_tensor_tensor(out_ap, a, sc, b, op0, op1),
                    reads, writes)

    def cp(eng, out_ap, in_ap, reads, writes):
        if eng == "act":
            return S.op("act", lambda e: e.copy(out_ap, in_ap), reads, writes)
        return S.op(eng, lambda e: e.tensor_copy(out_ap, in_ap), reads, writes)

    def mset(eng, ap, v, writes):
        return S.op(eng, lambda e: e.memset(ap, v), (), writes)

    class WStream:
        def __init__(self):
            self.blocks = []
            self.n_dma = 0
            self.n_get = 0

        def push(self, u):
            self.blocks.append(u)

        def _dma_to(self, i):
            while self.n_dma <= i and self.n_dma < len(self.blocks):
                b = self.n_dma
                u = self.blocks[b]
                sl = b % NW
                dma(wbf[:, sl, :], wq_d[u, :, :], [wq_reg[u]], [wbf.slots[sl]], "w%d" % sl)
                self.n_dma += 1

        def get(self):
            i = self.n_get
            self.n_get += 1
            self._dma_to(i + NW - 1)
            return i % NW

    W = WStream()
    cast_rr = [0]

    def precast(u, parts, regs=()):
        k = cast_rr[0] % 2
        cast_rr[0] += 1
        st_ = bass.AP(big32.t, k * 2048, [[KC * T, 128], [1, 2048]])
        ob_ = bass.AP(inner.t, k * 2048, [[16 * T, 128], [1, 2048]])
        sreg, oreg = big32.slots[k], inner.slots[k]
        for ap, c0, n in parts:
            dma(bass.AP(big32.t, k * 2048 + c0, [[KC * T, 128], [1, n]]), ap, list(regs), [sreg], "pc%d" % k)
        eng = ("pool", "act", "dve")[cast_rr[0] % 3]
        cp(eng, ob_, st_, [sreg], [oreg])
        dma(wq_d[u, :, :], ob_, [oreg], [wq_reg[u]], "pw%d" % k)

    def load_small(dst, src_ap, sem="c0"):
        dma(dst.t[:], src_ap, (), [dst.reg], sem)

    load_small(npre, npre_d)
    load_small(npost, npost_d)
    st_i = bass.AP(big32.t, 0, [[KC * T, 128], [1, 128]])
    dma(st_i, ident_d, (), [big32.reg], "c0")
    cp("dve", ident[:, :], st_i, [big32.reg], [ident.reg])
    st_t = bass.AP(big32.t, 0, [[KC * T, 128], [1, 384]])
    dma(st_t, tri_d.rearrange("p a b -> p (a b)"), [], [big32.reg], "c0")
    cp("dve", bass.AP(tri.t, 0, [[384, 128], [1, 384]]), st_t, [big32.reg], [tri.reg])
    mset("dve", eps_col[:, :], EPS, [eps_col.reg])
    mset("dve", ones_m[:, 0, :], 1.0 / 1024.0, [ones_m.reg])
    mset("dve", ones_m[:, 1, :], 1.0 / 512.0, [ones_m.reg])
    if n_e:
        load_small(evec, evec_d)
        load_small(convw, convw_d)
        load_small(s5p, s5p_d)
        for j in range(n_e):
            mset("pool", halo[j][:, :, :], 0.0, [halo[j].reg])
            mset("pool", carry[j][:, :, :], 0.0, [carry[j].reg])
    if n_o:
        nw = no1 * KC * 16
        wl_f = bass.AP(big32.t, 0, [[KC * T, 128], [1, nw]])
        dma(wl_f, wlr_d.rearrange("p j k c -> p (j k c)"), [], [big32.reg], "c0")
        cp("dve", bass.AP(wlr.t, 0, [[nw, 128], [1, nw]]), wl_f, [big32.reg], [wlr.reg])
        ng = no1 * 1024
        wg_f = bass.AP(big32.t, 0, [[KC * T, 32], [1, ng]])
        dma(wg_f, wgaug_d.rearrange("p j c -> p (j c)"), [], [big32.reg], "c0")
        cp("dve", bass.AP(wgaug.t, 0, [[ng, 32], [1, ng]]), wg_f, [big32.reg], [wgaug.reg])
        load_small(gng, gng_d)

    def s5_prologue(j):
        R = [s5t.reg]
        st = lambda k: s5t[:, k, :]
        lre, lim, ldt = s5p[:, j, 0, :], s5p[:, j, 1, :], s5p[:, j, 2, :]
        act(st(0), ldt, AF.Exp, [s5p.reg], R)
        tt("dve", st(1), lre, st(0), ALU.mult, [s5p.reg] + R, R)
        tt("dve", st(2), lim, st(0), ALU.mult, [s5p.reg] + R, R)
        act(st(3), st(1), AF.Exp, R, R)
        def reduce_to_pi(shift):
            RI = R + [s5i.reg]
            ts("dve", st(10), st(2), shift, None, ALU.add, None, R, R)
            ts("dve", st(15), st(10), 1.0 / TWO_PI, None, ALU.mult, None, R, R)
            cp("dve", s5i[:, :], st(15), R, [s5i.reg])
            cp("dve", st(15), s5i[:, :], [s5i.reg], R)
            stt("dve", st(4), st(15), -TWO_PI, st(10), ALU.mult, ALU.add, RI, R)
            ts("dve", st(15), st(4), math.pi, TWO_PI, ALU.is_gt, ALU.mult, R, R)
            tt("dve", st(4), st(4), st(15), ALU.subtract, R, R)
            ts("dve", st(15), st(4), -math.pi, TWO_PI, ALU.is_lt, ALU.mult, R, R)
            tt("dve", st(4), st(4), st(15), ALU.add, R, R)

        reduce_to_pi(0.0)
        act(st(5), st(4), AF.Sin, R, R)
        reduce_to_pi(0.5 * math.pi)
        act(st(6), st(4), AF.Sin, R, R)
        tt("dve", st(7), st(3), st(6), ALU.mult, R, R)
        tt("dve", st(8), st(3), st(5), ALU.mult, R, R)
        P0 = [pwr[j].reg, pwi[j].reg, npwi[j].reg]
        cp("dve", pwr[j][:, 0, :], st(7), R, P0)
        cp("dve", pwi[j][:, 0, :], st(8), R, P0)
        for k in range(8):
            tt("dve", st(10), pwr[j][:, k, :], pwr[j][:, k, :], ALU.mult, P0 + R, R)
            tt("dve", st(15), pwi[j][:, k, :], pwi[j][:, k, :], ALU.mult, P0 + R, R)
            tt("dve", pwr[j][:, k + 1, :], st(10), st(15), ALU.subtract, R + P0, P0)
            tt("dve", st(10), pwr[j][:, k, :], pwi[j][:, k, :], ALU.mult, P0 + R, R)
            ts("dve", pwi[j][:, k + 1, :], st(10), 2.0, None, ALU.mult, None, R + P0, P0)
        ts("dve", npwi[j][:, :, :], pwi[j][:, :, :], -1.0, None, ALU.mult, None, P0, P0)
        ts("dve", st(7), st(7), -1.0, None, ALU.add, None, R, R)
        tt("dve", st(9), lre, lre, ALU.mult, [s5p.reg] + R, R)
        tt("dve", st(10), lim, lim, ALU.mult, [s5p.reg] + R, R)
        tt("dve", st(9), st(9), st(10), ALU.add, R, R)
        S.op("dve", lambda e: e.reciprocal(st(9), st(9)), R, R)
        tt("dve", st(11), st(7), lre, ALU.mult, [s5p.reg] + R, R)
        tt("dve", st(10), st(8), lim, ALU.mult, [s5p.reg] + R, R)
        tt("dve", st(11), st(11), st(10), ALU.add, R, R)
        tt("dve", st(12), st(8), lre, ALU.mult, [s5p.reg] + R, R)
        tt("dve", st(10), st(7), lim, ALU.mult, [s5p.reg] + R, R)
        tt("dve", st(12), st(12), st(10), ALU.subtract, R, R)
        tt("dve", st(13), st(11), st(9), ALU.mult, R, R)
        tt("dve", st(14), st(12), st(9), ALU.mult, R, R)
        ts("dve", st(11), st(13), -1.0, None, ALU.mult, None, R, R)
        ts("dve", st(12), st(14), -1.0, None, ALU.mult, None, R, R)
        for q in range(8):
            cq = big32
            dma(bass.AP(cq.t, 0, [[KC * T, 128], [1, 1024]]),
                cpad_d[j, :, q * 1024:(q + 1) * 1024], [], [cq.reg], "c0")
            dst = big32
            for pp in range(4):
                P = q * 4 + pp
                cre = bass.AP(cq.t, pp * 256, [[KC * T, 128], [1, 128]])
                cim = bass.AP(cq.t, pp * 256 + 128, [[KC * T, 128], [1, 128]])
                ore = bass.AP(dst.t, 2048 + pp * 256, [[KC * T, 128], [1, 128]])
                oim = bass.AP(dst.t, 2048 + pp * 256 + 128, [[KC * T, 128], [1, 128]])
                ts("dve", ore, cre, s5t[:, 13, P:P + 1], None, ALU.mult, None,
                   [cq.reg] + R, [dst.reg])
                stt("dve", ore, cim, s5t[:, 12, P:P + 1], ore, ALU.mult, ALU.add,
                    [cq.reg] + R, [dst.reg])
                ts("dve", oim, cre, s5t[:, 12, P:P + 1], None, ALU.mult, None,
                   [cq.reg] + R, [dst.reg])
                stt("dve", oim, cim, s5t[:, 11, P:P + 1], oim, ALU.mult, ALU.add,
                    [cq.reg] + R, [dst.reg])
            dma(cprime_d[j, q, :, :], bass.AP(dst.t, 2048, [[KC * T, 128], [1, 1024]]),
                [dst.reg], [cprime_reg[j]], "c1")

    for j in range(n_e):
        s5_prologue(j)

    def handoff(buf):
        S.op("dve", lambda e: e.memset(bass.AP(buf.t, 0, [[buf_stride[buf], 128], [1, 1]]), 0.0),
             (), [buf.reg] + buf.slots)
    buf_stride = {big32: KC * T, inner: 16 * T}
    handoff(big32)
    handoff(inner)
    def precast_diag(u, j, items):
        k2 = cast_rr[0] % 2
        cast_rr[0] += 1
        oreg = inner.slots[k2]
        for m, (kc, k) in enumerate(items):
            ts("dve", bass.AP(inner.t, k2 * 2048 + m * 128, [[16 * T, 128], [1, 128]]), ident[:, :],
               convw[:, j, kc, k:k + 1], None, ALU.mult, None, [ident.reg, convw.reg], [oreg])
        dma(wq_d[u, :, :], bass.AP(inner.t, k2 * 2048, [[16 * T, 128], [1, 2048]]), [oreg], [wq_reg[u]],
            "pw%d" % k2)

    for j in range(n_e):
        base = j * EB
        for b in range(20):
            precast(base + b, [(w_in_e[j, b, :, :], 0, 2048)])
        taps = [(kc, k) for kc in range(KC) for k in range(CONV_K)]
        for b in range(16):
            precast_diag(base + 20 + b, j, taps[b * 16:(b + 1) * 16])
        for q in range(8):
            precast(base + 36 + q, [(bpad_d[j, q, :, :], 0, 1024), (cprime_d[j, q, :, :], 1024, 1024)],
                    [cprime_reg[j]])
        for b in range(4):
            precast(base + 44 + b, [(w_glu_d[j, b, :, :], 0, 2048)])
        for b in range(4):
            precast(base + 48 + b, [(w_pw_d[j, b, :, :], 0, 2048)])
        for b in range(8):
            precast(base + 52 + b, [(w_out_e[j, b, :, :], 0, 2048)])
    for j in range(n_o):
        base = ne1 * EB + j * 32
        for b in range(24):
            precast(base + b, [(w_in_o[j, b, :, :], 0, 2048)])
        for b in range(8):
            precast(base + 24 + b, [(w_out_o[j, b, :, :], 0, 2048)])
    handoff(big32)
    handoff(inner)

    def norm_stats(src_sq, nchunks, ones_idx, out_rstd, out_reg):
        p = psum()
        for kc in range(nchunks):
            mm(p[:, :], ones_m[:, ones_idx, :], src_sq[:, kc, :], kc == 0, kc == nchunks - 1,
               [ones_m.reg, src_sq.reg], [p.reg])
        rsqrt_eps(out_rstd, p[:, :], [p.reg], out_reg)

    def rsqrt_eps(out_ap, in_ap, reads, out_reg):
        act(out_ap, in_ap, AF.Ln, reads, [out_reg], bias=eps_col[:, 0:1])
        act(out_ap, out_ap, AF.Exp, [out_reg], [out_reg], scale=-0.5)

    def pre_norm(li):
        for kc in range(KC):
            act(sq[:, kc, :], xT[:, kc, :], AF.Square, [xT.reg], [sq.reg])
        norm_stats(sq, KC, 0, rstd[:, :], rstd.reg)
        for kc in range(KC):
            stt("dve", un[:, kc, :], xT[:, kc, :], npre[:, li, kc:kc + 1], rstd[:, :],
                ALU.mult, ALU.mult, [xT.reg, npre.reg, rstd.reg], [un.reg])

    def proj_fm(slot, m, src, nk, evac):
        p = psum()
        cw = 2048 // nk
        for kc in range(nk):
            mm(p[:, :], wbf[:, slot, kc * cw + m * 128: kc * cw + (m + 1) * 128], src[:, kc, :],
               kc == 0, kc == nk - 1, [wbf.slots[slot], src.reg], [p.reg])
        evac(p)

    def out_proj_and_residual(li):
        for b in range(8):
            slot = W.get()

            def ev(p, b=b):
                cp("act", ybuf[:, b, :], p[:, :], [p.reg], [ybuf.reg])
                act(sq[:, b, :], p[:, :], AF.Square, [p.reg], [sq.reg])
            proj_fm(slot, 0, inner, 16, ev)
        norm_stats(sq, KC, 0, rstd[:, :], rstd.reg)
        for kc in range(KC):
            stt("dve", ybuf[:, kc, :], ybuf[:, kc, :], npost[:, li, kc:kc + 1], rstd[:, :],
                ALU.mult, ALU.mult, [ybuf.reg, npost.reg, rstd.reg], [ybuf.reg])
            tt("pool", xT[:, kc, :], xT[:, kc, :], ybuf[:, kc, :], ALU.add,
               [xT.reg, ybuf.reg], [xT.reg])

    GELU_C = 2.0 * math.sqrt(2.0 / math.pi)

    def even_layer(li, j):
        S.fence()
        pre_norm(li)
        hp = hpad
        for i in range(4):
            mset("pool", hs[i][:, 0:PAD], 0.0, [hs[i].reg])
        cp("pool", hp[:, :, 0:HALO], halo[j][:, :, :], [halo[j].reg], [hp.reg])
        for b in range(20):
            slot = W.get()
            sec, bb = divmod(b, 4)
            for m in range(2):
                oc = bb * 2 + m
                if sec == 0:
                    ev = lambda p, oc=oc: cp("act", a_in[:, oc, :], p[:, :], [p.reg], [a_in.reg])
                elif sec == 1:
                    ev = lambda p, oc=oc: act(saz[:, oc, :], p[:, :], AF.Silu, [p.reg], [saz.reg])
                elif sec == 2:
                    ev = lambda p, oc=oc: cp("dve", valb[:, oc, :], p[:, :], [p.reg], [valb.reg])
                elif sec == 3:
                    def ev(p, oc=oc):
                        act(tmpA[:, :], p[:, :], AF.Sigmoid, [p.reg], [tmpA.reg])
                        tt("dve", hp[:, oc, HALO:HALO + T], valb[:, oc, :], tmpA[:, :], ALU.mult,
                           [valb.reg, tmpA.reg], [hp.reg])
                else:
                    ev = lambda p, oc=oc: act(sbz[:, oc, :], p[:, :], AF.Silu, [p.reg], [sbz.reg])
                proj_fm(slot, m, un, KC, ev)
        cidx = 0
        cslot_ = None
        for kc in range(KC):
            p = psum()
            for k in range(CONV_K):
                if cidx % 16 == 0:
                    cslot_ = W.get()
                m = cidx % 16
                mm(p[:, :], wbf[:, cslot_, m * 128:(m + 1) * 128], hp[:, kc, k:k + T], k == 0, k == CONV_K - 1,
                   [wbf.slots[cslot_], hp.reg], [p.reg])
                cidx += 1
            act(acc[:, kc, :], p[:, :], AF.Identity, [p.reg, evec.reg], [acc.reg],
                bias=evec[:, j, 2, kc:kc + 1])
        cp("act", halo[j][:, :, :], hp[:, :, T:T + HALO], [hp.reg], [halo[j].reg])
        bslot = {}
        cslot = {}

        def bu(P):
            q = P // 4
            if P % 4 == 0:
                bslot[q] = W.get()
            sl = bslot[q]
            kc = P // 4
            pr, pi_ = psum(), psum()
            off = (P % 4) * 256
            mm(pr[:, :], wbf[:, sl, off:off + 128], a_in[:, kc, :], True, True,
               [wbf.slots[sl], a_in.reg], [pr.reg])
            mm(pi_[:, :], wbf[:, sl, off + 128:off + 256], a_in[:, kc, :], True, True,
               [wbf.slots[sl], a_in.reg], [pi_.reg])
            cp("act", hs[0][:, PAD:PAD + T], pr[:, :], [pr.reg], [hs[0].reg])
            cp("act", hs[1][:, PAD:PAD + T], pi_[:, :], [pi_.reg], [hs[1].reg])
            cp("act", hs[0][:, PAD - 1:PAD], carry[j][:, P, 0:1], [carry[j].reg], [hs[0].reg])
            cp("act", hs[1][:, PAD - 1:PAD], carry[j][:, P, 1:2], [carry[j].reg], [hs[1].reg])

        def scan(P):
            A = (hs[0], hs[1])
            B = (hs[2], hs[3])
            lo, hi = PAD - 1, PAD + T
            for k in range(9):
                d = 1 << k
                rsh, ish = A[0][:, lo - d:hi - d], A[1][:, lo - d:hi - d]
                rd = [A[0].reg, A[1].reg, pwr[j].reg, pwi[j].reg, npwi[j].reg]
                pr_, pi_, npi_ = pwr[j][:, k, P:P + 1], pwi[j][:, k, P:P + 1], npwi[j][:, k, P:P + 1]
                stt("dve", B[0][:, lo:hi], rsh, pr_, A[0][:, lo:hi], ALU.mult, ALU.add, rd, [B[0].reg])
                stt("dve", B[0][:, lo:hi], ish, npi_, B[0][:, lo:hi], ALU.mult, ALU.add,
                    rd + [B[0].reg], [B[0].reg])
                stt("dve", B[1][:, lo:hi], rsh, pi_, A[1][:, lo:hi], ALU.mult, ALU.add, rd, [B[1].reg])
                stt("dve", B[1][:, lo:hi], ish, pr_, B[1][:, lo:hi], ALU.mult, ALU.add,
                    rd + [B[1].reg], [B[1].reg])
                A, B = B, A
            cp("act", sbre[:, :], A[0][:, PAD:PAD + T], [A[0].reg], [sbre.reg])
            cp("act", sbim[:, :], A[1][:, PAD:PAD + T], [A[1].reg], [sbim.reg])
            cp("act", carry[j][:, P, 0:1], A[0][:, PAD + T - 1:PAD + T], [A[0].reg], [carry[j].reg])
            cp("act", carry[j][:, P, 1:2], A[1][:, PAD + T - 1:PAD + T], [A[1].reg], [carry[j].reg])

        ypsum = {}

        def yout(P):
            sl = bslot[P // 4]
            kc = P // 4
            if P % 4 == 0:
                ypsum[kc] = psum()
            yp = ypsum[kc]
            off = 1024 + (P % 4) * 256
            mm(yp[:, :], wbf[:, sl, off:off + 128], sbre[:, :], P % 4 == 0, False,
               [wbf.slots[sl], sbre.reg], [yp.reg])
            mm(yp[:, :], wbf[:, sl, off + 128:off + 256], sbim[:, :], False, P % 4 == 3,
               [wbf.slots[sl], sbim.reg], [yp.reg])
            if P % 4 == 3:
                stt("dve", tmpA[:, :], a_in[:, kc, :], evec[:, j, 0, kc:kc + 1], yp[:, :],
                    ALU.mult, ALU.add, [a_in.reg, evec.reg, yp.reg], [tmpA.reg])
                tt("dve", tmpB[:, :], tmpA[:, :], tmpA[:, :], ALU.mult, [tmpA.reg], [tmpB.reg])
                ts("dve", tmpB[:, :], tmpB[:, :], 0.044715, 1.0, ALU.mult, ALU.add, [tmpB.reg], [tmpB.reg])
                tt("dve", tmpB[:, :], tmpB[:, :], tmpA[:, :], ALU.mult, [tmpA.reg, tmpB.reg], [tmpB.reg])
                act(tmpB[:, :], tmpB[:, :], AF.Sigmoid, [tmpB.reg], [tmpB.reg], scale=GELU_C)
                tt("dve", ya[:, kc, :], tmpA[:, :], tmpB[:, :], ALU.mult, [tmpA.reg, tmpB.reg], [ya.reg])

        for P in range(32):
            bu(P)
            scan(P)
            yout(P)
        for b in range(4):
            slot = W.get()
            for m in range(2):
                oc = b * 2 + m

                def ev(p, oc=oc):
                    act(tmpA[:, :], p[:, :], AF.Sigmoid, [p.reg, evec.reg], [tmpA.reg],
                        bias=evec[:, j, 1, oc:oc + 1])
                    tt("dve", tmpA[:, :], tmpA[:, :], ya[:, oc, :], ALU.mult, [tmpA.reg, ya.reg], [tmpA.reg])
                    tt("dve", inner[:, oc, :], tmpA[:, :], saz[:, oc, :], ALU.mult,
                       [tmpA.reg, saz.reg], [inner.reg])
                proj_fm(slot, m, ya, KC, ev)
        for kc in range(KC):
            cp("act", accb[:, kc, :], acc[:, kc, :], [acc.reg], [accb.reg])
            act(sq[:, kc, :], acc[:, kc, :], AF.Square, [acc.reg], [sq.reg])
        pm = psum()
        for kc in range(KC):
            mm(pm[:, :], ones_m[:, 0, :], accb[:, kc, :], kc == 0, kc == KC - 1,
               [ones_m.reg, accb.reg], [pm.reg])
        cp("dve", mean[:, :], pm[:, :], [pm.reg], [mean.reg])
        p2 = psum()
        for kc in range(KC):
            mm(p2[:, :], ones_m[:, 0, :], sq[:, kc, :], kc == 0, kc == KC - 1,
               [ones_m.reg, sq.reg], [p2.reg])
        tt("dve", tmpA[:, :], mean[:, :], mean[:, :], ALU.mult, [mean.reg], [tmpA.reg])
        tt("dve", tmpA[:, :], p2[:, :], tmpA[:, :], ALU.subtract, [p2.reg, tmpA.reg], [tmpA.reg])
        rsqrt_eps(rstd[:, :], tmpA[:, :], [tmpA.reg], rstd.reg)
        for kc in range(KC):
            tt("dve", tmpB[:, :], acc[:, kc, :], mean[:, :], ALU.subtract, [acc.reg, mean.reg], [tmpB.reg])
            tt("dve", tmpB[:, :], tmpB[:, :], rstd[:, :], ALU.mult, [tmpB.reg, rstd.reg], [tmpB.reg])
            act(accb[:, kc, :], tmpB[:, :], AF.Silu, [tmpB.reg, evec.reg], [accb.reg],
                bias=evec[:, j, 4, kc:kc + 1], scale=evec[:, j, 3, kc:kc + 1])
        for b in range(4):
            slot = W.get()
            for m in range(2):
                oc = b * 2 + m

                def ev(p, oc=oc):
                    stt("dve", inner[:, 8 + oc, :], p[:, :], evec[:, j, 5, oc:oc + 1], sbz[:, oc, :],
                        ALU.add, ALU.mult, [p.reg, evec.reg, sbz.reg], [inner.reg])
                proj_fm(slot, m, accb, KC, ev)
        out_proj_and_residual(li)

    def ktok_ap(s, c0, n):
        return G[2][:, 2 * s + c0 // 512, c0 % 512:c0 % 512 + n]

    def vtok_ap(s, c0, n):
        return G[3 + s // 2][:, (s % 2) * 4 + c0 // 512, c0 % 512:c0 % 512 + n]

    def rT_ap(oc, sl=slice(0, T)):
        return G[5 + oc // 8][:, oc % 8, sl]

    vregs = [G[3].reg, G[4].reg]
    rregs = [G[5].reg, G[6].reg]

    def odd_layer(li, j, tile_idx):
        S.fence()
        pre_norm(li)
        mset("dve", lraug[:, :], 1.0, [lraug.reg])
        if tile_idx == 0:
            mset("pool", gs[:, :, :], 0.0, [gs.reg])
            mset("pool", gsb[:, :, :], 0.0, [gsb.reg])
        else:
            dma(bass.AP(gs.t, 0, [[8 * 512, 128], [1, 8 * 512]]), gstate_d[j, :, :],
                [gstate_reg[j]], [gs.reg], "g")
            for kc in range(8):
                cp("pool", gsb[:, kc, :], gs[:, kc, :], [gs.reg], [gsb.reg])
        if _DBG <= 1:
            return
        for b in range(24):
            slot = W.get()
            if b < 4:
                for m in range(2):
                    oc = b * 2 + m
                    proj_fm(slot, m, un, KC,
                            lambda p, oc=oc: cp("act", qT[:, oc, :], p[:, :], [p.reg], [qT.reg]))
            elif b < 8:
                bb = b - 4
                for m in range(2):
                    oc = bb * 2 + m
                    proj_fm(slot, m, un, KC,
                            lambda p, oc=oc: cp("act", kT[:, oc, :], p[:, :], [p.reg], [kT.reg]))
                for s in range(4):
                    p = psum()
                    for kc in range(KC):
                        mm(p[:, 0:256], un[:, kc, s * 128:(s + 1) * 128], wbf[:, slot, kc * 256:(kc + 1) * 256],
                           kc == 0, kc == KC - 1, [un.reg, wbf.slots[slot]], [p.reg])
                    cp("dve", ktok_ap(s, bb * 256, 256), p[:, 0:256], [p.reg], [G[2].reg])
            elif b < 16:
                bb = b - 8
                for s in range(4):
                    p = psum()
                    for kc in range(KC):
                        mm(p[:, 0:256], un[:, kc, s * 128:(s + 1) * 128], wbf[:, slot, kc * 256:(kc + 1) * 256],
                           kc == 0, kc == KC - 1, [un.reg, wbf.slots[slot]], [p.reg])
                    cp("act" if s % 2 else "dve", vtok_ap(s, bb * 256, 256), p[:, 0:256],
                       [p.reg], [G[3 + s // 2].reg])
            else:
                bb = b - 16
                for m in range(2):
                    oc = bb * 2 + m
                    proj_fm(slot, m, un, KC,
                            lambda p, oc=oc: act(rT_ap(oc), p[:, :], AF.Silu, [p.reg], [G[5 + oc // 8].reg]))
        if _DBG <= 2:
            return
        p = psum()
        for kc in range(KC):
            mm(p[0:16, :], wlr[:, j, kc, :], un[:, kc, :], kc == 0, kc == KC - 1,
               [wlr.reg, un.reg], [p.reg])
        cp("act", lraug[0:16, :], p[0:16, :], [p.reg], [lraug.reg])
        if _DBG <= 3:
            return
        for s in range(4):
            tsl = slice(s * 128, (s + 1) * 128)
            for h2 in range(2):
                p = psum()
                mm(p[:, :], lraug[:, tsl], wgaug[:, j, h2 * 512:(h2 + 1) * 512], True, True,
                   [lraug.reg, wgaug.reg], [p.reg])
                act(etmp[:, h2 * 512:(h2 + 1) * 512], p[:, :], AF.Exp, [p.reg], [etmp.reg], scale=-1.0)
            act(sptok[:, :], etmp[:, :], AF.Ln, [etmp.reg], [sptok.reg], bias=1.0)
            for g4 in range(2):
                p = psum()
                for c in range(4):
                    kc = g4 * 4 + c
                    mm(p[:, c * 128:(c + 1) * 128], sptok[:, kc * 128:(kc + 1) * 128], tri[:, 0, :],
                       True, True, [sptok.reg, tri.reg], [p.reg])
                ebv = bass.AP(eb.t, g4 * 512, [[KC * 128, 128], [1, 512]])
                eiv = bass.AP(einv.t, g4 * 512, [[KC * 128, 128], [1, 512]])
                act(ebv, p[:, :], AF.Exp, [p.reg], [eb.reg], scale=-1.0 / 16.0)
                act(eiv, p[:, :], AF.Exp, [p.reg], [einv.reg], scale=1.0 / 16.0)
            for kc in range(KC):
                stt("dve", qdec[:, kc, :], qT[:, kc, tsl], 1.0 / 16.0, eb[:, kc, :], ALU.mult, ALU.mult,
                    [qT.reg, eb.reg], [qdec.reg])
                tt("pool", kdec[:, kc, :], kT[:, kc, tsl], einv[:, kc, :], ALU.mult,
                   [kT.reg, einv.reg], [kdec.reg])
            for h2 in range(2):
                p = psum()
                mm(p[:, :], tri[:, 1, :], sptok[:, h2 * 512:(h2 + 1) * 512], True, True,
                   [tri.reg, sptok.reg], [p.reg])
                act(etmp[:, h2 * 512:(h2 + 1) * 512], p[:, :], AF.Exp, [p.reg], [etmp.reg], scale=-1.0 / 16.0)
            for h2 in range(2):
                tt("dve", k2[:, h2 * 512:(h2 + 1) * 512], ktok_ap(s, h2 * 512, 512),
                   etmp[:, h2 * 512:(h2 + 1) * 512], ALU.mult, [G[2].reg, etmp.reg], [k2.reg])
            if _DBG <= 4:
                continue
            pa = psum()
            for h in range(4):
                for c in range(2):
                    mm(pa[:, h * 128:(h + 1) * 128], kdec[:, 2 * h + c, :], qdec[:, 2 * h + c, :],
                       c == 0, c == 1, [kdec.reg, qdec.reg], [pa.reg])
            for h in range(4):
                tt("dve", attn[:, h, :], pa[:, h * 128:(h + 1) * 128], tri[:, 2, :], ALU.mult,
                   [pa.reg, tri.reg], [attn.reg])
            if _SUB <= 1:
                continue
            for h in range(4):
                po = psum()
                for vc in range(4):
                    vcol = h * 512 + vc * 128
                    mm(po[:, vc * 128:(vc + 1) * 128], vtok_ap(s, vcol, 128), attn[:, h, :],
                       True, "a" in _VAR, vregs + [attn.reg], [po.reg])
                    if "a" in _VAR:
                        continue
                    for c in range(2):
                        mm(po[:, vc * 128:(vc + 1) * 128], gsb[:, 2 * h + c, vc * 128:(vc + 1) * 128],
                           qdec[:, 2 * h + c, :], False, c == 1, [gsb.reg, qdec.reg], [po.reg])
                osb_v = bass.AP(osb.t, 0, [[512, 128], [1, 512]])
                osq_v = bass.AP(osq.t, 0, [[512, 128], [1, 512]])
                if "b" in _VAR:
                    continue
                cp("dve", osb_v, po[:, :], [po.reg], [osb.reg])
                if "c" in _VAR:
                    continue
                act(osq_v, osb_v, AF.Square, [osb.reg], [osq.reg])
                if _SUB <= 2:
                    continue
                pn = psum()
                for vc in range(4):
                    mm(pn[:, 0:128], ones_m[:, 1, :], osq[:, vc, :], vc == 0, vc == 3,
                       [ones_m.reg, osq.reg], [pn.reg])
                rsqrt_eps(orstd[:, :], pn[:, 0:128], [pn.reg], orstd.reg)
                for vc in range(4):
                    stt("dve", osb[:, vc, :], osb[:, vc, :], gng[:, j, vc:vc + 1], orstd[:, :],
                        ALU.mult, ALU.mult, [osb.reg, gng.reg, orstd.reg], [osb.reg])
                    tt("pool", inner[:, h * 4 + vc, tsl], osb[:, vc, :], rT_ap(h * 4 + vc, tsl), ALU.mult,
                       [osb.reg] + rregs, [inner.reg])
                if _SUB <= 3:
                    continue
                for c in range(2):
                    kc = 2 * h + c
                    pst = psum()
                    mm(pst[:, :], k2[:, kc * 128:(kc + 1) * 128], vtok_ap(s, h * 512, 512),
                       True, True, [k2.reg] + vregs, [pst.reg])
                    stt("dve", gs[:, kc, :], gs[:, kc, :], eb[:, kc, 127:128], pst[:, :],
                        ALU.mult, ALU.add, [gs.reg, eb.reg, pst.reg], [gs.reg])
                    cp("act", gsb[:, kc, :], gs[:, kc, :], [gs.reg], [gsb.reg])
        if _DBG <= 5:
            return
        dma(gstate_d[j, :, :], bass.AP(gs.t, 0, [[8 * 512, 128], [1, 8 * 512]]),
            [gs.reg], [gstate_reg[j]], "g")
        if _DBG <= 6:
            return
        out_proj_and_residual(li)

    for t in range(NT):
        ie = io = 0
        for kind in layers:
            if kind == "e":
                for b in range(EB):
                    W.push(ie * EB + b)
                ie += 1
            else:
                for b in range(32):
                    W.push(ne1 * EB + io * 32 + b)
                io += 1

    for t in range(NT):
        for kc in range(KC):
            dma(xT[:, kc, :], xT_d[:, kc, t * T:(t + 1) * T], [], [xT.reg], "x")
        ie = io = 0
        for li, kind in enumerate(layers):
            if kind == "e":
                even_layer(li, ie)
                ie += 1
            else:
                odd_layer(li, io, t)
                io += 1
        for kc in range(KC):
            dma(yT_d[:, kc, t * T:(t + 1) * T], xT[:, kc, :], [xT.reg], [], "y")

    S.emit(nc, ["y"])
    es.close()
    return nc


def _wblocks(w, cw):
    din, dout = w.shape
    kc = din // 128
    nb = dout // cw
    a = w.reshape(kc, 128, nb, cw).transpose(2, 1, 0, 3)
    return np.ascontiguousarray(a.reshape(nb, 128, kc * cw))


def _vec(v):
    return np.ascontiguousarray(v.reshape(-1, 128).T)


def _pack(inp, layers):
    f = np.float32
    nl = len(layers)
    even = [i for i, k in enumerate(layers) if k == "e"]
    odd = [i for i, k in enumerate(layers) if k == "o"]
    ne1, no1 = max(len(even), 1), max(len(odd), 1)
    out = {}
    out["npre"] = np.ascontiguousarray(np.stack([_vec(inp["norm_pre"][i]) for i in range(max(nl, 1))], 1)).astype(f)
    out["npost"] = np.ascontiguousarray(np.stack([_vec(inp["norm_post"][i]) for i in range(max(nl, 1))], 1)).astype(f)
    out["ident"] = np.eye(128, dtype=f)
    jj, tt_ = np.meshgrid(np.arange(128), np.arange(128), indexing="ij")
    tri = np.zeros((128, 3, 128), f)
    tri[:, 0, :] = (jj <= tt_)
    tri[:, 1, :] = (jj > tt_)
    tri[:, 2, :] = (jj <= tt_)
    out["tri"] = tri
    evec = np.zeros((128, ne1, 6, KC), f)
    convw = np.zeros((128, ne1, KC, CONV_K), f)
    s5p = np.zeros((128, ne1, 3, 32), f)
    cpad = np.zeros((ne1, 128, 32, 2, 128), f)
    bpad = np.zeros((ne1, 8, 128, 4, 2, 128), f)
    w_in_e = np.zeros((ne1, 20, 128, 2048), f)
    w_glu = np.zeros((ne1, 4, 128, 2048), f)
    w_pw = np.zeros((ne1, 4, 128, 2048), f)
    w_out_e = np.zeros((ne1, 8, 128, 2048), f)
    for j in range(len(even)):
        for k, nm in enumerate(["s5_d", "s5_b_glu", "conv_b", "conv_ln_g", "conv_ln_b", "conv_b_pw"]):
            evec[:, j, k, :] = _vec(inp[nm][j])
        convw[:, j, :, :] = inp["conv_w"][j].T.reshape(KC, 128, CONV_K).transpose(1, 0, 2)
        for a in range(2):
            s5p[64 * a:64 * a + 64, j, 0, :] = inp["s5_lambda_re"][j][a::2, :].T
            s5p[64 * a:64 * a + 64, j, 1, :] = inp["s5_lambda_im"][j][a::2, :].T
            s5p[64 * a:64 * a + 64, j, 2, :] = inp["s5_log_dt"][j][a::2][None, :]
        for P in range(32):
            r = P % 4
            for a in range(2):
                g = 2 * P + a
                for ri, (bn, cn) in enumerate([("s5_b_re", "s5_c_re"), ("s5_b_im", "s5_c_im")]):
                    bpad[j, P // 4, 32 * r + 16 * a:32 * r + 16 * a + 16, P % 4, ri, 64 * a:64 * a + 64] = \
                        inp[bn][j, g].T
                    cpad[j, 64 * a:64 * a + 64, P, ri, 32 * r + 16 * a:32 * r + 16 * a + 16] = \
                        inp[cn][j, g].T
        w_in_e[j] = _wblocks(inp["ev_w_in"][j], 256)
        w_glu[j] = _wblocks(inp["s5_w_glu"][j], 256)
        w_pw[j] = _wblocks(inp["conv_w_pw"][j], 256)
        w_out_e[j] = _wblocks(inp["ev_w_out"][j], 128)
    out.update(evec=evec, convw=convw, s5p=s5p, cpad=cpad.reshape(ne1, 128, 32 * 2 * 128),
               bpad=bpad.reshape(ne1, 8, 128, 1024), w_in_e=w_in_e, w_glu=w_glu, w_pw=w_pw,
               w_out_e=w_out_e)
    w_in_o = np.zeros((no1, 24, 128, 2048), f)
    w_out_o = np.zeros((no1, 8, 128, 2048), f)
    wlr = np.zeros((128, no1, KC, 16), f)
    wgaug = np.zeros((32, no1, 1024), f)
    gng = np.zeros((128, no1, 4), f)
    for j in range(len(odd)):
        w = inp["od_w_in"][j]
        w_in_o[j] = _wblocks(np.ascontiguousarray(w[:, :6144]), 256)
        wlr[:, j, :, :] = w[:, 6144:6160].reshape(KC, 128, 16).transpose(1, 0, 2)
        wgaug[0:16, j, :] = inp["gla_w_gate_up"][j]
        wgaug[16, j, :] = inp["gla_b_gate"][j]
        gng[:, j, :] = _vec(inp["gla_norm_g"][j])
        w_out_o[j] = _wblocks(inp["od_w_out"][j], 128)
    out.update(w_in_o=w_in_o, w_out_o=w_out_o, wlr=wlr, wgaug=wgaug, gng=gng)
    return out


_LAYERS = ["e", "o", "e", "o"]


def run_layers(inp, layers, n_cores=8):
    inp = {k: np.asarray(v, dtype=np.float32) for k, v in inp.items()}
    x = inp["x"]
    B, L, _ = x.shape
    shared = _pack(inp, layers)
    nc = build_program(L, layers)
    in_maps = []
    for c in range(n_cores):
        b = c % B
        m = dict(shared)
        m["xT"] = np.ascontiguousarray(x[b].T.reshape(KC, 128, L).transpose(1, 0, 2))
        in_maps.append(m)
    res = run_bass_kernel_spmd(nc, in_maps, core_ids=list(range(n_cores)))
    out = np.empty((B, L, D), np.float32)
    for b in range(B):
        yT = np.asarray(res.results[b]["yT"])
        out[b] = yT.transpose(1, 0, 2).reshape(D, L).T
    return out


def kernel(**inputs):
    return run_layers(inputs, _LAYERS)
```

```python
import math
import os
from contextlib import ExitStack

_DBG = int(os.environ.get("KDBG", "99"))
_SUB = int(os.environ.get("KSUB", "99"))
_VAR = os.environ.get("KVAR", "")

import numpy as np
import concourse.bass as bass
import concourse.mybir as mybir
from concourse.bass_utils import run_bass_kernel_spmd

F32 = mybir.dt.float32
BF16 = mybir.dt.bfloat16
ALU = mybir.AluOpType
AF = mybir.ActivationFunctionType

D = 1024
KC = 8
T = 512
EPS = 1e-6
CONV_K = 31
HALO = CONV_K - 1
PAD = 264
TWO_PI = 2.0 * math.pi


class Reg:
    __slots__ = ("name", "w", "r", "rd")

    def __init__(self, name):
        self.name = name
        self.w = None
        self.r = {}
        self.rd = []


class Op:
    __slots__ = ("eng", "fn", "deps", "need_inc", "val", "dsem", "is_dma")

    def __init__(self, eng, fn, dsem=None):
        self.eng = eng
        self.fn = fn
        self.deps = []
        self.need_inc = False
        self.val = 0
        self.dsem = dsem
        self.is_dma = dsem is not None


ENGS = ("pe", "act", "dve", "pool", "sp")


class Sched:
    def __init__(self):
        self.ops = {e: [] for e in ENGS}
        self.dsem_names = []
        self.pending = {}

    def fence(self, engs=("pe", "act", "dve", "pool")):
        last = [self.ops[e][-1] for e in engs if self.ops[e]]
        for e in engs:
            self.pending.setdefault(e, []).extend(last)

    def _add_dep(self, o, d):
        if d is None or d is o:
            return
        if (not d.is_dma) and d.eng == "pe" and o.eng == "pe" and not o.is_dma:
            return
        o.deps.append(d)

    def op(self, eng, fn, reads=(), writes=(), dsem=None):
        o = Op(eng, fn, dsem)
        if eng in self.pending and not o.is_dma:
            for d in self.pending.pop(eng):
                if d.eng != eng:
                    self._add_dep(o, d)
        for r in reads:
            self._add_dep(o, r.w)
        for w in writes:
            self._add_dep(o, w.w)
            for d in w.r.values():
                self._add_dep(o, d)
            for d in w.rd:
                self._add_dep(o, d)
        for r in reads:
            if o.is_dma:
                r.rd.append(o)
            else:
                r.r[eng] = o
        for w in writes:
            w.w = o
            w.r = {}
            w.rd = []
        if dsem is not None and dsem not in self.dsem_names:
            self.dsem_names.append(dsem)
        self.ops[eng].append(o)
        return o

    def emit(self, nc, final_waits):
        with ExitStack() as es:
            esem = {e: es.enter_context(nc.semaphore("s_" + e)) for e in ENGS}
            dsem = {n: es.enter_context(nc.semaphore("d_" + n)) for n in self.dsem_names}
            for e in ENGS:
                for o in self.ops[e]:
                    for d in o.deps:
                        if not d.is_dma:
                            d.need_inc = True
            for e in ENGS:
                c = 0
                for o in self.ops[e]:
                    if o.is_dma:
                        continue
                    if o.need_inc:
                        c += 1
                        o.val = c
            dcount = {n: 0 for n in self.dsem_names}
            for e in ENGS:
                for o in self.ops[e]:
                    if o.is_dma:
                        dcount[o.dsem] += 16
                        o.val = dcount[o.dsem]
            block = es.enter_context(nc.Block())

            def run(engname, eobj):
                waited = {}
                for o in self.ops[engname]:
                    need = {}
                    for d in o.deps:
                        s = dsem[d.dsem] if d.is_dma else esem[d.eng]
                        k = id(s)
                        if k not in need or need[k][1] < d.val:
                            need[k] = (s, d.val)
                    for k, (s, v) in need.items():
                        if waited.get(k, 0) < v:
                            eobj.wait_ge(s, v)
                            waited[k] = v
                    ins = o.fn(eobj)
                    if o.is_dma:
                        ins.then_inc(dsem[o.dsem], 16)
                    elif o.need_inc:
                        ins.then_inc(esem[engname], 1)
                if engname == "sp":
                    for n in final_waits:
                        eobj.wait_ge(dsem[n], dcount[n])

            @block.tensor
            def _(e):
                run("pe", e)

            @block.scalar
            def _(e):
                run("act", e)

            @block.vector
            def _(e):
                run("dve", e)

            @block.gpsimd
            def _(e):
                run("pool", e)

            @block.sync
            def _(e):
                run("sp", e)


class Buf:
    def __init__(self, t, name, nslots=0):
        self.t = t
        self.reg = Reg(name)
        self.slots = [Reg("%s.%d" % (name, i)) for i in range(nslots)]

    def __getitem__(self, k):
        return self.t[k]


def build_program(L, layers):
    assert L % T == 0
    NT = L // T
    n_e = sum(1 for k in layers if k == "e")
    n_o = sum(1 for k in layers if k == "o")
    nl = len(layers)
    nc = bass.Bass("TRN2", target_bir_lowering=False)
    S = Sched()

    def din(name, shape, dt=F32):
        return nc.dram_tensor(name, list(shape), dt, kind="ExternalInput").ap()

    xT_d = din("xT", [128, KC, L])
    yT_d = nc.dram_tensor("yT", [128, KC, L], F32, kind="ExternalOutput").ap()
    npre_d = din("npre", [128, max(nl, 1), KC])
    npost_d = din("npost", [128, max(nl, 1), KC])
    ident_d = din("ident", [128, 128])
    tri_d = din("tri", [128, 3, 128])
    ne1, no1 = max(n_e, 1), max(n_o, 1)
    evec_d = din("evec", [128, ne1, 6, KC])
    convw_d = din("convw", [128, ne1, KC, CONV_K])
    s5p_d = din("s5p", [128, ne1, 3, 32])
    cpad_d = din("cpad", [ne1, 128, 32 * 2 * 128])
    w_in_e = din("w_in_e", [ne1, 20, 128, 2048])
    bpad_d = din("bpad", [ne1, 8, 128, 1024])
    w_glu_d = din("w_glu", [ne1, 4, 128, 2048])
    w_pw_d = din("w_pw", [ne1, 4, 128, 2048])
    w_out_e = din("w_out_e", [ne1, 8, 128, 2048])
    w_in_o = din("w_in_o", [no1, 24, 128, 2048])
    w_out_o = din("w_out_o", [no1, 8, 128, 2048])
    wlr_d = din("wlr", [128, no1, KC, 16])
    wgaug_d = din("wgaug", [32, no1, 1024])
    gng_d = din("gng", [128, no1, 4])
    cprime_d = nc.dram_tensor("cprime", [ne1, 8, 128, 1024], F32, kind="ExternalOutput").ap()
    cprime_reg = [Reg("cprime%d" % j) for j in range(ne1)]
    EB = 60
    NUQ = ne1 * EB + no1 * (24 + 8)
    wq_d = nc.dram_tensor("wq", [NUQ, 128, 2048], BF16, kind="Internal").ap()
    wq_reg = [Reg("wq%d" % i) for i in range(NUQ)]

    es = ExitStack()

    def sb(name, shape, dt=F32, nslots=0):
        return Buf(es.enter_context(nc.sbuf_tensor(name, list(shape), dt)), name, nslots)

    xT = sb("xTs", [128, KC, T])
    un = sb("un", [128, KC, T], BF16)
    sq = un
    rstd = sb("rstd", [128, T])
    big32 = sb("big32", [128, KC, T], nslots=2)
    valb = acc = ybuf = big32
    inner = sb("inner", [128, 16, T], BF16, nslots=2)
    ones_m = sb("ones_m", [128, 2, 128], BF16)
    eps_col = sb("eps_col", [128, 2])
    ident = sb("identb", [128, 128], BF16)
    tri = sb("trib", [128, 3, 128], BF16)
    NW = 4
    wbf = sb("wbf", [128, NW, 2048], BF16, nslots=NW)
    sct = [sb("sct%d" % i, [128, PAD + T]) for i in range(2)]
    npre = sb("npre_s", [128, max(nl, 1), KC])
    npost = sb("npost_s", [128, max(nl, 1), KC])
    evec = sb("evec_s", [128, ne1, 6, KC])
    convw = sb("convw_s", [128, ne1, KC, CONV_K])
    halo = [sb("halo%d" % j, [128, KC, HALO], BF16) for j in range(ne1)]
    carry = [sb("carry%d" % j, [128, 32, 2]) for j in range(ne1)]
    pwr = [sb("pwr%d" % j, [128, 9, 32]) for j in range(ne1)]
    pwi = [sb("pwi%d" % j, [128, 9, 32]) for j in range(ne1)]
    npwi = [sb("npwi%d" % j, [128, 9, 32]) for j in range(ne1)]
    wlr = sb("wlr_s", [128, no1, KC, 16], BF16)
    wgaug = sb("wgaug_s", [32, no1, 1024], BF16)
    gng = sb("gng_s", [128, no1, 4])
    gs = sb("gs", [128, 8, 512])
    gsb = sb("gsb", [128, 8, 512], BF16)
    gstate_d = nc.dram_tensor("gstate", [no1, 128, 8 * 512], F32, kind="ExternalOutput").ap()
    gstate_reg = [Reg("gstate%d" % j) for j in range(no1)]
    G = [sb("G%d" % i, [128, KC, T], BF16) for i in range(7)]
    a_in, saz, sbz, accb, ya = G[0], G[1], G[2], G[3], G[4]
    qT, kT, ktok_b, vtok_b, rT_b = G[0], G[1], G[2], (G[3], G[4]), (G[5], G[6])

    def phase_alloc(specs):
        out = {}
        with ExitStack() as pes:
            for name, shape, dt in specs:
                out[name] = Buf(pes.enter_context(nc.sbuf_tensor(name, list(shape), dt)), name)
        return out

    EV = phase_alloc([
        ("hpad", [128, KC, HALO + T], BF16),
        ("tmpA", [128, T], F32), ("tmpB", [128, T], F32),
        ("hs0", [128, PAD + T], F32), ("hs1", [128, PAD + T], F32),
        ("hs2", [128, PAD + T], F32), ("hs3", [128, PAD + T], F32),
        ("sbre", [128, T], BF16), ("sbim", [128, T], BF16),
    ])
    hpad, tmpA, tmpB = EV["hpad"], EV["tmpA"], EV["tmpB"]
    hs = [EV["hs0"], EV["hs1"], EV["hs2"], EV["hs3"]]

    class _MeanView:
        reg = hs[2].reg

        def __getitem__(self, k):
            return hs[2][:, 0:T]
    mean = _MeanView()
    sbre, sbim = EV["sbre"], EV["sbim"]
    OD = phase_alloc([
        ("lraug", [32, T], BF16), ("sptok", [128, 1024], BF16), ("etmp", [128, 1024], F32),
        ("k2", [128, 1024], BF16), ("qdec", [128, KC, 128], BF16), ("kdec", [128, KC, 128], BF16),
        ("eb", [128, KC, 128], F32), ("einv", [128, KC, 128], F32), ("attn", [128, 4, 128], BF16),
        ("osb", [128, 4, 128], F32), ("osq", [128, 4, 128], BF16), ("orstd", [128, 128], F32),
        ("s5t", [128, 16, 32], F32), ("s5p_s", [128, ne1, 3, 32], F32),
        ("s5i", [128, 32], mybir.dt.int32),
    ])
    s5t, s5p, s5i = OD["s5t"], OD["s5p_s"], OD["s5i"]
    lraug, sptok, etmp, k2, qdec, kdec = (OD[k] for k in ("lraug", "sptok", "etmp", "k2", "qdec", "kdec"))
    eb, einv, attn, osb, osq, orstd = (OD[k] for k in ("eb", "einv", "attn", "osb", "osq", "orstd"))

    NPS = 8
    ps = [Buf(es.enter_context(nc.psum_tensor("ps%d" % i, [128, 512], F32)), "ps%d" % i)
          for i in range(NPS)]
    ps_i = [0]

    def psum():
        p = ps[ps_i[0] % NPS]
        ps_i[0] += 1
        return p

    uniq = [0]

    def dma(out_ap, in_ap, reads, writes, sem):
        if sem in ("c0", "c1"):
            sem = "c%d" % uniq[0]
            uniq[0] += 1
        return S.op("sp", lambda e: e.dma_start(out=out_ap, in_=in_ap), reads, writes, dsem=sem)

    def mm(out_ap, lhsT, rhs, start, stop, reads, writes):
        return S.op("pe", lambda e: e.matmul(out_ap, lhsT, rhs, start=start, stop=stop),
                    reads, writes)

    def act(out_ap, in_ap, func, reads, writes, bias=None, scale=None):
        kw = {}
        if bias is not None:
            kw["bias"] = bias
        if scale is not None:
            kw["scale"] = scale
        return S.op("act", lambda e: e.activation(out_ap, in_ap, func, **kw), reads, writes)

    def tt(eng, out_ap, a, b, op, reads, writes):
        return S.op(eng, lambda e: e.tensor_tensor(out_ap, a, b, op), reads, writes)

    def ts(eng, out_ap, a, s1, s2, op0, op1, reads, writes):
        if s2 is None:
            return S.op(eng, lambda e: e.tensor_scalar(out_ap, a, s1, None, op0), reads, writes)
        return S.op(eng, lambda e: e.tensor_scalar(out_ap, a, s1, s2, op0, op1), reads, writes)

    def stt(eng, out_ap, a, sc, b, op0, op1, reads, writes):
        eng = "dve"
        return S.op(eng, lambda e: e.scalar_tensor_tensor(out_ap, a, sc, b, op0, op1),
                    reads, writes)

    def cp(eng, out_ap, in_ap, reads, writes):
        if eng == "act":
            return S.op("act", lambda e: e.copy(out_ap, in_ap), reads, writes)
        return S.op(eng, lambda e: e.tensor_copy(out_ap, in_ap), reads, writes)

    def mset(eng, ap, v, writes):
        return S.op(eng, lambda e: e.memset(ap, v), (), writes)

    class WStream:
        def __init__(self):
            self.blocks = []
            self.n_dma = 0
            self.n_get = 0

        def push(self, u):
            self.blocks.append(u)

        def _dma_to(self, i):
            while self.n_dma <= i and self.n_dma < len(self.blocks):
                b = self.n_dma
                u = self.blocks[b]
                sl = b % NW
                dma(wbf[:, sl, :], wq_d[u, :, :], [wq_reg[u]], [wbf.slots[sl]], "w%d" % sl)
                self.n_dma += 1

        def get(self):
            i = self.n_get
            self.n_get += 1
            self._dma_to(i + NW - 1)
            return i % NW

    W = WStream()
    cast_rr = [0]

    NPC = 4
    stage_t = [big32.t, big32.t, xT.t, xT.t]
    stage_o = [0, 2048, 0, 2048]
    stage_r = [big32.slots[0], big32.slots[1], Reg("xTs0"), Reg("xTs1")]
    inner_r = [Reg("inner_pc%d" % i) for i in range(NPC)]

    def precast(u, parts, regs=()):
        k = cast_rr[0] % NPC
        cast_rr[0] += 1
        st_ = bass.AP(stage_t[k], stage_o[k], [[KC * T, 128], [1, 2048]])
        ob_ = bass.AP(inner.t, k * 2048, [[16 * T, 128], [1, 2048]])
        sreg, oreg = stage_r[k], inner_r[k]
        for ap, c0, n in parts:
            dma(bass.AP(stage_t[k], stage_o[k] + c0, [[KC * T, 128], [1, n]]), ap, list(regs), [sreg], "pc%d" % k)
        eng = ("pool", "act", "dve")[cast_rr[0] % 3]
        cp(eng, ob_, st_, [sreg], [oreg])
        dma(wq_d[u, :, :], ob_, [oreg], [wq_reg[u]], "pw%d" % k)

    def load_small(dst, src_ap, sem="c0"):
        dma(dst.t[:], src_ap, (), [dst.reg], sem)

    load_small(npre, npre_d)
    load_small(npost, npost_d)
    st_i = bass.AP(big32.t, 0, [[KC * T, 128], [1, 128]])
    dma(st_i, ident_d, (), [big32.reg], "c0")
    cp("dve", ident[:, :], st_i, [big32.reg], [ident.reg])
    st_t = bass.AP(big32.t, 0, [[KC * T, 128], [1, 384]])
    dma(st_t, tri_d.rearrange("p a b -> p (a b)"), [], [big32.reg], "c0")
    cp("dve", bass.AP(tri.t, 0, [[384, 128], [1, 384]]), st_t, [big32.reg], [tri.reg])
    mset("dve", eps_col[:, :], EPS, [eps_col.reg])
    mset("dve", ones_m[:, 0, :], 1.0 / 1024.0, [ones_m.reg])
    mset("dve", ones_m[:, 1, :], 1.0 / 512.0, [ones_m.reg])
    if n_e:
        load_small(evec, evec_d)
        load_small(convw, convw_d)
        load_small(s5p, s5p_d)
        for j in range(n_e):
            mset("pool", halo[j][:, :, :], 0.0, [halo[j].reg])
            mset("pool", carry[j][:, :, :], 0.0, [carry[j].reg])
    if n_o:
        nw = no1 * KC * 16
        wl_f = bass.AP(big32.t, 0, [[KC * T, 128], [1, nw]])
        dma(wl_f, wlr_d.rearrange("p j k c -> p (j k c)"), [], [big32.reg], "c0")
        cp("dve", bass.AP(wlr.t, 0, [[nw, 128], [1, nw]]), wl_f, [big32.reg], [wlr.reg])
        ng = no1 * 1024
        wg_f = bass.AP(big32.t, 0, [[KC * T, 32], [1, ng]])
        dma(wg_f, wgaug_d.rearrange("p j c -> p (j c)"), [], [big32.reg], "c0")
        cp("dve", bass.AP(wgaug.t, 0, [[ng, 32], [1, ng]]), wg_f, [big32.reg], [wgaug.reg])
        load_small(gng, gng_d)

    def s5_prologue(j):
        R = [s5t.reg]
        st = lambda k: s5t[:, k, :]
        lre, lim, ldt = s5p[:, j, 0, :], s5p[:, j, 1, :], s5p[:, j, 2, :]
        act(st(0), ldt, AF.Exp, [s5p.reg], R)
        tt("dve", st(1), lre, st(0), ALU.mult, [s5p.reg] + R, R)
        tt("dve", st(2), lim, st(0), ALU.mult, [s5p.reg] + R, R)
        act(st(3), st(1), AF.Exp, R, R)
        def reduce_to_pi(shift):
            RI = R + [s5i.reg]
            ts("dve", st(10), st(2), shift, None, ALU.add, None, R, R)
            ts("dve", st(15), st(10), 1.0 / TWO_PI, None, ALU.mult, None, R, R)
            cp("dve", s5i[:, :], st(15), R, [s5i.reg])
            cp("dve", st(15), s5i[:, :], [s5i.reg], R)
            stt("dve", st(4), st(15), -TWO_PI, st(10), ALU.mult, ALU.add, RI, R)
            ts("dve", st(15), st(4), math.pi, TWO_PI, ALU.is_gt, ALU.mult, R, R)
            tt("dve", st(4), st(4), st(15), ALU.subtract, R, R)
            ts("dve", st(15), st(4), -math.pi, TWO_PI, ALU.is_lt, ALU.mult, R, R)
            tt("dve", st(4), st(4), st(15), ALU.add, R, R)

        reduce_to_pi(0.0)
        act(st(5), st(4), AF.Sin, R, R)
        reduce_to_pi(0.5 * math.pi)
        act(st(6), st(4), AF.Sin, R, R)
        tt("dve", st(7), st(3), st(6), ALU.mult, R, R)
        tt("dve", st(8), st(3), st(5), ALU.mult, R, R)
        P0 = [pwr[j].reg, pwi[j].reg, npwi[j].reg]
        cp("dve", pwr[j][:, 0, :], st(7), R, P0)
        cp("dve", pwi[j][:, 0, :], st(8), R, P0)
        for k in range(8):
            tt("dve", st(10), pwr[j][:, k, :], pwr[j][:, k, :], ALU.mult, P0 + R, R)
            tt("dve", st(15), pwi[j][:, k, :], pwi[j][:, k, :], ALU.mult, P0 + R, R)
            tt("dve", pwr[j][:, k + 1, :], st(10), st(15), ALU.subtract, R + P0, P0)
            tt("dve", st(10), pwr[j][:, k, :], pwi[j][:, k, :], ALU.mult, P0 + R, R)
            ts("dve", pwi[j][:, k + 1, :], st(10), 2.0, None, ALU.mult, None, R + P0, P0)
        ts("dve", npwi[j][:, :, :], pwi[j][:, :, :], -1.0, None, ALU.mult, None, P0, P0)
        ts("dve", st(7), st(7), -1.0, None, ALU.add, None, R, R)
        tt("dve", st(9), lre, lre, ALU.mult, [s5p.reg] + R, R)
        tt("dve", st(10), lim, lim, ALU.mult, [s5p.reg] + R, R)
        tt("dve", st(9), st(9), st(10), ALU.add, R, R)
        S.op("dve", lambda e: e.reciprocal(st(9), st(9)), R, R)
        tt("dve", st(11), st(7), lre, ALU.mult, [s5p.reg] + R, R)
        tt("dve", st(10), st(8), lim, ALU.mult, [s5p.reg] + R, R)
        tt("dve", st(11), st(11), st(10), ALU.add, R, R)
        tt("dve", st(12), st(8), lre, ALU.mult, [s5p.reg] + R, R)
        tt("dve", st(10), st(7), lim, ALU.mult, [s5p.reg] + R, R)
        tt("dve", st(12), st(12), st(10), ALU.subtract, R, R)
        tt("dve", st(13), st(11), st(9), ALU.mult, R, R)
        tt("dve", st(14), st(12), st(9), ALU.mult, R, R)
        ts("dve", st(11), st(13), -1.0, None, ALU.mult, None, R, R)
        ts("dve", st(12), st(14), -1.0, None, ALU.mult, None, R, R)
        for q in range(8):
            cq = big32
            dma(bass.AP(cq.t, 0, [[KC * T, 128], [1, 1024]]),
                cpad_d[j, :, q * 1024:(q + 1) * 1024], [], [cq.reg], "c0")
            dst = big32
            for pp in range(4):
                P = q * 4 + pp
                cre = bass.AP(cq.t, pp * 256, [[KC * T, 128], [1, 128]])
                cim = bass.AP(cq.t, pp * 256 + 128, [[KC * T, 128], [1, 128]])
                ore = bass.AP(dst.t, 2048 + pp * 256, [[KC * T, 128], [1, 128]])
                oim = bass.AP(dst.t, 2048 + pp * 256 + 128, [[KC * T, 128], [1, 128]])
                ts("dve", ore, cre, s5t[:, 13, P:P + 1], None, ALU.mult, None,
                   [cq.reg] + R, [dst.reg])
                stt("dve", ore, cim, s5t[:, 12, P:P + 1], ore, ALU.mult, ALU.add,
                    [cq.reg] + R, [dst.reg])
                ts("dve", oim, cre, s5t[:, 12, P:P + 1], None, ALU.mult, None,
                   [cq.reg] + R, [dst.reg])
                stt("dve", oim, cim, s5t[:, 11, P:P + 1], oim, ALU.mult, ALU.add,
                    [cq.reg] + R, [dst.reg])
            dma(cprime_d[j, q, :, :], bass.AP(dst.t, 2048, [[KC * T, 128], [1, 1024]]),
                [dst.reg], [cprime_reg[j]], "c1")

    for j in range(n_e):
        s5_prologue(j)

    def handoff(buf, slot_regs):
        S.op("dve", lambda e: e.memset(bass.AP(buf.t, 0, [[buf_stride[buf], 128], [1, 1]]), 0.0),
             (), [buf.reg] + list(slot_regs))
    buf_stride = {big32: KC * T, inner: 16 * T, xT: KC * T}

    def handoff_all():
        handoff(big32, stage_r[0:2])
        handoff(xT, stage_r[2:4])
        handoff(inner, inner_r)
    handoff_all()
    def precast_diag(u, j, items):
        k2 = cast_rr[0] % NPC
        cast_rr[0] += 1
        oreg = inner_r[k2]
        for m, (kc, k) in enumerate(items):
            ts("dve", bass.AP(inner.t, k2 * 2048 + m * 128, [[16 * T, 128], [1, 128]]), ident[:, :],
               convw[:, j, kc, k:k + 1], None, ALU.mult, None, [ident.reg, convw.reg], [oreg])
        dma(wq_d[u, :, :], bass.AP(inner.t, k2 * 2048, [[16 * T, 128], [1, 2048]]), [oreg], [wq_reg[u]],
            "pw%d" % k2)

    for j in range(n_e):
        base = j * EB
        for b in range(20):
            precast(base + b, [(w_in_e[j, b, :, :], 0, 2048)])
        taps = [(kc, k) for kc in range(KC) for k in range(CONV_K)]
        for b in range(16):
            precast_diag(base + 20 + b, j, taps[b * 16:(b + 1) * 16])
        for q in range(8):
            precast(base + 36 + q, [(bpad_d[j, q, :, :], 0, 1024), (cprime_d[j, q, :, :], 1024, 1024)],
                    [cprime_reg[j]])
        for b in range(4):
            precast(base + 44 + b, [(w_glu_d[j, b, :, :], 0, 2048)])
        for b in range(4):
            precast(base + 48 + b, [(w_pw_d[j, b, :, :], 0, 2048)])
        for b in range(8):
            precast(base + 52 + b, [(w_out_e[j, b, :, :], 0, 2048)])
    for j in range(n_o):
        base = ne1 * EB + j * 32
        for b in range(24):
            precast(base + b, [(w_in_o[j, b, :, :], 0, 2048)])
        for b in range(8):
            precast(base + 24 + b, [(w_out_o[j, b, :, :], 0, 2048)])
    handoff_all()

    def norm_stats(src_sq, nchunks, ones_idx, out_rstd, out_reg):
        p = psum()
        for kc in range(nchunks):
            mm(p[:, :], ones_m[:, ones_idx, :], src_sq[:, kc, :], kc == 0, kc == nchunks - 1,
               [ones_m.reg, src_sq.reg], [p.reg])
        rsqrt_eps(out_rstd, p[:, :], [p.reg], out_reg)

    def rsqrt_eps(out_ap, in_ap, reads, out_reg):
        act(out_ap, in_ap, AF.Ln, reads, [out_reg], bias=eps_col[:, 0:1])
        act(out_ap, out_ap, AF.Exp, [out_reg], [out_reg], scale=-0.5)

    def pre_norm(li):
        for kc in range(KC):
            act(sq[:, kc, :], xT[:, kc, :], AF.Square, [xT.reg], [sq.reg])
        norm_stats(sq, KC, 0, rstd[:, :], rstd.reg)
        for kc in range(KC):
            stt("dve", un[:, kc, :], xT[:, kc, :], npre[:, li, kc:kc + 1], rstd[:, :],
                ALU.mult, ALU.mult, [xT.reg, npre.reg, rstd.reg], [un.reg])

    def proj_fm(slot, m, src, nk, evac):
        p = psum()
        cw = 2048 // nk
        for kc in range(nk):
            mm(p[:, :], wbf[:, slot, kc * cw + m * 128: kc * cw + (m + 1) * 128], src[:, kc, :],
               kc == 0, kc == nk - 1, [wbf.slots[slot], src.reg], [p.reg])
        evac(p)

    def out_proj_and_residual(li):
        for b in range(8):
            slot = W.get()

            def ev(p, b=b):
                cp("act", ybuf[:, b, :], p[:, :], [p.reg], [ybuf.reg])
                act(sq[:, b, :], p[:, :], AF.Square, [p.reg], [sq.reg])
            proj_fm(slot, 0, inner, 16, ev)
        norm_stats(sq, KC, 0, rstd[:, :], rstd.reg)
        for kc in range(KC):
            stt("dve", ybuf[:, kc, :], ybuf[:, kc, :], npost[:, li, kc:kc + 1], rstd[:, :],
                ALU.mult, ALU.mult, [ybuf.reg, npost.reg, rstd.reg], [ybuf.reg])
            tt("dve", xT[:, kc, :], xT[:, kc, :], ybuf[:, kc, :], ALU.add,
               [xT.reg, ybuf.reg], [xT.reg])

    GELU_C = 2.0 * math.sqrt(2.0 / math.pi)

    def even_layer(li, j):
        S.fence()
        pre_norm(li)
        hp = hpad
        for i in range(4):
            mset("pool", hs[i][:, 0:PAD], 0.0, [hs[i].reg])
        cp("pool", hp[:, :, 0:HALO], halo[j][:, :, :], [halo[j].reg], [hp.reg])
        for b in range(20):
            slot = W.get()
            sec, bb = divmod(b, 4)
            for m in range(2):
                oc = bb * 2 + m
                if sec == 0:
                    ev = lambda p, oc=oc: cp("act", a_in[:, oc, :], p[:, :], [p.reg], [a_in.reg])
                elif sec == 1:
                    ev = lambda p, oc=oc: act(saz[:, oc, :], p[:, :], AF.Silu, [p.reg], [saz.reg])
                elif sec == 2:
                    ev = lambda p, oc=oc: cp("dve", valb[:, oc, :], p[:, :], [p.reg], [valb.reg])
                elif sec == 3:
                    def ev(p, oc=oc):
                        act(tmpA[:, :], p[:, :], AF.Sigmoid, [p.reg], [tmpA.reg])
                        tt("dve", hp[:, oc, HALO:HALO + T], valb[:, oc, :], tmpA[:, :], ALU.mult,
                           [valb.reg, tmpA.reg], [hp.reg])
                else:
                    ev = lambda p, oc=oc: act(sbz[:, oc, :], p[:, :], AF.Silu, [p.reg], [sbz.reg])
                proj_fm(slot, m, un, KC, ev)
        cidx = 0
        cslot_ = None
        for kc in range(KC):
            p = psum()
            for k in range(CONV_K):
                if cidx % 16 == 0:
                    cslot_ = W.get()
                m = cidx % 16
                mm(p[:, :], wbf[:, cslot_, m * 128:(m + 1) * 128], hp[:, kc, k:k + T], k == 0, k == CONV_K - 1,
                   [wbf.slots[cslot_], hp.reg], [p.reg])
                cidx += 1
            act(acc[:, kc, :], p[:, :], AF.Identity, [p.reg, evec.reg], [acc.reg],
                bias=evec[:, j, 2, kc:kc + 1])
        cp("act", halo[j][:, :, :], hp[:, :, T:T + HALO], [hp.reg], [halo[j].reg])
        bslot = {}
        cslot = {}

        def bu(P):
            q = P // 4
            if P % 4 == 0:
                bslot[q] = W.get()
            sl = bslot[q]
            kc = P // 4
            pr, pi_ = psum(), psum()
            off = (P % 4) * 256
            mm(pr[:, :], wbf[:, sl, off:off + 128], a_in[:, kc, :], True, True,
               [wbf.slots[sl], a_in.reg], [pr.reg])
            mm(pi_[:, :], wbf[:, sl, off + 128:off + 256], a_in[:, kc, :], True, True,
               [wbf.slots[sl], a_in.reg], [pi_.reg])
            cp("act", hs[0][:, PAD:PAD + T], pr[:, :], [pr.reg], [hs[0].reg])
            cp("act", hs[1][:, PAD:PAD + T], pi_[:, :], [pi_.reg], [hs[1].reg])
            cp("act", hs[0][:, PAD - 1:PAD], carry[j][:, P, 0:1], [carry[j].reg], [hs[0].reg])
            cp("act", hs[1][:, PAD - 1:PAD], carry[j][:, P, 1:2], [carry[j].reg], [hs[1].reg])

        def scan(P):
            A = (hs[0], hs[1])
            B = (hs[2], hs[3])
            lo, hi = PAD - 1, PAD + T
            for k in range(9):
                d = 1 << k
                rsh, ish = A[0][:, lo - d:hi - d], A[1][:, lo - d:hi - d]
                rd = [A[0].reg, A[1].reg, pwr[j].reg, pwi[j].reg, npwi[j].reg]
                pr_, pi_, npi_ = pwr[j][:, k, P:P + 1], pwi[j][:, k, P:P + 1], npwi[j][:, k, P:P + 1]
                stt("dve", B[0][:, lo:hi], rsh, pr_, A[0][:, lo:hi], ALU.mult, ALU.add, rd, [B[0].reg])
                stt("dve", B[0][:, lo:hi], ish, npi_, B[0][:, lo:hi], ALU.mult, ALU.add,
                    rd + [B[0].reg], [B[0].reg])
                stt("dve", B[1][:, lo:hi], rsh, pi_, A[1][:, lo:hi], ALU.mult, ALU.add, rd, [B[1].reg])
                stt("dve", B[1][:, lo:hi], ish, pr_, B[1][:, lo:hi], ALU.mult, ALU.add,
                    rd + [B[1].reg], [B[1].reg])
                A, B = B, A
            cp("act", sbre[:, :], A[0][:, PAD:PAD + T], [A[0].reg], [sbre.reg])
            cp("act", sbim[:, :], A[1][:, PAD:PAD + T], [A[1].reg], [sbim.reg])
            cp("act", carry[j][:, P, 0:1], A[0][:, PAD + T - 1:PAD + T], [A[0].reg], [carry[j].reg])
            cp("act", carry[j][:, P, 1:2], A[1][:, PAD + T - 1:PAD + T], [A[1].reg], [carry[j].reg])

        ypsum = {}

        def yout(P):
            sl = bslot[P // 4]
            kc = P // 4
            if P % 4 == 0:
                ypsum[kc] = psum()
            yp = ypsum[kc]
            off = 1024 + (P % 4) * 256
            mm(yp[:, :], wbf[:, sl, off:off + 128], sbre[:, :], P % 4 == 0, False,
               [wbf.slots[sl], sbre.reg], [yp.reg])
            mm(yp[:, :], wbf[:, sl, off + 128:off + 256], sbim[:, :], False, P % 4 == 3,
               [wbf.slots[sl], sbim.reg], [yp.reg])
            if P % 4 == 3:
                stt("dve", tmpA[:, :], a_in[:, kc, :], evec[:, j, 0, kc:kc + 1], yp[:, :],
                    ALU.mult, ALU.add, [a_in.reg, evec.reg, yp.reg], [tmpA.reg])
                tt("dve", tmpB[:, :], tmpA[:, :], tmpA[:, :], ALU.mult, [tmpA.reg], [tmpB.reg])
                ts("dve", tmpB[:, :], tmpB[:, :], 0.044715, 1.0, ALU.mult, ALU.add, [tmpB.reg], [tmpB.reg])
                tt("dve", tmpB[:, :], tmpB[:, :], tmpA[:, :], ALU.mult, [tmpA.reg, tmpB.reg], [tmpB.reg])
                act(tmpB[:, :], tmpB[:, :], AF.Sigmoid, [tmpB.reg], [tmpB.reg], scale=GELU_C)
                tt("dve", ya[:, kc, :], tmpA[:, :], tmpB[:, :], ALU.mult, [tmpA.reg, tmpB.reg], [ya.reg])

        for P in range(32):
            bu(P)
            scan(P)
            yout(P)
        for b in range(4):
            slot = W.get()
            for m in range(2):
                oc = b * 2 + m

                def ev(p, oc=oc):
                    act(tmpA[:, :], p[:, :], AF.Sigmoid, [p.reg, evec.reg], [tmpA.reg],
                        bias=evec[:, j, 1, oc:oc + 1])
                    tt("dve", tmpA[:, :], tmpA[:, :], ya[:, oc, :], ALU.mult, [tmpA.reg, ya.reg], [tmpA.reg])
                    tt("dve", inner[:, oc, :], tmpA[:, :], saz[:, oc, :], ALU.mult,
                       [tmpA.reg, saz.reg], [inner.reg])
                proj_fm(slot, m, ya, KC, ev)
        for kc in range(KC):
            cp("act", accb[:, kc, :], acc[:, kc, :], [acc.reg], [accb.reg])
            act(sq[:, kc, :], acc[:, kc, :], AF.Square, [acc.reg], [sq.reg])
        pm = psum()
        for kc in range(KC):
            mm(pm[:, :], ones_m[:, 0, :], accb[:, kc, :], kc == 0, kc == KC - 1,
               [ones_m.reg, accb.reg], [pm.reg])
        cp("dve", mean[:, :], pm[:, :], [pm.reg], [mean.reg])
        p2 = psum()
        for kc in range(KC):
            mm(p2[:, :], ones_m[:, 0, :], sq[:, kc, :], kc == 0, kc == KC - 1,
               [ones_m.reg, sq.reg], [p2.reg])
        tt("dve", tmpA[:, :], mean[:, :], mean[:, :], ALU.mult, [mean.reg], [tmpA.reg])
        tt("dve", tmpA[:, :], p2[:, :], tmpA[:, :], ALU.subtract, [p2.reg, tmpA.reg], [tmpA.reg])
        rsqrt_eps(rstd[:, :], tmpA[:, :], [tmpA.reg], rstd.reg)
        for kc in range(KC):
            tt("dve", tmpB[:, :], acc[:, kc, :], mean[:, :], ALU.subtract, [acc.reg, mean.reg], [tmpB.reg])
            tt("dve", tmpB[:, :], tmpB[:, :], rstd[:, :], ALU.mult, [tmpB.reg, rstd.reg], [tmpB.reg])
            act(accb[:, kc, :], tmpB[:, :], AF.Silu, [tmpB.reg, evec.reg], [accb.reg],
                bias=evec[:, j, 4, kc:kc + 1], scale=evec[:, j, 3, kc:kc + 1])
        for b in range(4):
            slot = W.get()
            for m in range(2):
                oc = b * 2 + m

                def ev(p, oc=oc):
                    stt("dve", inner[:, 8 + oc, :], p[:, :], evec[:, j, 5, oc:oc + 1], sbz[:, oc, :],
                        ALU.add, ALU.mult, [p.reg, evec.reg, sbz.reg], [inner.reg])
                proj_fm(slot, m, accb, KC, ev)
        out_proj_and_residual(li)

    def ktok_ap(s, c0, n):
        return G[2][:, 2 * s + c0 // 512, c0 % 512:c0 % 512 + n]

    def vtok_ap(s, c0, n):
        return G[3 + s // 2][:, (s % 2) * 4 + c0 // 512, c0 % 512:c0 % 512 + n]

    def rT_ap(oc, sl=slice(0, T)):
        return G[5 + oc // 8][:, oc % 8, sl]

    vregs = [G[3].reg, G[4].reg]
    rregs = [G[5].reg, G[6].reg]

    def odd_layer(li, j, tile_idx):
        S.fence()
        pre_norm(li)
        mset("dve", lraug[:, :], 1.0, [lraug.reg])
        if tile_idx == 0:
            mset("pool", gs[:, :, :], 0.0, [gs.reg])
            mset("pool", gsb[:, :, :], 0.0, [gsb.reg])
        else:
            dma(bass.AP(gs.t, 0, [[8 * 512, 128], [1, 8 * 512]]), gstate_d[j, :, :],
                [gstate_reg[j]], [gs.reg], "g")
            for kc in range(8):
                cp("pool", gsb[:, kc, :], gs[:, kc, :], [gs.reg], [gsb.reg])
        if _DBG <= 1:
            return
        for b in range(24):
            slot = W.get()
            if b < 4:
                for m in range(2):
                    oc = b * 2 + m
                    proj_fm(slot, m, un, KC,
                            lambda p, oc=oc: cp("act", qT[:, oc, :], p[:, :], [p.reg], [qT.reg]))
            elif b < 8:
                bb = b - 4
                for m in range(2):
                    oc = bb * 2 + m
                    proj_fm(slot, m, un, KC,
                            lambda p, oc=oc: cp("act", kT[:, oc, :], p[:, :], [p.reg], [kT.reg]))
                for s in range(4):
                    p = psum()
                    for kc in range(KC):
                        mm(p[:, 0:256], un[:, kc, s * 128:(s + 1) * 128], wbf[:, slot, kc * 256:(kc + 1) * 256],
                           kc == 0, kc == KC - 1, [un.reg, wbf.slots[slot]], [p.reg])
                    cp("dve", ktok_ap(s, bb * 256, 256), p[:, 0:256], [p.reg], [G[2].reg])
            elif b < 16:
                bb = b - 8
                for s in range(4):
                    p = psum()
                    for kc in range(KC):
                        mm(p[:, 0:256], un[:, kc, s * 128:(s + 1) * 128], wbf[:, slot, kc * 256:(kc + 1) * 256],
                           kc == 0, kc == KC - 1, [un.reg, wbf.slots[slot]], [p.reg])
                    cp("act" if s % 2 else "dve", vtok_ap(s, bb * 256, 256), p[:, 0:256],
                       [p.reg], [G[3 + s // 2].reg])
            else:
                bb = b - 16
                for m in range(2):
                    oc = bb * 2 + m
                    proj_fm(slot, m, un, KC,
                            lambda p, oc=oc: act(rT_ap(oc), p[:, :], AF.Silu, [p.reg], [G[5 + oc // 8].reg]))
        if _DBG <= 2:
            return
        p = psum()
        for kc in range(KC):
            mm(p[0:16, :], wlr[:, j, kc, :], un[:, kc, :], kc == 0, kc == KC - 1,
               [wlr.reg, un.reg], [p.reg])
        cp("act", lraug[0:16, :], p[0:16, :], [p.reg], [lraug.reg])
        if _DBG <= 3:
            return
        for s in range(4):
            tsl = slice(s * 128, (s + 1) * 128)
            for h2 in range(2):
                p = psum()
                mm(p[:, :], lraug[:, tsl], wgaug[:, j, h2 * 512:(h2 + 1) * 512], True, True,
                   [lraug.reg, wgaug.reg], [p.reg])
                act(etmp[:, h2 * 512:(h2 + 1) * 512], p[:, :], AF.Exp, [p.reg], [etmp.reg], scale=-1.0)
            act(sptok[:, :], etmp[:, :], AF.Ln, [etmp.reg], [sptok.reg], bias=1.0)
            for g4 in range(2):
                p = psum()
                for c in range(4):
                    kc = g4 * 4 + c
                    mm(p[:, c * 128:(c + 1) * 128], sptok[:, kc * 128:(kc + 1) * 128], tri[:, 0, :],
                       True, True, [sptok.reg, tri.reg], [p.reg])
                ebv = bass.AP(eb.t, g4 * 512, [[KC * 128, 128], [1, 512]])
                eiv = bass.AP(einv.t, g4 * 512, [[KC * 128, 128], [1, 512]])
                act(ebv, p[:, :], AF.Exp, [p.reg], [eb.reg], scale=-1.0 / 16.0)
                act(eiv, p[:, :], AF.Exp, [p.reg], [einv.reg], scale=1.0 / 16.0)
            for kc in range(KC):
                stt("dve", qdec[:, kc, :], qT[:, kc, tsl], 1.0 / 16.0, eb[:, kc, :], ALU.mult, ALU.mult,
                    [qT.reg, eb.reg], [qdec.reg])
                tt("pool", kdec[:, kc, :], kT[:, kc, tsl], einv[:, kc, :], ALU.mult,
                   [kT.reg, einv.reg], [kdec.reg])
            for h2 in range(2):
                p = psum()
                mm(p[:, :], tri[:, 1, :], sptok[:, h2 * 512:(h2 + 1) * 512], True, True,
                   [tri.reg, sptok.reg], [p.reg])
                act(etmp[:, h2 * 512:(h2 + 1) * 512], p[:, :], AF.Exp, [p.reg], [etmp.reg], scale=-1.0 / 16.0)
            for h2 in range(2):
                tt("dve", k2[:, h2 * 512:(h2 + 1) * 512], ktok_ap(s, h2 * 512, 512),
                   etmp[:, h2 * 512:(h2 + 1) * 512], ALU.mult, [G[2].reg, etmp.reg], [k2.reg])
            if _DBG <= 4:
                continue
            pa = psum()
            for h in range(4):
                for c in range(2):
                    mm(pa[:, h * 128:(h + 1) * 128], kdec[:, 2 * h + c, :], qdec[:, 2 * h + c, :],
                       c == 0, c == 1, [kdec.reg, qdec.reg], [pa.reg])
            for h in range(4):
                tt("dve", attn[:, h, :], pa[:, h * 128:(h + 1) * 128], tri[:, 2, :], ALU.mult,
                   [pa.reg, tri.reg], [attn.reg])
            if _SUB <= 1:
                continue
            for h in range(4):
                po = psum()
                for vc in range(4):
                    vcol = h * 512 + vc * 128
                    mm(po[:, vc * 128:(vc + 1) * 128], vtok_ap(s, vcol, 128), attn[:, h, :],
                       True, "a" in _VAR, vregs + [attn.reg], [po.reg])
                    if "a" in _VAR:
                        continue
                    for c in range(2):
                        mm(po[:, vc * 128:(vc + 1) * 128], gsb[:, 2 * h + c, vc * 128:(vc + 1) * 128],
                           qdec[:, 2 * h + c, :], False, c == 1, [gsb.reg, qdec.reg], [po.reg])
                osb_v = bass.AP(osb.t, 0, [[512, 128], [1, 512]])
                osq_v = bass.AP(osq.t, 0, [[512, 128], [1, 512]])
                if "b" in _VAR:
                    continue
                cp("dve", osb_v, po[:, :], [po.reg], [osb.reg])
                if "c" in _VAR:
                    continue
                act(osq_v, osb_v, AF.Square, [osb.reg], [osq.reg])
                if _SUB <= 2:
                    continue
                pn = psum()
                for vc in range(4):
                    mm(pn[:, 0:128], ones_m[:, 1, :], osq[:, vc, :], vc == 0, vc == 3,
                       [ones_m.reg, osq.reg], [pn.reg])
                rsqrt_eps(orstd[:, :], pn[:, 0:128], [pn.reg], orstd.reg)
                for vc in range(4):
                    stt("dve", osb[:, vc, :], osb[:, vc, :], gng[:, j, vc:vc + 1], orstd[:, :],
                        ALU.mult, ALU.mult, [osb.reg, gng.reg, orstd.reg], [osb.reg])
                    tt("pool", inner[:, h * 4 + vc, tsl], osb[:, vc, :], rT_ap(h * 4 + vc, tsl), ALU.mult,
                       [osb.reg] + rregs, [inner.reg])
                if _SUB <= 3:
                    continue
                for c in range(2):
                    kc = 2 * h + c
                    pst = psum()
                    mm(pst[:, :], k2[:, kc * 128:(kc + 1) * 128], vtok_ap(s, h * 512, 512),
                       True, True, [k2.reg] + vregs, [pst.reg])
                    stt("dve", gs[:, kc, :], gs[:, kc, :], eb[:, kc, 127:128], pst[:, :],
                        ALU.mult, ALU.add, [gs.reg, eb.reg, pst.reg], [gs.reg])
                    cp("act", gsb[:, kc, :], gs[:, kc, :], [gs.reg], [gsb.reg])
        if _DBG <= 5:
            return
        dma(gstate_d[j, :, :], bass.AP(gs.t, 0, [[8 * 512, 128], [1, 8 * 512]]),
            [gs.reg], [gstate_reg[j]], "g")
        if _DBG <= 6:
            return
        out_proj_and_residual(li)

    for t in range(NT):
        ie = io = 0
        for kind in layers:
            if kind == "e":
                for b in range(EB):
                    W.push(ie * EB + b)
                ie += 1
            else:
                for b in range(32):
                    W.push(ne1 * EB + io * 32 + b)
                io += 1

    for t in range(NT):
        for kc in range(KC):
            dma(xT[:, kc, :], xT_d[:, kc, t * T:(t + 1) * T], [], [xT.reg], "x")
        ie = io = 0
        for li, kind in enumerate(layers):
            if kind == "e":
                even_layer(li, ie)
                ie += 1
            else:
                odd_layer(li, io, t)
                io += 1
        for kc in range(KC):
            dma(yT_d[:, kc, t * T:(t + 1) * T], xT[:, kc, :], [xT.reg], [], "y")

    S.emit(nc, ["y"])
    es.close()
    return nc


def _wblocks(w, cw):
    din, dout = w.shape
    kc = din // 128
    nb = dout // cw
    a = w.reshape(kc, 128, nb, cw).transpose(2, 1, 0, 3)
    return np.ascontiguousarray(a.reshape(nb, 128, kc * cw))


def _vec(v):
    return np.ascontiguousarray(v.reshape(-1, 128).T)


def _pack(inp, layers):
    f = np.float32
    nl = len(layers)
    even = [i for i, k in enumerate(layers) if k == "e"]
    odd = [i for i, k in enumerate(layers) if k == "o"]
    ne1, no1 = max(len(even), 1), max(len(odd), 1)
    out = {}
    out["npre"] = np.ascontiguousarray(np.stack([_vec(inp["norm_pre"][i]) for i in range(max(nl, 1))], 1)).astype(f)
    out["npost"] = np.ascontiguousarray(np.stack([_vec(inp["norm_post"][i]) for i in range(max(nl, 1))], 1)).astype(f)
    out["ident"] = np.eye(128, dtype=f)
    jj, tt_ = np.meshgrid(np.arange(128), np.arange(128), indexing="ij")
    tri = np.zeros((128, 3, 128), f)
    tri[:, 0, :] = (jj <= tt_)
    tri[:, 1, :] = (jj > tt_)
    tri[:, 2, :] = (jj <= tt_)
    out["tri"] = tri
    evec = np.zeros((128, ne1, 6, KC), f)
    convw = np.zeros((128, ne1, KC, CONV_K), f)
    s5p = np.zeros((128, ne1, 3, 32), f)
    cpad = np.zeros((ne1, 128, 32, 2, 128), f)
    bpad = np.zeros((ne1, 8, 128, 4, 2, 128), f)
    w_in_e = np.zeros((ne1, 20, 128, 2048), f)
    w_glu = np.zeros((ne1, 4, 128, 2048), f)
    w_pw = np.zeros((ne1, 4, 128, 2048), f)
    w_out_e = np.zeros((ne1, 8, 128, 2048), f)
    for j in range(len(even)):
        for k, nm in enumerate(["s5_d", "s5_b_glu", "conv_b", "conv_ln_g", "conv_ln_b", "conv_b_pw"]):
            evec[:, j, k, :] = _vec(inp[nm][j])
        convw[:, j, :, :] = inp["conv_w"][j].T.reshape(KC, 128, CONV_K).transpose(1, 0, 2)
        for a in range(2):
            s5p[64 * a:64 * a + 64, j, 0, :] = inp["s5_lambda_re"][j][a::2, :].T
            s5p[64 * a:64 * a + 64, j, 1, :] = inp["s5_lambda_im"][j][a::2, :].T
            s5p[64 * a:64 * a + 64, j, 2, :] = inp["s5_log_dt"][j][a::2][None, :]
        for P in range(32):
            r = P % 4
            for a in range(2):
                g = 2 * P + a
                for ri, (bn, cn) in enumerate([("s5_b_re", "s5_c_re"), ("s5_b_im", "s5_c_im")]):
                    bpad[j, P // 4, 32 * r + 16 * a:32 * r + 16 * a + 16, P % 4, ri, 64 * a:64 * a + 64] = \
                        inp[bn][j, g].T
                    cpad[j, 64 * a:64 * a + 64, P, ri, 32 * r + 16 * a:32 * r + 16 * a + 16] = \
                        inp[cn][j, g].T
        w_in_e[j] = _wblocks(inp["ev_w_in"][j], 256)
        w_glu[j] = _wblocks(inp["s5_w_glu"][j], 256)
        w_pw[j] = _wblocks(inp["conv_w_pw"][j], 256)
        w_out_e[j] = _wblocks(inp["ev_w_out"][j], 128)
    out.update(evec=evec, convw=convw, s5p=s5p, cpad=cpad.reshape(ne1, 128, 32 * 2 * 128),
               bpad=bpad.reshape(ne1, 8, 128, 1024), w_in_e=w_in_e, w_glu=w_glu, w_pw=w_pw,
               w_out_e=w_out_e)
    w_in_o = np.zeros((no1, 24, 128, 2048), f)
    w_out_o = np.zeros((no1, 8, 128, 2048), f)
    wlr = np.zeros((128, no1, KC, 16), f)
    wgaug = np.zeros((32, no1, 1024), f)
    gng = np.zeros((128, no1, 4), f)
    for j in range(len(odd)):
        w = inp["od_w_in"][j]
        w_in_o[j] = _wblocks(np.ascontiguousarray(w[:, :6144]), 256)
        wlr[:, j, :, :] = w[:, 6144:6160].reshape(KC, 128, 16).transpose(1, 0, 2)
        wgaug[0:16, j, :] = inp["gla_w_gate_up"][j]
        wgaug[16, j, :] = inp["gla_b_gate"][j]
        gng[:, j, :] = _vec(inp["gla_norm_g"][j])
        w_out_o[j] = _wblocks(inp["od_w_out"][j], 128)
    out.update(w_in_o=w_in_o, w_out_o=w_out_o, wlr=wlr, wgaug=wgaug, gng=gng)
    return out


_LAYERS = ["e", "o", "e", "o"]


def run_layers(inp, layers, n_cores=8):
    inp = {k: np.asarray(v, dtype=np.float32) for k, v in inp.items()}
    x = inp["x"]
    B, L, _ = x.shape
    shared = _pack(inp, layers)
    nc = build_program(L, layers)
    in_maps = []
    for c in range(n_cores):
        b = c % B
        m = dict(shared)
        m["xT"] = np.ascontiguousarray(x[b].T.reshape(KC, 128, L).transpose(1, 0, 2))
        in_maps.append(m)
    res = run_bass_kernel_spmd(nc, in_maps, core_ids=list(range(n_cores)))
    out = np.empty((B, L, D), np.float32)
    for b in range(B):
        yT = np.asarray(res.results[b]["yT"])
        out[b] = yT.transpose(1, 0, 2).reshape(D, L).T
    return out


def kernel(**inputs):
    return run_layers(inputs, _LAYERS)
```
